# Optimizing a Trainium2 kernel written in Bass

```python
import jax
import jax.numpy as jnp
from jax import lax
import numpy as np

D_MODEL = 1024
BATCH = 4
SEQ = 8192
DEPTH = 1

ML_HEADS = 4
ML_HEAD_DIM = 128
ML_WIDTH = ML_HEADS * ML_HEAD_DIM
ML_CHUNK = 128
CONV_WIDTH = 4
ATT_Q_HEADS = 8
ATT_KV_HEADS = 2
ATT_HEAD_DIM = 64
ATT_WIDTH = ATT_Q_HEADS * ATT_HEAD_DIM
ATT_KV_WIDTH = ATT_KV_HEADS * ATT_HEAD_DIM
WINDOW = 128
ROPE_THETA = 10000.0
D_MIX = ML_WIDTH + ATT_WIDTH
COL_SIZES = (2 * ML_WIDTH, ML_WIDTH, ML_WIDTH, ML_HEADS, ML_HEADS, ATT_WIDTH, ATT_KV_WIDTH, ATT_KV_WIDTH)
SPLIT_POINTS = tuple(sum(COL_SIZES[:i + 1]) for i in range(len(COL_SIZES) - 1))
D_IN_PROJ = sum(COL_SIZES)
N_GROUPS = 4
EXPERTS_PER_GROUP = 8
N_EXPERTS = N_GROUPS * EXPERTS_PER_GROUP
TOP_K_INNER = 2
D_EXPERT = 256
LN_EPS = 1e-5
ALPHA = (2.0 * DEPTH) ** 0.25
BETA = (8.0 * DEPTH) ** -0.25

kernel_name = "hymba_mlstm_swa_hiermoe_deepnorm"


def layer_norm(x, w, b):
    xf = x.astype(jnp.float32)
    mu = jnp.mean(xf, axis=-1, keepdims=True)
    var = jnp.mean(jnp.square(xf - mu), axis=-1, keepdims=True)
    return ((xf - mu) * lax.rsqrt(var + LN_EPS) * w + b).astype(x.dtype)


def causal_depthwise_conv(u, w, b):
    S = u.shape[1]
    up = jnp.pad(u, ((0, 0), (CONV_WIDTH - 1, 0), (0, 0)))
    out = b
    for j in range(CONV_WIDTH):
        out = out + w[j] * up[:, j:j + S]
    return out


def _to_chunks(a):
    B, S, H = a.shape[:3]
    a = a.reshape((B, S // ML_CHUNK, ML_CHUNK, H) + a.shape[3:])
    perm = (1, 0, 3, 2) + tuple(range(4, a.ndim))
    return a.transpose(perm)


def mlstm_chunkwise(q, k, v, i_pre, f_pre):
    B, S, H, D = q.shape
    L = ML_CHUNK
    f32 = jnp.float32
    qc = _to_chunks(q.astype(f32))
    kc = _to_chunks(k.astype(f32) * (D ** -0.5))
    vc = _to_chunks(v.astype(f32))
    ic = _to_chunks(i_pre.astype(f32))
    lfc = _to_chunks(jax.nn.log_sigmoid(f_pre.astype(f32)))
    causal = jnp.tril(jnp.ones((L, L), dtype=bool))

    def step(carry, xs):
        C, n, m = carry
        q_, k_, v_, i_, lf_ = xs
        b = jnp.cumsum(lf_, axis=-1)
        log_intra = jnp.where(causal, b[..., :, None] - b[..., None, :] + i_[..., None, :], -jnp.inf)
        log_inter = b + m[..., None]
        m_row = jnp.maximum(log_inter, jnp.max(log_intra, axis=-1))
        w_intra = jnp.exp(log_intra - m_row[..., None])
        w_inter = jnp.exp(log_inter - m_row)
        s = jnp.einsum("bhld,bhsd->bhls", q_, k_) * w_intra
        num = (w_inter[..., None] * jnp.einsum("bhed,bhld->bhle", C, q_)
               + jnp.einsum("bhls,bhse->bhle", s, v_))
        den = w_inter * jnp.einsum("bhd,bhld->bhl", n, q_) + jnp.sum(s, axis=-1)
        h = num / jnp.maximum(jnp.abs(den), jnp.exp(-m_row))[..., None]
        b_last = b[..., -1]
        log_w = b_last[..., None] - b + i_
        m_new = jnp.maximum(b_last + m, jnp.max(log_w, axis=-1))
        decay = jnp.exp(b_last + m - m_new)
        w = jnp.exp(log_w - m_new[..., None])
        C_new = decay[..., None, None] * C + jnp.einsum("bhs,bhse,bhsd->bhed", w, v_, k_)
        n_new = decay[..., None] * n + jnp.einsum("bhs,bhsd->bhd", w, k_)
        return (C_new, n_new, m_new), h

    init = (jnp.zeros((B, H, D, D), f32), jnp.zeros((B, H, D), f32), jnp.zeros((B, H), f32))
    _, hc = lax.scan(step, init, (qc, kc, vc, ic, lfc))
    return hc.transpose(1, 0, 3, 2, 4).reshape(B, S, H, D)


def rope(x):
    S, D = x.shape[1], x.shape[3]
    half = D // 2
    inv_freq = ROPE_THETA ** (-jnp.arange(half, dtype=jnp.float32) / half)
    ang = jnp.arange(S, dtype=jnp.float32)[:, None] * inv_freq[None, :]
    cos = jnp.cos(ang)[None, :, None, :]
    sin = jnp.sin(ang)[None, :, None, :]
    xf = x.astype(jnp.float32)
    x1, x2 = xf[..., :half], xf[..., half:]
    return jnp.concatenate([x1 * cos - x2 * sin, x1 * sin + x2 * cos], axis=-1).astype(x.dtype)


def swa_with_sinks(q, k, v, sinks):
    B, S, HQ, D = q.shape
    HKV = k.shape[2]
    G = HQ // HKV
    Lb = WINDOW
    NB = S // Lb
    qb = q.reshape(B, NB, Lb, HKV, G, D)
    kb = k.reshape(B, NB, Lb, HKV, D)
    vb = v.reshape(B, NB, Lb, HKV, D)
    pad = ((0, 0), (1, 0), (0, 0), (0, 0), (0, 0))
    kk = jnp.concatenate([jnp.pad(kb, pad)[:, :-1], kb], axis=2)
    vv = jnp.concatenate([jnp.pad(vb, pad)[:, :-1], vb], axis=2)
    scores = jnp.einsum("bnqhgd,bnkhd->bnhgqk", qb, kk).astype(jnp.float32) * (D ** -0.5)
    qi = jnp.arange(Lb)[:, None]
    kj = jnp.arange(2 * Lb)[None, :]
    diff = Lb + qi - kj
    band = (diff >= 0) & (diff < WINDOW)
    blk = jnp.arange(NB)[:, None, None]
    valid = band[None] & ((blk * Lb + kj[None] - Lb) >= 0)
    scores = jnp.where(valid[None, :, None, None], scores, -jnp.inf)
    sink = jnp.broadcast_to(sinks.astype(jnp.float32).reshape(1, 1, HKV, G, 1, 1),
                            scores.shape[:-1] + (1,))
    probs = jax.nn.softmax(jnp.concatenate([scores, sink], axis=-1), axis=-1)[..., :-1]
    out = jnp.einsum("bnhgqk,bnkhd->bnqhgd", probs.astype(v.dtype), vv)
    return out.reshape(B, S, HQ * D)


def hybrid_mixer(x, w_in, conv_w, conv_b, gate_bias, norm_w, sinks, w_out):
    B, S, _ = x.shape
    proj = x @ w_in
    ml_qk, ml_v, ml_o, ml_i, ml_f, a_q, a_k, a_v = jnp.split(proj, SPLIT_POINTS, axis=-1)
    ml_qk = jax.nn.silu(causal_depthwise_conv(ml_qk, conv_w, conv_b))
    ml_q, ml_k = jnp.split(ml_qk, 2, axis=-1)
    hd = (B, S, ML_HEADS, ML_HEAD_DIM)
    h = mlstm_chunkwise(ml_q.reshape(hd), ml_k.reshape(hd), ml_v.reshape(hd),
                        ml_i + gate_bias[0], ml_f + gate_bias[1])
    mu = jnp.mean(h, axis=-1, keepdims=True)
    var = jnp.mean(jnp.square(h - mu), axis=-1, keepdims=True)
    h = ((h - mu) * lax.rsqrt(var + LN_EPS)).reshape(B, S, ML_WIDTH) * norm_w
    ml_out = (jax.nn.sigmoid(ml_o.astype(jnp.float32)) * h).astype(x.dtype)
    q = rope(a_q.reshape(B, S, ATT_Q_HEADS, ATT_HEAD_DIM))
    k = rope(a_k.reshape(B, S, ATT_KV_HEADS, ATT_HEAD_DIM))
    v = a_v.reshape(B, S, ATT_KV_HEADS, ATT_HEAD_DIM)
    att_out = swa_with_sinks(q, k, v, sinks)
    return jnp.concatenate([ml_out, att_out], axis=-1) @ w_out


def hierarchical_moe(x, w_group_router, b_group_router, w_expert_router, b_expert_router,
                     w_exp_gate, w_exp_up, w_exp_down):
    B, S, Dm = x.shape
    xt = x.reshape(B * S, Dm)
    g_logits = (xt @ w_group_router + b_group_router).astype(jnp.float32)
    g_prob = jax.nn.softmax(g_logits, axis=-1)
    g_idx = jnp.argmax(g_logits, axis=-1)
    g_p = jnp.take_along_axis(g_prob, g_idx[:, None], axis=-1)[:, 0]
    e_logits = (xt @ w_expert_router + b_expert_router).astype(jnp.float32)
    e_logits = e_logits.reshape(-1, N_GROUPS, EXPERTS_PER_GROUP)
    e_in = jnp.take_along_axis(e_logits, g_idx[:, None, None], axis=1)[:, 0]
    top_v, top_i = lax.top_k(e_in, TOP_K_INNER)
    top_w = jax.nn.softmax(top_v, axis=-1) * g_p[:, None]
    expert_id = g_idx[:, None] * EXPERTS_PER_GROUP + top_i
    combine = jnp.sum(jax.nn.one_hot(expert_id, N_EXPERTS, dtype=jnp.float32) * top_w[..., None],
                      axis=1).astype(x.dtype)
    out = jnp.zeros_like(xt)
    for e in range(N_EXPERTS):
        h = jax.nn.silu(xt @ w_exp_gate[e]) * (xt @ w_exp_up[e])
        out = out + combine[:, e:e + 1] * (h @ w_exp_down[e])
    return out.reshape(B, S, Dm)


def setup_inputs(seed: int = 0) -> dict:
    key = jax.random.key(seed)
    ks = jax.random.split(key, 21)
    f32 = jnp.float32

    def nrm(k, shape, scale):
        return jax.random.normal(k, shape, f32) * scale

    x = nrm(ks[0], (BATCH, SEQ, D_MODEL), 1.0)
    w_in = nrm(ks[1], (DEPTH, D_MODEL, D_IN_PROJ), D_MODEL ** -0.5)
    conv_w = nrm(ks[2], (DEPTH, CONV_WIDTH, 2 * ML_WIDTH), CONV_WIDTH ** -0.5)
    conv_b = nrm(ks[3], (DEPTH, 2 * ML_WIDTH), 0.02)
    i_bias = nrm(ks[4], (DEPTH, ML_HEADS), 0.1)
    f_bias = jnp.linspace(3.0, 6.0, ML_HEADS, dtype=f32)[None, :] + nrm(ks[5], (DEPTH, ML_HEADS), 0.1)
    mlstm_gate_bias = jnp.stack([i_bias, f_bias], axis=1)
    mlstm_norm_w = 1.0 + nrm(ks[6], (DEPTH, ML_WIDTH), 0.02)
    attn_sinks = nrm(ks[7], (DEPTH, ATT_Q_HEADS), 0.5)
    w_out = nrm(ks[8], (DEPTH, D_MIX, D_MODEL), (D_MIX ** -0.5) * BETA)
    ln1_w = 1.0 + nrm(ks[9], (DEPTH, D_MODEL), 0.02)
    ln1_b = nrm(ks[10], (DEPTH, D_MODEL), 0.02)
    w_group_router = nrm(ks[11], (DEPTH, D_MODEL, N_GROUPS), D_MODEL ** -0.5)
    b_group_router = nrm(ks[12], (DEPTH, N_GROUPS), 0.01)
    w_expert_router = nrm(ks[13], (DEPTH, D_MODEL, N_EXPERTS), D_MODEL ** -0.5)
    b_expert_router = nrm(ks[14], (DEPTH, N_EXPERTS), 0.01)
    w_exp_gate = nrm(ks[15], (DEPTH, N_EXPERTS, D_MODEL, D_EXPERT), D_MODEL ** -0.5)
    w_exp_up = nrm(ks[16], (DEPTH, N_EXPERTS, D_MODEL, D_EXPERT), D_MODEL ** -0.5)
    w_exp_down = nrm(ks[17], (DEPTH, N_EXPERTS, D_EXPERT, D_MODEL), (D_EXPERT ** -0.5) * BETA)
    ln2_w = 1.0 + nrm(ks[18], (DEPTH, D_MODEL), 0.02)
    ln2_b = nrm(ks[19], (DEPTH, D_MODEL), 0.02)
    return {"x": x, "w_in": w_in, "conv_w": conv_w, "conv_b": conv_b,
            "mlstm_gate_bias": mlstm_gate_bias, "mlstm_norm_w": mlstm_norm_w,
            "attn_sinks": attn_sinks, "w_out": w_out, "ln1_w": ln1_w, "ln1_b": ln1_b,
            "w_group_router": w_group_router, "b_group_router": b_group_router,
            "w_expert_router": w_expert_router, "b_expert_router": b_expert_router,
            "w_exp_gate": w_exp_gate, "w_exp_up": w_exp_up, "w_exp_down": w_exp_down,
            "ln2_w": ln2_w, "ln2_b": ln2_b}


def reference(x, w_in, conv_w, conv_b, mlstm_gate_bias, mlstm_norm_w, attn_sinks, w_out,
              ln1_w, ln1_b, w_group_router, b_group_router, w_expert_router, b_expert_router,
              w_exp_gate, w_exp_up, w_exp_down, ln2_w, ln2_b):
    for l in range(DEPTH):
        y = hybrid_mixer(x, w_in[l], conv_w[l], conv_b[l], mlstm_gate_bias[l], mlstm_norm_w[l],
                         attn_sinks[l], w_out[l])
        x = layer_norm(ALPHA * x + y, ln1_w[l], ln1_b[l])
        y = hierarchical_moe(x, w_group_router[l], b_group_router[l], w_expert_router[l],
                             b_expert_router[l], w_exp_gate[l], w_exp_up[l], w_exp_down[l])
        x = layer_norm(ALPHA * x + y, ln2_w[l], ln2_b[l])
    return x
```

```python
import os
import types
from contextlib import ExitStack

import numpy as np
import concourse.bass as bass
import concourse.mybir as mybir
from concourse.bass_utils import run_bass_kernel_spmd

F32 = mybir.dt.float32
BF16 = mybir.dt.bfloat16
AF = mybir.ActivationFunctionType
ALU = mybir.AluOpType
AX = mybir.AxisListType

NCORES = 8
D = 1024
T = 4096
NT = T // 128
NE = 32
DE = 256
ALPHA = 2.0 ** 0.25
LN_EPS = 1e-5
TP = 1024
NPASS = T // TP
TPT = TP // 128
GP = TP // 512

ENGS = ("pe", "act", "dve", "pool", "sp")


def _freeze(fn):
    if fn.__closure__ is None:
        return fn
    cells = []
    for c in fn.__closure__:
        try:
            cells.append(types.CellType(c.cell_contents))
        except ValueError:
            cells.append(c)
    return types.FunctionType(fn.__code__, fn.__globals__, fn.__name__, fn.__defaults__, tuple(cells))


class _Probe:
    def __init__(self):
        self.n = 512
        self.passes = 1

    def __getattr__(self, name):
        def f(*a, **k):
            if name == "then_inc":
                return self
            ap = k.get("in_") if name == "bn_stats" else k.get("out", a[0] if a else None)
            try:
                sz = 1
                for d in ap.shape[1:]:
                    sz *= d
                self.n = sz
            except Exception:
                self.n = 512
            if name == "matmul":
                lhs = k.get("lhsT", a[1] if len(a) > 1 else None)
                try:
                    if lhs.dtype == F32:
                        self.passes = 4
                except Exception:
                    pass
            return self
        return f


class Buf:
    __slots__ = ("name", "w", "r", "const")

    def __init__(self, name, const=False):
        self.name = name
        self.w = None
        self.r = []
        self.const = const


class Prog:
    LAT = 120.0

    def __init__(self, nc, stack):
        self.nc = nc
        self.stack = stack
        self.nodes = []
        self.fence = {}
        self.since_fence = []

    def _deps(self, engine, reads, writes, extra):
        deps = set()
        for d in extra:
            if d is not None:
                deps.add(d)
        for b in reads:
            if b.w is not None:
                deps.add(b.w)
        for b in writes:
            if b.w is not None:
                deps.add(b.w)
            deps.update(b.r)
        if engine in self.fence:
            deps.add(self.fence[engine])
        return deps

    def _mark(self, nid, reads, writes):
        for b in reads:
            if not b.const:
                b.r.append(nid)
        for b in writes:
            b.w = nid
            b.r = []

    def _add(self, node, reads, writes, extra):
        nid = len(self.nodes)
        node["id"] = nid
        node["deps"] = self._deps(node["engine"], reads, writes, extra)
        self.nodes.append(node)
        self.since_fence.append(nid)
        self._mark(nid, reads, writes)
        return nid

    def op(self, engine, fn, reads=(), writes=(), extra=(), n=None):
        fns = [_freeze(f) for f in (fn if isinstance(fn, (list, tuple)) else [fn])]
        dur = 0.0
        for f in fns:
            pr = _Probe()
            f(pr)
            sz = pr.n if n is None else n
            if engine == "pe":
                dur += 25.0 + sz * pr.passes / 2.35
            elif engine == "pool":
                dur += 300.0 + sz * 1.7
            elif engine == "act":
                dur += 200.0 + sz * 0.9
            else:
                dur += 150.0 + sz * 1.25
        return self._add(dict(engine=engine, kind="op", fns=fns, dur=dur), reads, writes, extra)

    def dma(self, engine, out, in_, reads=(), writes=(), key=None, extra=(), nbytes=1 << 20):
        if key is None:
            key = (writes[0].name if (writes and not writes[0].name.startswith("dram:")) else reads[0].name + "_st")
        return self._add(dict(engine=engine, kind="dma", out=out, in_=in_, key=key, dur=2000.0 + nbytes / 150.0), reads, writes, extra)

    def wait(self, engine, toks):
        return self._add(dict(engine=engine, kind="wait", dur=50.0), (), (), toks)

    def barrier(self):
        prev = list(self.since_fence)
        ids = {}
        for e in ENGS:
            ids[e] = self._add(dict(engine=e, kind="wait", dur=50.0), (), (), prev)
        self.fence = ids
        self.since_fence = list(ids.values())

    def _schedule(self):
        import heapq
        nodes = self.nodes
        N = len(nodes)
        children = [[] for _ in range(N)]
        indeg = [0] * N
        for nd in nodes:
            indeg[nd["id"]] = len(nd["deps"])
            for d in nd["deps"]:
                children[d].append(nd["id"])
        finish = [0.0] * N
        free = {e: 0.0 for e in ENGS}
        blevel = [0.0] * N
        for nid in range(N - 1, -1, -1):
            m = 0.0
            for c in children[nid]:
                v = blevel[c] + self.LAT
                if v > m:
                    m = v
            blevel[nid] = m + nodes[nid]["dur"]
        PRIO = "cp"
        key_of = (lambda nid: (-blevel[nid], nid)) if PRIO == "cp" else (lambda nid: (nid, nid))
        byt = {e: [] for e in ENGS}
        byi = {e: [] for e in ENGS}
        order = {e: [] for e in ENGS}

        def push(nid):
            nd = nodes[nid]
            e = nd["engine"]
            rt = 0.0
            for d in nd["deps"]:
                f = finish[d] + (0.0 if nodes[d]["engine"] == e and nodes[d]["kind"] != "dma" else self.LAT)
                if f > rt:
                    rt = f
            heapq.heappush(byt[e], (rt, nid))

        for nd in nodes:
            if indeg[nd["id"]] == 0:
                push(nd["id"])
        done = 0
        while done < N:
            best = None
            for e in ENGS:
                while byt[e] and byt[e][0][0] <= free[e]:
                    rt, nid = heapq.heappop(byt[e])
                    heapq.heappush(byi[e], key_of(nid))
                if byi[e]:
                    cand = (free[e], byi[e][0][1], e, True)
                elif byt[e]:
                    cand = (byt[e][0][0], byt[e][0][1], e, False)
                else:
                    continue
                if best is None or cand[:2] < best[:2]:
                    best = cand
            start, nid, e, from_i = best
            if from_i:
                heapq.heappop(byi[e])
            else:
                heapq.heappop(byt[e])
            nd = nodes[nid]
            if nd["kind"] == "dma":
                free[e] = start + 70.0
                finish[nid] = start + nd["dur"]
            else:
                free[e] = start + nd["dur"]
                finish[nid] = free[e]
            order[e].append(nid)
            done += 1
            for c in children[nid]:
                indeg[c] -= 1
                if indeg[c] == 0:
                    push(c)
        self.est_total = max(finish) if N else 0.0
        return order

    def run(self):
        nodes = self.nodes
        order = self._schedule()
        sem = {e: self.stack.enter_context(self.nc.semaphore("s_" + e)) for e in ENGS}
        dsem, dcnt, dq = {}, {}, {}
        tok = [None] * len(nodes)
        for e in ENGS:
            c = 0
            for nid in order[e]:
                nd = nodes[nid]
                if nd["kind"] == "op":
                    c += 1
                    tok[nid] = (e, c)
                elif nd["kind"] == "dma":
                    k = nd["key"]
                    if k not in dsem:
                        dsem[k] = self.stack.enter_context(self.nc.semaphore("d_" + k.replace(":", "_")))
                        dcnt[k] = 0
                        dq[k] = e
                    assert dq[k] == e, f"dma key {k} used from two queues"
                    dcnt[k] += 16
                    tok[nid] = (k, dcnt[k])
        def expand(nid, acc, seen):
            for d in nodes[nid]["deps"]:
                if d in seen:
                    continue
                seen.add(d)
                if nodes[d]["kind"] == "wait":
                    expand(d, acc, seen)
                else:
                    acc.append(d)
        wait_closure = {}
        for nd in nodes:
            if nd["kind"] == "wait":
                acc = []
                expand(nd["id"], acc, set())
                best = {}
                for d in acc:
                    k, c = tok[d]
                    if best.get(k, 0) < c:
                        best[k] = c
                wait_closure[nd["id"]] = best

        def semof(k):
            return sem[k] if k in sem else dsem[k]

        streams = {}
        for e in ENGS:
            waited = {}
            items = []
            for nid in order[e]:
                nd = nodes[nid]
                need = {}
                for d in nd["deps"]:
                    if nodes[d]["kind"] == "wait":
                        for k, c in wait_closure[d].items():
                            if need.get(k, 0) < c:
                                need[k] = c
                    else:
                        k, c = tok[d]
                        if need.get(k, 0) < c:
                            need[k] = c
                waits = []
                for k, c in need.items():
                    if k == "pe" and e == "pe":
                        continue
                    if waited.get(k, 0) >= c:
                        continue
                    waited[k] = c
                    waits.append((k, c))
                items.append((nd, waits))
            streams[e] = items

        def play(e, eng):
            for nd, waits in streams[e]:
                for (k, c) in waits:
                    eng.wait_ge(semof(k), c)
                if nd["kind"] == "op":
                    fns = nd["fns"]
                    for f in fns[:-1]:
                        f(eng)
                    fns[-1](eng).then_inc(sem[e], 1)
                elif nd["kind"] == "dma":
                    eng.dma_start(out=nd["out"], in_=nd["in_"]).then_inc(dsem[nd["key"]], 16)

        with self.nc.Block() as block:
            @block.tensor
            def _(eng):
                play("pe", eng)

            @block.scalar
            def _(eng):
                play("act", eng)

            @block.vector
            def _(eng):
                play("dve", eng)

            @block.gpsimd
            def _(eng):
                play("pool", eng)

            @block.sync
            def _(eng):
                play("sp", eng)


class Ctx:
    pass


def phase2(nc, P, C, st):
    sb = lambda n, s, d: st.enter_context(nc.sbuf_tensor("sb_" + n, s, d))
    NBUF = 2 if NPASS > 2 else 1
    x1T_2 = [sb(f"x1T{i}", [128, 8, TP], BF16) for i in range(NBUF)]
    acc_2 = [sb(f"acc{i}", [128, TPT, D], F32) for i in range(NBUF)]
    NW = 4 if NPASS > 2 else 3
    wg = [sb(f"wg{i}", [128, 8, DE], BF16) for i in range(NW)]
    wu = [sb(f"wu{i}", [128, 8, DE], BF16) for i in range(NW)]
    wd = [sb(f"wd{i}", [128, 2, D], BF16) for i in range(NW)]
    x1t = [sb(f"x1t{i}", [128, D], F32) for i in range(2)]
    x1b = [sb(f"x1b{i}", [128, D], BF16) for i in range(2)]
    hT = [[sb(f"hT{i}{j}", [128, 512], BF16) for j in range(2)] for i in range(2)]
    sg = [sb(f"sg{i}", [128, 512], BF16) for i in range(3)]
    comb_2 = [sb(f"comb{i}", [128, TPT, NE], F32) for i in range(NBUF)]
    ln2w = sb("ln2w", [128, D], F32)
    ln2b = sb("ln2b", [128, D], F32)
    wr = sb("wr", [128, 8, 36], BF16)
    rb = sb("rb", [128, 36], F32)
    ident = sb("ident2", [128, 128], BF16)
    rt4 = [sb(f"rtr{i}", [128, 528], F32) for i in range(2)]
    ot = [sb(f"ot{i}", [128, D], F32) for i in range(2)]
    stats_2 = [sb(f"stats2{i}", [128, 2, 6], F32) for i in range(2)]
    mv_2 = [sb(f"mv2{i}", [128, 4], F32) for i in range(2)]
    ps = [st.enter_context(nc.psum_tensor(f"ps2_{i}", [128, 512], F32)) for i in range(8)]
    B_ps = [Buf(f"ps2_{i}") for i in range(8)]

    B_x1T_2 = [[Buf(f"x1T_{i}_{t}") for t in range(TPT)] for i in range(NBUF)]
    B_acc_2 = [[Buf(f"acc_{i}_{t}") for t in range(TPT)] for i in range(NBUF)]
    B_w = [Buf(f"w2_{i}") for i in range(NW)]
    B_x1t = [Buf(f"x1t_{i}") for i in range(2)]
    B_x1b = [Buf(f"x1b_{i}") for i in range(2)]
    B_hT = [[Buf(f"hT_{i}{j}") for j in range(2)] for i in range(2)]
    B_sg = [Buf(f"sg_{i}") for i in range(3)]
    B_comb_2 = [[Buf(f"comb_{i}_{t}") for t in range(TPT)] for i in range(NBUF)]
    B_c = Buf("const2", const=True)
    B_rt4 = [Buf(f"rt_{i}") for i in range(2)]
    B_ot = [Buf(f"ot_{i}") for i in range(2)]
    B_st2 = [Buf(f"stats2_{i}") for i in range(2)]

    ctoks = []
    ctoks.append(P.dma("sp", ln2w[:], C.ln2w, writes=[B_c], key="const2"))
    ctoks.append(P.dma("sp", ln2b[:], C.ln2b, writes=[B_c], key="const2"))
    ctoks.append(P.dma("sp", rb[:], C.rb, writes=[B_c], key="const2"))
    ctoks.append(P.dma("pool", wr[:], C.wr.rearrange("(c p) n -> p c n", p=128), writes=[B_c], key="const2p"))
    ctoks.append(P.dma("pool", ident[:], C.ident, writes=[B_c], key="const2p"))
    c_all = [ctoks[2], ctoks[4]]

    def load_expert(q):
        s = q % NW
        e = q % NE
        P.dma("pool", wg[s][:], C.weg[e].rearrange("(c p) n -> p c n", p=128), writes=[B_w[s]])
        P.dma("pool", wu[s][:], C.weu[e].rearrange("(c p) n -> p c n", p=128), writes=[B_w[s]])
        P.dma("pool", wd[s][:], C.wed[e].rearrange("(c p) n -> p c n", p=128), writes=[B_w[s]])

    LG, OHG, EIN, E2, OH1, OH2, CG = 0, 36, 40, 48, 56, 64, 72
    EX4 = 80
    SC = 96


    load_expert(0)
    load_expert(1)
    for p in range(NPASS):
        x1T, acc, comb = x1T_2[p % NBUF], acc_2[p % NBUF], comb_2[p % NBUF]
        B_x1T, B_acc, B_comb = B_x1T_2[p % NBUF], B_acc_2[p % NBUF], B_comb_2[p % NBUF]
        for t in range(TPT):
            tg = p * TPT + t
            s2 = t % 2
            P.dma("sp", x1t[s2][:], C.x1s[tg * 128:(tg + 1) * 128, :], reads=[C.B_x1s[tg]], writes=[B_x1t[s2]])
            P.op("act", lambda e, s2=s2, t=t: e.activation(out=acc[:, t, :], in_=x1t[s2][:], func=AF.Copy, scale=ALPHA),
                 reads=[B_x1t[s2]], writes=[B_acc[t]])
            P.op("act", lambda e, s2=s2: e.activation(out=x1b[s2][:], in_=x1t[s2][:], func=AF.Copy),
                 reads=[B_x1t[s2]], writes=[B_x1b[s2]])
            bk = t % 2
            tpv = ps[bk][:].bitcast(BF16) if hasattr(ps[bk][:], "bitcast") else None
            P.op("pe", [lambda e, c=c, s2=s2, tpv=tpv: e.transpose(tpv[:, c * 128:(c + 1) * 128], x1b[s2][:, c * 128:(c + 1) * 128], ident[:])
                        for c in range(8)],
                 reads=[B_x1b[s2], B_c], writes=[B_ps[bk]], extra=c_all)
            P.op("dve", lambda e, t=t, tpv=tpv: e.tensor_copy(out=x1T[:, :, t * 128:(t + 1) * 128],
                                                             in_=tpv[:, 0:1024].rearrange("p (c n) -> p c n", c=8)),
                 reads=[B_ps[bk]], writes=[B_x1T[t]])
            rbk = 2 + ((t // 4) % 2)
            tb = t % 4
            P.op("pe", [lambda e, c=c, t=t, rbk=rbk, tb=tb: e.matmul(ps[rbk][:, tb * 36:(tb + 1) * 36], x1T[:, c, t * 128:(t + 1) * 128], wr[:, c, :],
                                                                    start=(c == 0), stop=(c == 7)) for c in range(8)],
                 reads=[B_x1T[t], B_c], writes=[B_ps[rbk]], extra=c_all)
            if tb != 3:
                continue
            t0 = t - 3
            rt = rt4[(t // 4) % 2]
            R = [B_rt4[(t // 4) % 2]]
            NBT = 4
            L3 = rt[:, 0:144].rearrange("p (b k) -> p b k", b=NBT)
            G4 = L3[:, :, 0:4]
            EL = L3[:, :, 4:36].rearrange("p b (g e) -> p b g e", g=4)
            OHG = rt[:, 144:160].rearrange("p (b k) -> p b k", b=NBT)
            D4 = rt[:, 160:176].rearrange("p (b k) -> p b k", b=NBT)
            TMP = rt[:, 176:304].rearrange("p (b g e) -> p b g e", b=NBT, g=4)
            EIN = rt[:, 304:336].rearrange("p (b k) -> p b k", b=NBT)
            E2 = rt[:, 336:368].rearrange("p (b k) -> p b k", b=NBT)
            OH1 = rt[:, 368:400].rearrange("p (b k) -> p b k", b=NBT)
            OH2 = rt[:, 400:432].rearrange("p (b k) -> p b k", b=NBT)
            CG = rt[:, 432:464].rearrange("p (b k) -> p b k", b=NBT)
            T1 = rt[:, 464:496].rearrange("p (b k) -> p b k", b=NBT)
            sc = lambda i, rt=rt: rt[:, 496 + 4 * i:500 + 4 * i]
            bc = lambda ap, k: ap.unsqueeze(2).to_broadcast([128, NBT, k])
            P.op("dve", lambda e, rbk=rbk: e.tensor_tensor(out=L3, in0=ps[rbk][:, 0:144].rearrange("p (b k) -> p b k", b=NBT),
                                                          in1=rb[:].unsqueeze(1).to_broadcast([128, NBT, 36]), op=ALU.add),
                 reads=[B_ps[rbk], B_c], writes=R)
            P.op("dve", lambda e: e.reduce_max(out=sc(0), in_=G4, axis=AX.X), reads=R, writes=R)
            P.op("dve", lambda e: e.tensor_tensor(out=OHG, in0=G4, in1=bc(sc(0), 4), op=ALU.is_equal), reads=R, writes=R)
            P.op("dve", lambda e: e.tensor_tensor(out=D4, in0=G4, in1=bc(sc(0), 4), op=ALU.subtract), reads=R, writes=R)
            P.op("act", lambda e: e.activation(out=D4, in_=D4, func=AF.Exp), reads=R, writes=R)
            P.op("dve", lambda e: e.reduce_sum(out=sc(1), in_=D4, axis=AX.X), reads=R, writes=R)
            P.op("dve", lambda e: e.reciprocal(out=sc(1), in_=sc(1)), reads=R, writes=R)
            P.op("dve", lambda e: e.tensor_tensor(out=TMP, in0=EL, in1=OHG.unsqueeze(3).to_broadcast([128, NBT, 4, 8]), op=ALU.mult), reads=R, writes=R)
            P.op("dve", lambda e: e.reduce_sum(out=EIN, in_=TMP.rearrange("p b g e -> p b e g"), axis=AX.X), reads=R, writes=R)
            P.op("dve", lambda e: e.reduce_max(out=sc(2), in_=EIN, axis=AX.X), reads=R, writes=R)
            P.op("dve", lambda e: e.tensor_tensor(out=OH1, in0=EIN, in1=bc(sc(2), 8), op=ALU.is_equal), reads=R, writes=R)
            P.op("dve", lambda e: e.scalar_tensor_tensor(out=E2, in0=OH1, scalar=-1e30, in1=EIN, op0=ALU.mult, op1=ALU.add), reads=R, writes=R)
            P.op("dve", lambda e: e.reduce_max(out=sc(3), in_=E2, axis=AX.X), reads=R, writes=R)
            P.op("dve", lambda e: e.tensor_tensor(out=OH2, in0=E2, in1=bc(sc(3), 8), op=ALU.is_equal), reads=R, writes=R)
            P.op("dve", lambda e: e.tensor_tensor(out=sc(4), in0=sc(3), in1=sc(2), op=ALU.subtract), reads=R, writes=R)
            P.op("act", lambda e: e.activation(out=sc(4), in_=sc(4), func=AF.Exp), reads=R, writes=R)
            P.op("dve", lambda e: e.tensor_scalar(out=sc(4), in0=sc(4), scalar1=1.0, scalar2=None, op0=ALU.add), reads=R, writes=R)
            P.op("dve", lambda e: e.reciprocal(out=sc(4), in_=sc(4)), reads=R, writes=R)
            P.op("dve", lambda e: e.tensor_tensor(out=sc(5), in0=sc(4), in1=sc(1), op=ALU.mult), reads=R, writes=R)
            P.op("dve", lambda e: e.tensor_tensor(out=sc(6), in0=sc(1), in1=sc(5), op=ALU.subtract), reads=R, writes=R)
            P.op("dve", lambda e: e.tensor_tensor(out=CG, in0=OH1, in1=bc(sc(5), 8), op=ALU.mult), reads=R, writes=R)
            P.op("dve", lambda e: e.tensor_tensor(out=T1, in0=OH2, in1=bc(sc(6), 8), op=ALU.mult), reads=R, writes=R)
            P.op("dve", lambda e: e.tensor_tensor(out=CG, in0=CG, in1=T1, op=ALU.add), reads=R, writes=R)
            P.op("dve", lambda e, t0=t0: e.tensor_tensor(out=comb[:, t0:t0 + NBT, :].rearrange("p b (g e) -> p b g e", g=4),
                                                        in0=OHG.unsqueeze(3).to_broadcast([128, NBT, 4, 8]),
                                                        in1=CG.unsqueeze(2).to_broadcast([128, NBT, 4, 8]), op=ALU.mult),
                 reads=R, writes=B_comb[t0:t0 + NBT])

        if C.dbg:
            for t in range(TPT):
                tg = p * TPT + t
                C.out_toks.append(P.dma("sp", C.dbgc[tg * 128:(tg + 1) * 128, :], comb[:, t, :], reads=[B_comb[t]], writes=[C.B_dbg], key="dbgc"))
        steps = [(e, g) for e in range(NE - 2) for g in range(GP)] + [(e, g) for g in range(GP) for e in (NE - 2, NE - 1)]

        def emit_gu(k):
            e, g = steps[k]
            s = (p * NE + e) % NW
            for j in range(2):
                pr = (2 * k + j) % 2
                bg, bu = 2 * pr, 2 * pr + 1
                P.op("pe", [lambda en, c=c, j=j, s=s, g=g, bg=bg: en.matmul(ps[bg][:], wg[s][:, c, j * 128:(j + 1) * 128],
                                                                          x1T[:, c, g * 512:(g + 1) * 512], start=(c == 0), stop=(c == 7))
                            for c in range(8)],
                     reads=[B_w[s]] + B_x1T[4 * g:4 * g + 4], writes=[B_ps[bg]])
                P.op("pe", [lambda en, c=c, j=j, s=s, g=g, bu=bu: en.matmul(ps[bu][:], wu[s][:, c, j * 128:(j + 1) * 128],
                                                                          x1T[:, c, g * 512:(g + 1) * 512], start=(c == 0), stop=(c == 7))
                            for c in range(8)],
                     reads=[B_w[s]] + B_x1T[4 * g:4 * g + 4], writes=[B_ps[bu]])
                si = (2 * k + j) % 3
                P.op("act", lambda en, bg=bg, si=si: en.activation(out=sg[si][:], in_=ps[bg][:], func=AF.Silu),
                     reads=[B_ps[bg]], writes=[B_sg[si]])
                P.op("dve", lambda en, bu=bu, si=si, k=k, j=j: en.tensor_tensor(out=hT[k % 2][j][:], in0=ps[bu][:], in1=sg[si][:], op=ALU.mult),
                     reads=[B_ps[bu], B_sg[si]], writes=[B_hT[k % 2][j]])

        def emit_down(k):
            e, g = steps[k]
            s = (p * NE + e) % NW
            for tt in range(4):
                t = 4 * g + tt
                pr = (4 * k + tt) % 2
                b0, b1 = 4 + 2 * pr, 5 + 2 * pr
                fns = []
                for n, bk in ((0, b0), (1, b1)):
                    for j in range(2):
                        fns.append(lambda en, n=n, bk=bk, j=j, tt=tt, k=k, s=s: en.matmul(
                            ps[bk][:], hT[k % 2][j][:, tt * 128:(tt + 1) * 128], wd[s][:, j, n * 512:(n + 1) * 512],
                            start=(j == 0), stop=(j == 1)))
                P.op("pe", fns, reads=[B_w[s], B_hT[k % 2][0], B_hT[k % 2][1]], writes=[B_ps[b0], B_ps[b1]])
                for n, bk in ((0, b0), (1, b1)):
                    P.op("dve", lambda en, bk=bk, n=n, t=t, e=e: en.scalar_tensor_tensor(
                        out=acc[:, t, n * 512:(n + 1) * 512], in0=ps[bk][:], scalar=comb[:, t, e:e + 1],
                        in1=acc[:, t, n * 512:(n + 1) * 512], op0=ALU.mult, op1=ALU.add),
                        reads=[B_ps[bk], B_comb[t], B_acc[t]], writes=[B_acc[t]])

        for k in range(len(steps) + 1):
            if k < len(steps):
                emit_gu(k)
            if k >= 1:
                emit_down(k - 1)
            if k < len(steps):
                e, g = steps[k]
                q = p * NE + e
                if g == 0 and q + 2 < NPASS * NE:
                    load_expert(q + 2)

        for t in range(TPT):
            tg = p * TPT + t
            o = t % 2
            stats, mv, B_st = stats_2[t % 2], mv_2[t % 2], B_st2[t % 2]
            for hh in range(2):
                P.op("dve", lambda e, hh=hh, t=t: e.bn_stats(out=stats[:, hh, :], in_=acc[:, t, hh * 512:(hh + 1) * 512]),
                     reads=[B_acc[t]], writes=[B_st])
            P.op("dve", lambda e: e.bn_aggr(out=mv[:, 0:2], in_=stats[:]), reads=[B_st], writes=[B_st])
            P.op("dve", lambda e: e.tensor_scalar(out=mv[:, 2:3], in0=mv[:, 1:2], scalar1=LN_EPS, scalar2=None, op0=ALU.add),
                 reads=[B_st], writes=[B_st])
            P.op("act", lambda e: e.activation(out=mv[:, 2:3], in_=mv[:, 2:3], func=AF.Sqrt), reads=[B_st], writes=[B_st])
            P.op("dve", lambda e: e.reciprocal(out=mv[:, 2:3], in_=mv[:, 2:3]), reads=[B_st], writes=[B_st])
            P.op("dve", lambda e: e.tensor_scalar(out=mv[:, 3:4], in0=mv[:, 0:1], scalar1=-1.0, scalar2=mv[:, 2:3], op0=ALU.mult, op1=ALU.mult),
                 reads=[B_st], writes=[B_st])
            P.op("act", lambda e, t=t, o=o: e.activation(out=ot[o][:], in_=acc[:, t, :], func=AF.Identity, bias=mv[:, 3:4], scale=mv[:, 2:3]),
                 reads=[B_acc[t], B_st], writes=[B_ot[o]])
            P.op("pool", lambda e, o=o: e.tensor_tensor(out=ot[o][:], in0=ot[o][:], in1=ln2w[:], op=ALU.mult),
                 reads=[B_ot[o], B_c], writes=[B_ot[o]], extra=c_all)
            P.op("pool", lambda e, o=o: e.tensor_tensor(out=ot[o][:], in0=ot[o][:], in1=ln2b[:], op=ALU.add),
                 reads=[B_ot[o], B_c], writes=[B_ot[o]], extra=c_all)
            C.out_toks.append(P.dma("sp", C.out[tg * 128:(tg + 1) * 128, :], ot[o][:], reads=[B_ot[o]], writes=[C.B_out]))


QK0, GI0, GF0, V0, O0, AQ0, AK0, AV0, NIN = 0, 1024, 1028, 1032, 1544, 2056, 2568, 2696, 2824
DSC = 128.0 ** -0.5
NG = 16


def phase1(nc, P, C, st):
    sb = lambda n, s, d: st.enter_context(nc.sbuf_tensor("sb_" + n, s, d))
    win = sb("win", [128, 8, NIN], BF16)
    wout = sb("wout", [128, 8, D], BF16)
    xTg = [sb(f"xTg{i}", [128, 8, 512], BF16) for i in range(2)]
    praw = [sb(f"praw{i}", [128, 515], F32) for i in range(2)]
    halo = sb("halo", [128, 8, 3], F32)
    cv = [sb(f"cv{i}", [128, 512], F32) for i in range(2)]
    onecol = sb("onecol", [128, 1], F32)
    qkT2 = [sb(f"qkT{i}", [128, 8, 512], BF16) for i in range(2)]
    ones4 = sb("ones4", [4, 512], F32)
    gA2 = [sb(f"gA{i}", [4, 512], F32) for i in range(2)]
    gF = sb("gF", [4, 512], F32)
    gT = sb("gT", [4, 512], F32)
    gB = sb("gB", [4, 512], F32)
    gM = sb("gM", [4, 512], F32)
    gN2 = [sb(f"gN{i}", [4, 512], F32) for i in range(2)]
    gst = sb("gst", [4, 2], F32)
    M_bc2 = [sb(f"M_bc{i}", [128, 4, 512], F32) for i in range(2)]
    gtm = [sb(f"gtm{i}", [128, 8], F32) for i in range(2)]
    vext = [sb(f"vext{i}", [128, 4, 129], BF16) for i in range(2)]
    vaext = [sb(f"vaext{i}", [128, 2, 65], BF16) for i in range(2)]
    aqs2 = [sb(f"aqs{i}", [128, 512], F32) for i in range(2)]
    aks2 = [sb(f"aks{i}", [128, 256], F32) for i in range(2)]
    rt1 = sb("rt1", [128, 256], F32)
    rt2 = sb("rt2", [128, 256], F32)
    qrot = sb("qrot", [128, 512], BF16)
    krot = sb("krot", [128, 128], BF16)
    qT = sb("qT", [64, 1024], BF16)
    kT = [sb(f"kT{i}", [64, 256], BF16) for i in range(2)]
    eo2 = [sb(f"eo{i}", [128, 512], F32) for i in range(2)]
    WT2 = [sb(f"WT{i}", [128, 512], F32) for i in range(2)]
    WI2 = [sb(f"WI{i}", [128, 512], F32) for i in range(2)]
    PT2 = [sb(f"PT{i}", [128, 512], BF16) for i in range(2)]
    qTw2 = [sb(f"qTw{i}", [128, 512], BF16) for i in range(2)]
    kw2 = [sb(f"kw{i}", [128, 512], BF16) for i in range(2)]
    CT = sb("CT", [128, 4, 129], F32)
    CTb = sb("CTb", [128, 4, 129], BF16)
    hh = sb("hh", [128, 512], F32)
    sm = [sb(f"sm{i}", [128, 16], F32) for i in range(2)]
    sm2 = [sb(f"sm2{i}", [128, 24], F32) for i in range(2)]
    sm3 = [sb(f"sm3{i}", [128, 16], F32) for i in range(2)]
    st4 = sb("st4", [128, 4, 6], F32)
    mv4 = sb("mv4", [128, 4, 2], F32)
    PTa = [sb(f"PTa{i}", [128, 512], BF16) for i in range(4)]
    concat2 = [sb(f"concat{i}", [128, D], BF16) for i in range(2)]
    concatT = sb("concatT", [128, D], BF16)
    r1 = [sb(f"r1{i}", [128, D], F32) for i in range(2)]
    lst = sb("lst", [128, 2, 6], F32)
    lmv = sb("lmv", [128, 4], F32)
    ident = sb("ident1", [128, 128], BF16)
    identf = sb("identf1", [128, 128], F32)
    m128 = sb("m128", [128, 128], BF16)
    mbcur = sb("mbcur", [128, 512], BF16)
    mbprev = sb("mbprev", [128, 512], BF16)
    mbprev0 = sb("mbprev0", [128, 512], BF16)
    sel4 = sb("sel4", [4, 512], F32)
    normw = sb("normw", [128, 512], F32)
    ln1w = sb("ln1w", [128, D], F32)
    ln1b = sb("ln1b", [128, D], F32)
    cw = sb("cw", [128, 8, 4], F32)
    cb = sb("cb", [128, 8], F32)
    esink = sb("esink", [128, 8], F32)
    gbi = sb("gbi", [4, 1], F32)
    gbf = sb("gbf", [4, 1], F32)
    preib = sb("preib", [4, 1], F32)
    prefs = sb("prefs", [4, 1], F32)
    flag128 = sb("flag128", [128, 1], F32)
    ropeb = [sb(f"rope{i}", [128, 64], F32) for i in range(2)]

    ps = [st.enter_context(nc.psum_tensor(f"ps1_{i}", [128, 512], F32)) for i in range(8)]
    B_ps = [Buf(f"ps1_{i}") for i in range(8)]
    import os
    alias = {}
    cfg = "F=0,1,4,5;P=2,3;T=6,7"
    bank_set = {}
    for part in cfg.split(";"):
        k, v = part.split("=")
        if v in ("F", "P", "T"):
            alias[k] = v
        else:
            bank_set[k] = tuple(int(x) for x in v.split(","))
    bank_i = {"F": 0, "P": 0, "T": 0}

    def nb(pool):
        pool = alias.get(pool, pool)
        b = bank_set[pool][bank_i[pool] % len(bank_set[pool])]
        bank_i[pool] += 1
        return b

    mroles = ""
    mrole_map = {kv.split("=")[0]: int(kv.split("=")[1]) for kv in mroles.split(",")} if mroles else None

    def nbm(role):
        if mrole_map is None:
            return nb("F")
        return mrole_map[role]

    B_c = Buf("const1", const=True)
    B_wk, B_wv, B_wq, B_wo, B_wout, B_cid, B_csm, B_ones, B_esink = (Buf(n, const=True) for n in
        ("c_wk", "c_wv", "c_wq", "c_wo", "c_wout", "c_cid", "c_csm", "c_ones", "c_esink"))
    B_xTg = [Buf(f"xTg_{i}") for i in range(2)]
    B_praw = [Buf(f"praw_{i}") for i in range(2)]
    B_halo = [Buf(f"halo_{j}") for j in range(8)]
    B_cv = [Buf(f"cv_{i}") for i in range(2)]
    B_qkT2 = [[Buf(f"qkT_{i}_{j}") for j in range(8)] for i in range(2)]
    B_gF, B_gT, B_gB, B_gM, B_gst = (Buf(n) for n in ("gF", "gT", "gB", "gM", "gst"))
    B_gA2 = [Buf(f"gA_{i}") for i in range(2)]
    B_gN2 = [Buf(f"gN_{i}") for i in range(2)]
    B_Mbc2 = [[Buf(f"Mbc_{i}_{h}") for h in range(4)] for i in range(2)]
    B_gtm = [Buf(f"gtm_{i}") for i in range(2)]
    B_vext = [Buf(f"vext_{i}") for i in range(2)]
    B_vaext = [Buf(f"vaext_{i}") for i in range(2)]
    B_rt1, B_rt2, B_qrot, B_krot, B_qT = (Buf(n) for n in ("rt1", "rt2", "qrot", "krot", "qT"))
    B_aqs2 = [Buf(f"aqs_{i}") for i in range(2)]
    B_aks2 = [Buf(f"aks_{i}") for i in range(2)]
    B_rope = [Buf(f"rope_{i}") for i in range(2)]
    B_kT = [Buf(f"kT_{i}") for i in range(2)]
    B_CT, B_CTb, B_hh = (Buf(n) for n in ("CT", "CTb", "hh"))
    B_WT2, B_WI2, B_PT2, B_qTw2, B_kw2 = ([Buf(f"{n}_{i}") for i in range(2)] for n in ("WT", "WI", "PT", "qTw", "kw"))
    B_eo2 = [Buf(f"eo_{i}") for i in range(2)]
    B_sm = [Buf(f"sm_{i}") for i in range(2)]
    B_sm2 = [Buf(f"sm2_{i}") for i in range(2)]
    B_sm3 = [Buf(f"sm3_{i}") for i in range(2)]
    B_st4, B_mv4, B_lst = Buf("st4"), Buf("mv4"), Buf("lst")
    B_PTa = [Buf(f"PTa_{i}") for i in range(4)]
    B_concat2 = [Buf(f"concat_{i}") for i in range(2)]
    B_concatT = Buf("concatT")
    B_r1 = [Buf(f"r1_{i}") for i in range(2)]

    ck = []
    for (dst, src) in ((identf[:], C.ident), (sel4[:], C.sel4),
                       (normw[:], C.normw), (ln1w[:], C.ln1w), (ln1b[:], C.ln1b), (cw[:], C.cw), (cb[:], C.cb), (esink[:], C.sinks),
                       (gbi[:], C.gb[0:4, :]), (gbf[:], C.gb[4:8, :]), (preib[:], C.preib), (prefs[:], C.prefs), (flag128[:], C.flag128)):
        ck.append(P.dma("sp", dst, src, writes=[B_csm], key="const1", nbytes=65536))
    wsrc = C.win.rearrange("(c p) n -> p c n", p=128)

    def load_x(G):
        s = G % 2
        P.dma("pool", xTg[s][:], C.xT[:, G * 512:(G + 1) * 512].rearrange("(c p) n -> p c n", p=128), writes=[B_xTg[s]])

    P.dma("pool", ident[:], C.ident, writes=[B_cid], key="const1i", nbytes=65536)
    P.dma("pool", win[:, :, 512:1032], wsrc[:, :, 512:1032], writes=[B_wk], key="const1k")
    load_x(0)
    P.dma("pool", win[:, :, V0:V0 + 512], wsrc[:, :, V0:V0 + 512], writes=[B_wv], key="const1v")
    for (dst, src) in ((m128[:], C.mcur[:, 0:128]), (mbcur[:], C.mbcur), (mbprev[:], C.mbprev), (mbprev0[:], C.mbprev0)):
        P.dma("pool", dst, src, writes=[B_cid], key="const1i", nbytes=65536)
    load_x(1)
    P.dma("pool", win[:, :, 0:512], wsrc[:, :, 0:512], writes=[B_wq], key="const1q")
    P.dma("pool", win[:, :, O0:NIN], wsrc[:, :, O0:NIN], writes=[B_wo], key="const1o")
    P.dma("pool", wout[:], C.wout.rearrange("(c p) n -> p c n", p=128), writes=[B_wout], key="const1w")
    call = []
    t_init = []
    t_init.append(P.op("pool", lambda e: e.memset(onecol[:], 1.0), writes=[B_ones]))
    t_init.append(P.op("pool", lambda e: e.memset(ones4[:], 1.0), writes=[B_ones]))
    t_init.append(P.op("pool", lambda e: e.memset(halo[:], 0.0), writes=B_halo))
    t_init.append(P.op("pool", lambda e: e.memset(gst[:], 0.0), writes=[B_gst]))
    for i in range(2):
        t_init.append(P.op("pool", lambda e, i=i: e.memset(M_bc2[i][:], 0.0), writes=B_Mbc2[i]))
    t_init.append(P.op("pool", lambda e: e.memset(CT[:], 0.0), writes=[B_CT]))
    for i in range(2):
        t_init.append(P.op("pool", lambda e, i=i: e.memset(vext[i][:], 1.0), writes=[B_vext[i]]))
        t_init.append(P.op("pool", lambda e, i=i: e.memset(vaext[i][:], 1.0), writes=[B_vaext[i]]))
    t_init.append(P.op("act", lambda e: e.activation(out=esink[:], in_=esink[:], func=AF.Exp), reads=[B_csm], writes=[B_esink]))

    def load_xtm(to):
        s = to % 2
        P.dma("sp", r1[s][:], C.xown[to * 128:(to + 1) * 128, :], writes=[B_r1[s]], nbytes=524288)

    def load_rope(to):
        s = (to + 1) % 2
        P.dma("sp", ropeb[s][:], C.rope[(to + 1) * 128:(to + 2) * 128, :], writes=[B_rope[s]], nbytes=32768)

    cnt = {"praw": 0, "cv": 0, "tile": 0, "E": 0}
    load_xtm(0)
    load_rope(-1)
    load_rope(0)

    def rstd_chain(var_ap, out_ap, Bs):
        P.op("dve", lambda e: e.tensor_scalar(out=out_ap, in0=var_ap, scalar1=LN_EPS, scalar2=None, op0=ALU.add), reads=Bs, writes=Bs)
        P.op("act", lambda e: e.activation(out=out_ap, in_=out_ap, func=AF.Ln), reads=Bs, writes=Bs)
        P.op("act", lambda e: e.activation(out=out_ap, in_=out_ap, func=AF.Exp, scale=-0.5), reads=Bs, writes=Bs)

    for G in range(NG):
        pre = G < 8
        xs = G % 2
        gs, gp = G % 2, (G + 1) % 2
        qkT, B_qkT = qkT2[gs], B_qkT2[gs]
        gA, gN, B_gA, B_gN = gA2[gs], gN2[gs], B_gA2[gs], B_gN2[gs]
        M_bc, B_Mbc = M_bc2[gs], B_Mbc2[gs]
        M_bcp, B_Mbcp = M_bc2[gp], B_Mbc2[gp]
        if 1 <= G and G + 1 < NG:
            load_x(G + 1)
        if G == 8:
            P.op("pool", lambda e: e.tensor_scalar(out=halo[:], in0=halo[:], scalar1=flag128[:, 0:1], scalar2=None, op0=ALU.mult),
                 reads=B_halo + [B_csm], writes=B_halo)
        for j in ((list(range(4, 8)) + ([0, 1, 2, 3] if G == 7 else [])) if pre else range(8)):
            halo_only = pre and j < 4
            bk = nb("F")
            P.op("pe", [lambda e, c=c, j=j, bk=bk: e.matmul(ps[bk][:], win[:, c, j * 128:(j + 1) * 128], xTg[xs][:, c, :],
                                                           start=(c == 0), stop=(c == 7)) for c in range(8)],
                 reads=[B_xTg[xs], B_wk if j >= 4 else B_wq], writes=[B_ps[bk]])
            r = cnt["praw"] % 2
            cnt["praw"] += 1
            P.op("pool", lambda e, r=r, j=j: e.tensor_copy(out=praw[r][:, 0:3], in_=halo[:, j, :]), reads=[B_halo[j]], writes=[B_praw[r]])
            P.op("act", lambda e, r=r, bk=bk: e.activation(out=praw[r][:, 3:515], in_=ps[bk][:], func=AF.Copy),
                 reads=[B_ps[bk]], writes=[B_praw[r]])
            P.op("pool", lambda e, r=r, j=j: e.tensor_copy(out=halo[:, j, :], in_=praw[r][:, 512:515]), reads=[B_praw[r]], writes=[B_halo[j]])
            if halo_only:
                continue
            c2 = cnt["cv"] % 2
            cnt["cv"] += 1
            P.op("act", lambda e, r=r, j=j, c2=c2: e.activation(out=cv[c2][:], in_=praw[r][:, 3:515], func=AF.Identity, scale=cw[:, j, 3:4], bias=cb[:, j:j + 1]),
                 reads=[B_praw[r], B_csm], writes=[B_cv[c2]])
            for tap in (2, 1, 0):
                P.op("dve", lambda e, r=r, j=j, c2=c2, tap=tap: e.scalar_tensor_tensor(out=cv[c2][:], in0=praw[r][:, tap:tap + 512], scalar=cw[:, j, tap:tap + 1],
                                                                                      in1=cv[c2][:], op0=ALU.mult, op1=ALU.add),
                     reads=[B_praw[r], B_cv[c2]], writes=[B_cv[c2]])
            P.op("act", lambda e, j=j, c2=c2: e.activation(out=qkT[:, j, :], in_=cv[c2][:], func=AF.Silu), reads=[B_cv[c2]], writes=[B_qkT[j]])
        bi = nb("F")
        P.op("pe", [lambda e, c=c: e.matmul(ps[bi][0:4, :], win[:, c, GI0:GI0 + 4], xTg[xs][:, c, :], start=(c == 0), stop=(c == 7)) for c in range(8)],
             reads=[B_xTg[xs], B_wk], writes=[B_ps[bi]])
        if pre:
            P.op("dve", lambda e: e.tensor_scalar(out=gA[:], in0=ps[bi][0:4, :], scalar1=gbi[:, 0:1], scalar2=preib[:, 0:1], op0=ALU.add, op1=ALU.add),
                 reads=[B_ps[bi], B_csm], writes=[B_gA])
        else:
            P.op("dve", lambda e: e.tensor_scalar(out=gA[:], in0=ps[bi][0:4, :], scalar1=gbi[:, 0:1], scalar2=None, op0=ALU.add),
                 reads=[B_ps[bi], B_csm], writes=[B_gA])
        bf = nb("F")
        P.op("pe", [lambda e, c=c: e.matmul(ps[bf][0:4, :], win[:, c, GF0:GF0 + 4], xTg[xs][:, c, :], start=(c == 0), stop=(c == 7)) for c in range(8)],
             reads=[B_xTg[xs], B_wk], writes=[B_ps[bf]])
        P.op("dve", lambda e: e.tensor_scalar(out=gF[:], in0=ps[bf][0:4, :], scalar1=gbf[:, 0:1], scalar2=None, op0=ALU.add),
             reads=[B_ps[bf], B_csm], writes=[B_gF])
        P.op("dve", lambda e: e.tensor_scalar(out=gT[:], in0=gF[:], scalar1=-1.0, scalar2=None, op0=ALU.mult), reads=[B_gF], writes=[B_gT])
        P.op("dve", lambda e: e.tensor_tensor(out=gT[:], in0=gT[:], in1=gF[:], op=ALU.max), reads=[B_gF, B_gT], writes=[B_gT])
        P.op("act", lambda e: e.activation(out=gT[:], in_=gT[:], func=AF.Exp, scale=-1.0), reads=[B_gT], writes=[B_gT])
        P.op("dve", lambda e: e.tensor_scalar(out=gT[:], in0=gT[:], scalar1=1.0, scalar2=None, op0=ALU.add), reads=[B_gT], writes=[B_gT])
        P.op("act", lambda e: e.activation(out=gT[:], in_=gT[:], func=AF.Ln), reads=[B_gT], writes=[B_gT])
        P.op("dve", lambda e: e.scalar_tensor_tensor(out=gF[:], in0=gF[:], scalar=0.0, in1=gT[:], op0=ALU.min, op1=ALU.subtract),
             reads=[B_gF, B_gT], writes=[B_gF])
        if pre:
            P.op("dve", lambda e: e.tensor_scalar(out=gF[:], in0=gF[:], scalar1=prefs[:, 0:1], scalar2=None, op0=ALU.mult), reads=[B_gF, B_csm], writes=[B_gF])
        P.op("dve", lambda e: e.tensor_tensor_scan(out=gB[:], data0=ones4[:], data1=gF[:], initial=gst[:, 0:1], op0=ALU.mult, op1=ALU.add),
             reads=[B_gF, B_gst, B_ones], writes=[B_gB])
        P.op("pool", lambda e: e.tensor_tensor(out=gA[:], in0=gA[:], in1=gB[:], op=ALU.subtract), reads=[B_gA, B_gB], writes=[B_gA])
        P.op("dve", lambda e: e.tensor_tensor_scan(out=gM[:], data0=ones4[:], data1=gA[:], initial=gst[:, 1:2], op0=ALU.mult, op1=ALU.max),
             reads=[B_gA, B_gst, B_ones], writes=[B_gM])
        P.op("pool", lambda e: e.tensor_copy(out=gst[:, 0:1], in_=gB[:, 511:512]), reads=[B_gB], writes=[B_gst])
        P.op("pool", lambda e: e.tensor_copy(out=gst[:, 1:2], in_=gM[:, 511:512]), reads=[B_gM], writes=[B_gst])
        P.op("dve", lambda e: e.scalar_tensor_tensor(out=gN[:], in0=gB[:], scalar=-1.0, in1=gM[:], op0=ALU.mult, op1=ALU.subtract),
             reads=[B_gB, B_gM], writes=[B_gN])
        for h in range(4):
            bk = nb("F")
            if pre:
                P.op("pe", lambda e, h=h, bk=bk: e.matmul(ps[bk][:, 0:4], sel4[:, h * 128:(h + 1) * 128], gM[:, 127:512:128], start=True, stop=True),
                     reads=[B_gM, B_csm], writes=[B_ps[bk]], n=16)
                P.op("act", lambda e, h=h, bk=bk: e.activation(out=M_bc[:, h, 127:512:128], in_=ps[bk][:, 0:4], func=AF.Copy), reads=[B_ps[bk]], writes=[B_Mbc[h]])
            else:
                P.op("pe", lambda e, h=h, bk=bk: e.matmul(ps[bk][:], sel4[:, h * 128:(h + 1) * 128], gM[:], start=True, stop=True),
                     reads=[B_gM, B_csm], writes=[B_ps[bk]], n=2048)
                P.op("act", lambda e, h=h, bk=bk: e.activation(out=M_bc[:, h, :], in_=ps[bk][:], func=AF.Copy), reads=[B_ps[bk]], writes=[B_Mbc[h]])

        for t in range(4):
            cols = slice(t * 128, (t + 1) * 128)
            to = (G - 8) * 4 + t
            halo_tile = (G == 7 and t == 3)
            own = not pre
            ti = cnt["tile"]
            cnt["tile"] += 1
            vs = ti % 2
            ts_ = ti % 2
            eo, B_eo = eo2[ti % 2], B_eo2[ti % 2]
            aqs, B_aqs = aqs2[ti % 2], B_aqs2[ti % 2]
            aks, B_aks = aks2[ti % 2], B_aks2[ti % 2]
            concat, B_concat = concat2[ti % 2], B_concat2[ti % 2]
            WT, WI, PT, qTw, kw = WT2[ti % 2], WI2[ti % 2], PT2[ti % 2], qTw2[ti % 2], kw2[ti % 2]
            B_WT, B_WI, B_PT, B_qTw, B_kw = B_WT2[ti % 2], B_WI2[ti % 2], B_PT2[ti % 2], B_qTw2[ti % 2], B_kw2[ti % 2]
            bv = nb("P")
            fns = []
            wr_ = [B_ps[bv]]
            if own:
                bo = nb("P")
                wr_.append(B_ps[bo])
            for c in range(8):
                fns.append(lambda e, c=c, bv=bv: e.matmul(ps[bv][:], xTg[xs][:, c, cols], win[:, c, V0:V0 + 512], start=(c == 0), stop=(c == 7)))
                if own:
                    fns.append(lambda e, c=c, bo=bo: e.matmul(ps[bo][:], xTg[xs][:, c, cols], win[:, c, O0:O0 + 512], start=(c == 0), stop=(c == 7)))
            P.op("pe", fns, reads=[B_xTg[xs], B_wv, B_wo], writes=wr_)
            P.op("act", lambda e, bv=bv, vs=vs: e.activation(out=vext[vs][:, :, 0:128], in_=ps[bv][:].rearrange("p (h n) -> p h n", h=4), func=AF.Copy),
                 reads=[B_ps[bv]], writes=[B_vext[vs]])
            if own:
                P.op("act", lambda e, bo=bo: e.activation(out=eo[:], in_=ps[bo][:], func=AF.Exp, scale=-1.0), reads=[B_ps[bo]], writes=[B_eo])
                P.op("act", lambda e: e.activation(out=eo[:], in_=eo[:], func=AF.Ln, bias=onecol[:, 0:1], scale=1.0), reads=[B_eo, B_ones], writes=[B_eo])
                P.op("act", lambda e: e.activation(out=eo[:], in_=eo[:], func=AF.Exp, scale=-1.0), reads=[B_eo], writes=[B_eo])
            if own or halo_tile:
                fns = []
                wr_ = []
                bkv = nb("P")
                wr_.append(B_ps[bkv])
                if own:
                    bq = nb("P")
                    wr_.append(B_ps[bq])
                for c in range(8):
                    if own:
                        fns.append(lambda e, c=c, bq=bq: e.matmul(ps[bq][:], xTg[xs][:, c, cols], win[:, c, AQ0:AQ0 + 512], start=(c == 0), stop=(c == 7)))
                    fns.append(lambda e, c=c, bkv=bkv: e.matmul(ps[bkv][:, 0:256], xTg[xs][:, c, cols], win[:, c, AK0:AK0 + 256], start=(c == 0), stop=(c == 7)))
                P.op("pe", fns, reads=[B_xTg[xs], B_wo], writes=wr_)
                if own:
                    P.op("act", lambda e, bq=bq: e.activation(out=aqs[:], in_=ps[bq][:], func=AF.Copy), reads=[B_ps[bq]], writes=[B_aqs])
                P.op("act", lambda e, bkv=bkv: e.activation(out=aks[:], in_=ps[bkv][:, 0:256], func=AF.Copy), reads=[B_ps[bkv]], writes=[B_aks], n=256)

            bg = nbm("B")
            P.op("pe", [lambda e, bg=bg: e.matmul(ps[bg][:, 0:4], gA[:, cols], identf[0:4, 0:4], start=True, stop=True),
                        lambda e, bg=bg: e.matmul(ps[bg][:, 4:8], gN[:, cols], identf[0:4, 0:4], start=True, stop=True)],
                 reads=[B_gA, B_gN, B_csm], writes=[B_ps[bg]], n=8)
            P.op("dve", lambda e, bg=bg: e.tensor_copy(out=gtm[ts_][:], in_=ps[bg][:, 0:8]), reads=[B_ps[bg]], writes=[B_gtm[ts_]], n=8)
            ccol = t * 128 + 127
            Mp = M_bcp[:, :, 511] if t == 0 else M_bc[:, :, t * 128 - 1]
            B_Mp = B_Mbcp if t == 0 else B_Mbc

            if own:
                bs = nbm("S")
                P.op("pe", [lambda e, h=h, bs=bs: e.matmul(ps[bs][:, h * 128:(h + 1) * 128], qkT[:, 4 + h, cols], qkT[:, h, cols], start=True, stop=True)
                            for h in range(4)], reads=B_qkT, writes=[B_ps[bs]])
                for h in range(4):
                    P.op("act", lambda e, h=h: e.activation(out=WT[:, h * 128:(h + 1) * 128], in_=M_bc[:, h, cols], func=AF.Exp, scale=-1.0,
                                                            bias=gtm[ts_][:, h:h + 1]),
                         reads=[B_Mbc[h], B_gtm[ts_]], writes=[B_WT])
                P.op("pool", lambda e: e.tensor_tensor(out=WT[:].rearrange("p (h n) -> p h n", h=4), in0=WT[:].rearrange("p (h n) -> p h n", h=4),
                                                      in1=m128[:].unsqueeze(1).to_broadcast([128, 4, 128]), op=ALU.mult), reads=[B_WT, B_cid], writes=[B_WT])
                P.op("dve", lambda e, bs=bs: e.scalar_tensor_tensor(out=PT[:], in0=ps[bs][:], scalar=DSC, in1=WT[:], op0=ALU.mult, op1=ALU.mult),
                     reads=[B_ps[bs], B_WT], writes=[B_PT])
                for h in range(4):
                    mp_h = M_bcp[:, h, 511:512] if t == 0 else M_bc[:, h, t * 128 - 1:t * 128]
                    P.op("act", lambda e, h=h, mp_h=mp_h: e.activation(out=WI[:, h * 128:(h + 1) * 128], in_=M_bc[:, h, cols], func=AF.Exp, scale=-1.0, bias=mp_h),
                         reads=[B_Mbc[h]] + B_Mp, writes=[B_WI])
                P.op("pool", lambda e: e.tensor_tensor(out=qTw[:].rearrange("p (h n) -> p h n", h=4), in0=qkT[:, 0:4, cols],
                                                      in1=WI[:].rearrange("p (h n) -> p h n", h=4), op=ALU.mult),
                     reads=B_qkT[0:4] + [B_WI], writes=[B_qTw])
                bn = [nbm("C"), nbm("D")]
                fns = []
                for h in range(4):
                    o_ = (h % 2) * 129
                    fns.append(lambda e, h=h, o_=o_: e.matmul(ps[bn[h // 2]][:, o_:o_ + 129], qTw[:, h * 128:(h + 1) * 128], CTb[:, h, :], start=True, stop=False))
                    fns.append(lambda e, h=h, o_=o_: e.matmul(ps[bn[h // 2]][:, o_:o_ + 129], PT[:, h * 128:(h + 1) * 128], vext[vs][:, h, :], start=False, stop=True))
                P.op("pe", fns, reads=[B_qTw, B_CTb, B_PT, B_vext[vs]], writes=[B_ps[bn[0]], B_ps[bn[1]]])
                s2 = sm2[ts_]
                S2 = [B_sm2[ts_]]
                P.op("act", lambda e, s2=s2: e.activation(out=s2[:, 0:4], in_=gtm[ts_][:, 4:8], func=AF.Exp), reads=[B_gtm[ts_]], writes=S2)
                for b in range(2):
                    P.op("dve", lambda e, b=b, s2=s2: e.tensor_scalar(out=s2[:, 20 + 2 * b:22 + 2 * b], in0=ps[bn[b]][:, 128:258:129], scalar1=-1.0, scalar2=None, op0=ALU.mult),
                         reads=[B_ps[bn[b]]] + S2, writes=S2)
                    P.op("dve", lambda e, b=b, s2=s2: e.tensor_tensor(out=s2[:, 20 + 2 * b:22 + 2 * b], in0=s2[:, 20 + 2 * b:22 + 2 * b], in1=ps[bn[b]][:, 128:258:129], op=ALU.max),
                         reads=[B_ps[bn[b]]] + S2, writes=S2)
                    P.op("dve", lambda e, b=b, s2=s2: e.tensor_tensor(out=s2[:, 4 + 2 * b:6 + 2 * b], in0=s2[:, 20 + 2 * b:22 + 2 * b], in1=s2[:, 2 * b:2 * b + 2], op=ALU.max),
                         reads=S2, writes=S2)
                P.op("dve", lambda e, s2=s2: e.reciprocal(out=s2[:, 8:12], in_=s2[:, 4:8]), reads=S2, writes=S2)
                for h in range(4):
                    o_ = (h % 2) * 129
                    P.op("act", lambda e, h=h, o_=o_, s2=s2: e.activation(out=hh[:, h * 128:(h + 1) * 128], in_=ps[bn[h // 2]][:, o_:o_ + 128], func=AF.Identity,
                                                                         scale=s2[:, 8 + h:9 + h]),
                         reads=[B_ps[bn[h // 2]]] + S2, writes=[B_hh])
                for h in range(4):
                    P.op("dve", lambda e, h=h: e.bn_stats(out=st4[:, h, :], in_=hh[:, h * 128:(h + 1) * 128]), reads=[B_hh], writes=[B_st4])
                for h in range(4):
                    P.op("dve", lambda e, h=h: e.bn_aggr(out=mv4[:, h, :], in_=st4[:, h, :]), reads=[B_st4], writes=[B_mv4])
                rstd_chain(mv4[:, :, 1], s2[:, 12:16], S2 + [B_mv4])
                P.op("dve", lambda e, s2=s2: e.scalar_tensor_tensor(out=s2[:, 16:20], in0=mv4[:, :, 0], scalar=-1.0, in1=s2[:, 12:16], op0=ALU.mult, op1=ALU.mult),
                     reads=S2 + [B_mv4], writes=S2)
                for h in range(4):
                    P.op("act", lambda e, h=h, s2=s2: e.activation(out=hh[:, h * 128:(h + 1) * 128], in_=hh[:, h * 128:(h + 1) * 128], func=AF.Identity,
                                                                 scale=s2[:, 12 + h:13 + h], bias=s2[:, 16 + h:17 + h]),
                         reads=[B_hh] + S2, writes=[B_hh])
                P.op("pool", lambda e: e.tensor_tensor(out=hh[:], in0=hh[:], in1=normw[:], op=ALU.mult), reads=[B_hh, B_csm], writes=[B_hh])
                P.op("dve", lambda e: e.tensor_tensor(out=concat[:, 0:512], in0=hh[:], in1=eo[:], op=ALU.mult), reads=[B_hh, B_eo], writes=[B_concat])

            btp = nbm("B")
            tpv = ps[btp][:].bitcast(BF16)[:, 512:1024]
            P.op("pe", [lambda e, h=h, tpv=tpv: e.transpose(tpv[:, h * 128:(h + 1) * 128], qkT[:, 4 + h, cols], ident[:]) for h in range(4)],
                 reads=B_qkT[4:8] + [B_cid], writes=[B_ps[btp]], n=128)
            s1 = sm[ts_]
            S1 = [B_sm[ts_]]
            P.op("dve", lambda e, s1=s1: e.tensor_tensor(out=s1[:, 0:4], in0=gtm[ts_][:, 0:4], in1=M_bc[:, :, ccol], op=ALU.subtract),
                 reads=[B_gtm[ts_]] + B_Mbc, writes=S1)
            P.op("dve", lambda e, s1=s1, Mp=Mp: e.tensor_tensor(out=s1[:, 4:8], in0=Mp, in1=M_bc[:, :, ccol], op=ALU.subtract),
                 reads=B_Mp + B_Mbc, writes=S1)
            P.op("act", lambda e, s1=s1: e.activation(out=s1[:, 8:16], in_=s1[:, 0:8], func=AF.Exp), reads=S1, writes=S1)
            for h in range(4):
                P.op("dve", lambda e, h=h, s1=s1, tpv=tpv: e.tensor_scalar(out=kw[:, h * 128:(h + 1) * 128], in0=tpv[:, h * 128:(h + 1) * 128],
                                                                          scalar1=s1[:, 8 + h:9 + h], scalar2=DSC, op0=ALU.mult, op1=ALU.mult),
                     reads=[B_ps[btp]] + S1, writes=[B_kw], n=128)
            bu = [nbm("C"), nbm("D")]
            fns = []
            for h in range(4):
                o_ = (h % 2) * 129
                fns.append(lambda e, h=h, o_=o_: e.matmul(ps[bu[h // 2]][:, o_:o_ + 129], kw[:, h * 128:(h + 1) * 128], vext[vs][:, h, :], start=True, stop=True))
            P.op("pe", fns, reads=[B_kw, B_vext[vs]], writes=[B_ps[bu[0]], B_ps[bu[1]]])
            for h in range(4):
                o_ = (h % 2) * 129
                P.op("dve", lambda e, h=h, o_=o_, s1=s1: e.scalar_tensor_tensor(out=CT[:, h, :], in0=CT[:, h, :], scalar=s1[:, 12 + h:13 + h],
                                                                               in1=ps[bu[h // 2]][:, o_:o_ + 129], op0=ALU.mult, op1=ALU.add),
                     reads=[B_CT, B_ps[bu[h // 2]]] + S1, writes=[B_CT])
            if own or halo_tile:
                P.op("pool", lambda e: e.tensor_copy(out=CTb[:], in_=CT[:]), reads=[B_CT], writes=[B_CTb])

            if own or halo_tile:
                ks = (to + 1) % 2
                kp = to % 2
                rsl = (to + 1) % 2
                cosb = lambda n: ropeb[rsl][:, 0:32].unsqueeze(1).to_broadcast([128, n, 32])
                sinb = lambda n: ropeb[rsl][:, 32:64].unsqueeze(1).to_broadcast([128, n, 32])

                def do_rope(src, dst, n, Bsrc, Bdst):
                    v = src.rearrange("p (h t d) -> p h t d", h=n, t=2)
                    o = dst.rearrange("p (h t d) -> p h t d", h=n, t=2)
                    a = rt1[:, 0:n * 32].rearrange("p (h d) -> p h d", h=n)
                    b = rt2[:, 0:n * 32].rearrange("p (h d) -> p h d", h=n)
                    cn, sn = cosb(n), sinb(n)
                    P.op("pool", lambda e: e.tensor_tensor(out=a, in0=v[:, :, 0, :], in1=cn, op=ALU.mult), reads=[Bsrc, B_rope[rsl]], writes=[B_rt1])
                    P.op("pool", lambda e: e.tensor_tensor(out=b, in0=v[:, :, 1, :], in1=sn, op=ALU.mult), reads=[Bsrc, B_rope[rsl]], writes=[B_rt2])
                    P.op("pool", lambda e: e.tensor_tensor(out=o[:, :, 0, :], in0=a, in1=b, op=ALU.subtract), reads=[B_rt1, B_rt2], writes=[Bdst])
                    P.op("pool", lambda e: e.tensor_tensor(out=a, in0=v[:, :, 0, :], in1=sn, op=ALU.mult), reads=[Bsrc, B_rope[rsl]], writes=[B_rt1])
                    P.op("pool", lambda e: e.tensor_tensor(out=b, in0=v[:, :, 1, :], in1=cn, op=ALU.mult), reads=[Bsrc, B_rope[rsl]], writes=[B_rt2])
                    P.op("pool", lambda e: e.tensor_tensor(out=o[:, :, 1, :], in0=a, in1=b, op=ALU.add), reads=[B_rt1, B_rt2], writes=[Bdst])

                do_rope(aks[:, 0:128], krot[:], 2, B_aks, B_krot)
                bk2 = nb("T")
                tk = ps[bk2][:].bitcast(BF16)
                P.op("pe", [lambda e, j=j, tk=tk: e.transpose(tk[0:64, j * 128:(j + 1) * 128], krot[:, j * 64:(j + 1) * 64], ident[:]) for j in range(2)],
                     reads=[B_krot, B_cid], writes=[B_ps[bk2]])
                P.op("act", lambda e, tk=tk, ks=ks: e.activation(out=kT[ks][:], in_=tk[0:64, 0:256], func=AF.Copy), reads=[B_ps[bk2]], writes=[B_kT[ks]])
                P.op("pool", lambda e, ks=ks: e.tensor_copy(out=vaext[ks][:, :, 0:64], in_=aks[:, 128:256].rearrange("p (h d) -> p h d", h=2)),
                     reads=[B_aks], writes=[B_vaext[ks]])
            if own:
                do_rope(aqs[:], qrot[:], 8, B_aqs, B_qrot)
                bq2 = nb("T")
                tq = ps[bq2][:].bitcast(BF16)
                P.op("pe", [lambda e, hq=hq, tq=tq: e.transpose(tq[0:64, hq * 128:(hq + 1) * 128], qrot[:, hq * 64:(hq + 1) * 64], ident[:]) for hq in range(8)],
                     reads=[B_qrot, B_cid], writes=[B_ps[bq2]])
                P.op("act", lambda e, tq=tq: e.activation(out=qT[:], in_=tq[0:64, 0:1024], func=AF.Copy), reads=[B_ps[bq2]], writes=[B_qT])
                for j in range(2):
                    for kb in range(2):
                        slot = kp if kb == 0 else ks
                        bsc = nb("T")
                        msk = mbcur if kb == 1 else (mbprev0 if to == 0 else mbprev)
                        P.op("pe", [lambda e, j=j, slot=slot, bsc=bsc: e.matmul(ps[bsc][:], kT[slot][:, j * 128:(j + 1) * 128], qT[:, j * 512:(j + 1) * 512], start=True, stop=False),
                                    lambda e, bsc=bsc, msk=msk: e.matmul(ps[bsc][:], ident[:], msk[:], start=False, stop=True)],
                             reads=[B_kT[slot], B_qT, B_cid], writes=[B_ps[bsc]])
                        P.op("act", lambda e, bsc=bsc, j=j, kb=kb: e.activation(out=PTa[2 * j + kb][:], in_=ps[bsc][:], func=AF.Exp, scale=0.125),
                             reads=[B_ps[bsc]], writes=[B_PTa[2 * j + kb]])
                bpv = [nb("T"), nb("T")]
                for j in range(2):
                    fns = []
                    for g in range(4):
                        fns.append(lambda e, j=j, g=g: e.matmul(ps[bpv[j]][:, g * 65:(g + 1) * 65], PTa[2 * j][:, g * 128:(g + 1) * 128], vaext[kp][:, j, :], start=True, stop=False))
                        fns.append(lambda e, j=j, g=g: e.matmul(ps[bpv[j]][:, g * 65:(g + 1) * 65], PTa[2 * j + 1][:, g * 128:(g + 1) * 128], vaext[ks][:, j, :], start=False, stop=True))
                    P.op("pe", fns, reads=[B_PTa[2 * j], B_PTa[2 * j + 1], B_vaext[kp], B_vaext[ks]], writes=[B_ps[bpv[j]]])
                s3 = sm3[ts_]
                S3 = [B_sm3[ts_]]
                for j in range(2):
                    P.op("dve", lambda e, j=j, s3=s3: e.tensor_tensor(out=s3[:, 4 * j:4 * j + 4], in0=ps[bpv[j]][:, 64:260:65], in1=esink[:, 4 * j:4 * j + 4], op=ALU.add),
                         reads=[B_ps[bpv[j]], B_esink], writes=S3)
                P.op("dve", lambda e, s3=s3: e.reciprocal(out=s3[:, 8:16], in_=s3[:, 0:8]), reads=S3, writes=S3)
                for j in range(2):
                    P.op("dve", lambda e, j=j, s3=s3: e.tensor_tensor(out=concat[:, 512 + 256 * j:768 + 256 * j].rearrange("p (g d) -> p g d", g=4),
                                                                     in0=ps[bpv[j]][:, 0:260].rearrange("p (g d) -> p g d", g=4)[:, :, 0:64],
                                                                     in1=s3[:, 8 + 4 * j:12 + 4 * j].unsqueeze(2).to_broadcast([128, 4, 64]), op=ALU.mult),
                         reads=[B_ps[bpv[j]]] + S3, writes=[B_concat])

                bct = nb("T")
                tcv = ps[bct][:].bitcast(BF16)
                P.op("pe", [lambda e, c=c, tcv=tcv: e.transpose(tcv[:, c * 128:(c + 1) * 128], concat[:, c * 128:(c + 1) * 128], ident[:]) for c in range(8)],
                     reads=[B_concat, B_cid], writes=[B_ps[bct]])
                P.op("act", lambda e, tcv=tcv: e.activation(out=concatT[:], in_=tcv[:, 0:1024], func=AF.Copy), reads=[B_ps[bct]], writes=[B_concatT])
                by = [nb("T"), nb("T")]
                fns = []
                for n in range(2):
                    for c in range(8):
                        fns.append(lambda e, n=n, c=c: e.matmul(ps[by[n]][:], concatT[:, c * 128:(c + 1) * 128], wout[:, c, n * 512:(n + 1) * 512], start=(c == 0), stop=(c == 7)))
                P.op("pe", fns, reads=[B_concatT, B_wout], writes=[B_ps[by[0]], B_ps[by[1]]])
                rs = to % 2
                for n in range(2):
                    P.op("dve", lambda e, n=n, rs=rs: e.scalar_tensor_tensor(out=r1[rs][:, n * 512:(n + 1) * 512], in0=r1[rs][:, n * 512:(n + 1) * 512], scalar=ALPHA,
                                                                            in1=ps[by[n]][:], op0=ALU.mult, op1=ALU.add),
                         reads=[B_r1[rs], B_ps[by[n]]], writes=[B_r1[rs]])
                for n in range(2):
                    P.op("dve", lambda e, n=n, rs=rs: e.bn_stats(out=lst[:, n, :], in_=r1[rs][:, n * 512:(n + 1) * 512]), reads=[B_r1[rs]], writes=[B_lst])
                P.op("dve", lambda e: e.bn_aggr(out=lmv[:, 0:2], in_=lst[:]), reads=[B_lst], writes=[B_lst])
                rstd_chain(lmv[:, 1:2], lmv[:, 2:3], [B_lst])
                P.op("dve", lambda e: e.tensor_scalar(out=lmv[:, 3:4], in0=lmv[:, 0:1], scalar1=-1.0, scalar2=lmv[:, 2:3], op0=ALU.mult, op1=ALU.mult),
                     reads=[B_lst], writes=[B_lst])
                P.op("act", lambda e, rs=rs: e.activation(out=r1[rs][:], in_=r1[rs][:], func=AF.Identity, bias=lmv[:, 3:4], scale=lmv[:, 2:3]),
                     reads=[B_r1[rs], B_lst], writes=[B_r1[rs]])
                P.op("pool", lambda e, rs=rs: e.tensor_tensor(out=r1[rs][:], in0=r1[rs][:], in1=ln1w[:], op=ALU.mult), reads=[B_r1[rs], B_csm], writes=[B_r1[rs]])
                P.op("pool", lambda e, rs=rs: e.tensor_tensor(out=r1[rs][:], in0=r1[rs][:], in1=ln1b[:], op=ALU.add), reads=[B_r1[rs], B_csm], writes=[B_r1[rs]])
                if to + 1 < NT:
                    load_xtm(to + 1)
                    load_rope(to + 1)
                tok = P.dma("sp", C.x1s[to * 128:(to + 1) * 128, :], r1[rs][:], reads=[B_r1[rs]], writes=[C.B_x1s[to]], key=f"x1st{rs}")
                C.x1_toks.append(tok)


def build(mode="full"):
    nc = bass.Bass("TRN2", target_bir_lowering=False)
    C = Ctx()
    dt = lambda n, s, d=F32, kind="ExternalInput": nc.dram_tensor(n, s, d, kind=kind).ap()
    C.dbg = False
    if mode != "p2":
        C.xT = dt("xT", [D, 2 * T])
        C.xown = dt("xown", [T, D])
        C.win = dt("win", [D, NIN])
        C.wout = dt("wout", [D, D])
        C.mcur = dt("mcur", [128, 512])
        C.mbcur = dt("mbcur", [128, 512])
        C.mbprev = dt("mbprev", [128, 512])
        C.mbprev0 = dt("mbprev0", [128, 512])
        C.sel4 = dt("sel4", [4, 512])
        C.normw = dt("normw", [128, 512])
        C.ln1w = dt("ln1w", [128, D])
        C.ln1b = dt("ln1b", [128, D])
        C.cw = dt("cw", [128, 8, 4])
        C.cb = dt("cb", [128, 8])
        C.sinks = dt("sinks", [128, 8])
        C.gb = dt("gb", [8, 1])
        C.preib = dt("preib", [4, 1])
        C.prefs = dt("prefs", [4, 1])
        C.flag128 = dt("flag128", [128, 1])
        C.rope = dt("rope", [(NT + 1) * 128, 64])
    C.ident = dt("ident", [128, 128])
    if mode != "p1":
        C.ln2w = dt("ln2w", [128, D])
        C.ln2b = dt("ln2b", [128, D])
        C.rb = dt("rb", [128, 36])
        C.wr = dt("wr", [D, 36])
        C.weg = dt("weg", [NE, D, DE])
        C.weu = dt("weu", [NE, D, DE])
        C.wed = dt("wed", [NE, DE, D])
        C.out = dt("out", [T, D], kind="ExternalOutput")
    C.x1s = dt("x1s", [T, D], kind={"full": "Internal", "p1": "ExternalOutput", "p2": "ExternalInput"}[mode])
    C.B_x1s = [Buf(f"dram:x1s_{t}") for t in range(NT)]
    C.B_out = Buf("dram:out")
    C.out_toks = []
    C.x1_toks = []
    with ExitStack() as st:
        P = Prog(nc, st)
        if mode != "p2":
            with ExitStack() as st1:
                phase1(nc, P, C, st1)
                P.barrier()
        if mode != "p1":
            with ExitStack() as st2:
                phase2(nc, P, C, st2)
        P.wait("sp", C.out_toks + C.x1_toks)
        P.run()
    return nc


def _rep(v, n=128):
    return np.ascontiguousarray(np.broadcast_to(np.asarray(v, np.float32)[None, :], (n, v.shape[0])))


def host_inputs(inp, mode="full"):
    f32 = np.float32
    x = np.asarray(inp["x"], f32)
    w_in = np.asarray(inp["w_in"], f32)[0]
    qk, v, o, gi, gf, aq, ak, av = np.split(w_in, [1024, 1536, 2048, 2052, 2056, 2568, 2696], axis=1)
    win = np.ascontiguousarray(np.concatenate([qk, gi, gf, v, o, aq, ak, av], axis=1))
    conv_w = np.asarray(inp["conv_w"], f32)[0]
    cw = np.ascontiguousarray(conv_w.T.reshape(8, 128, 4).transpose(1, 0, 2))
    cb = np.ascontiguousarray(np.asarray(inp["conv_b"], f32)[0].reshape(8, 128).T)
    gb = np.ascontiguousarray(np.asarray(inp["mlstm_gate_bias"], f32)[0].reshape(8, 1))
    kq = np.arange(128)
    m = (kq[:, None] <= kq[None, :]).astype(f32)
    mcur = np.ascontiguousarray(np.tile(m, (1, 4)))
    NEG = -30000.0
    mbcur = np.ascontiguousarray((1.0 - mcur) * NEG)
    mbprev = np.ascontiguousarray(mcur * NEG)
    sel4 = np.zeros((4, 4, 128), f32)
    for h in range(4):
        sel4[h, h, :] = 1.0
    sel4 = sel4.reshape(4, 512)
    inv_freq = (10000.0 ** (-np.arange(32, dtype=f32) / 32.0)).astype(f32)
    wr = np.concatenate([np.asarray(inp["w_group_router"], f32)[0], np.asarray(inp["w_expert_router"], f32)[0]], axis=1)
    rbv = np.concatenate([np.asarray(inp["b_group_router"], f32)[0], np.asarray(inp["b_expert_router"], f32)[0]], axis=0)
    common = dict(ident=np.eye(128, dtype=f32))
    if mode != "p2":
        common.update(win=win, wout=np.ascontiguousarray(np.asarray(inp["w_out"], f32)[0]), mcur=mcur, mbcur=mbcur, mbprev=mbprev, sel4=sel4,
                      normw=_rep(np.asarray(inp["mlstm_norm_w"], f32)[0]), ln1w=_rep(np.asarray(inp["ln1_w"], f32)[0]),
                      ln1b=_rep(np.asarray(inp["ln1_b"], f32)[0]), cw=cw, cb=cb, sinks=_rep(np.asarray(inp["attn_sinks"], f32)[0]), gb=gb)
    if mode != "p1":
        common.update(ln2w=_rep(np.asarray(inp["ln2_w"], f32)[0]), ln2b=_rep(np.asarray(inp["ln2_b"], f32)[0]), rb=_rep(rbv),
                      wr=np.ascontiguousarray(wr), weg=np.asarray(inp["w_exp_gate"], f32)[0], weu=np.asarray(inp["w_exp_up"], f32)[0],
                      wed=np.asarray(inp["w_exp_down"], f32)[0])
    maps = []
    for c in range(NCORES):
        b, h = c // 2, c % 2
        d = dict(common)
        if mode != "p2":
            own = x[b, h * T:(h + 1) * T]
            pre = x[b, 0:T]
            d["xT"] = np.ascontiguousarray(np.concatenate([pre, own], axis=0).T)
            d["xown"] = np.ascontiguousarray(own)
            d["mbprev0"] = mbprev if h == 1 else np.full_like(mbprev, NEG)
            d["preib"] = np.full((4, 1), 0.0 if h == 1 else -1e30, f32)
            d["prefs"] = np.full((4, 1), 1.0 if h == 1 else 0.0, f32)
            d["flag128"] = np.full((128, 1), 1.0 if h == 1 else 0.0, f32)
            pos = (np.arange(-128, T) + h * T).astype(f32)
            ang = pos[:, None] * inv_freq[None, :]
            d["rope"] = np.ascontiguousarray(np.concatenate([np.cos(ang), np.sin(ang)], axis=1).astype(f32))
        maps.append(d)
    return maps


_NC_CACHE = {}


def kernel(**inputs):
    if "full" not in _NC_CACHE:
        _NC_CACHE["full"] = build("full")
    nc = _NC_CACHE["full"]
    maps = host_inputs(inputs, "full")
    res = run_bass_kernel_spmd(nc, maps, core_ids=list(range(NCORES)))
    out = np.stack([np.asarray(r["out"], np.float32) for r in res.results])
    return np.ascontiguousarray(out.reshape(4, 2 * T, D))
```

```python
import os
import types
from contextlib import ExitStack

import numpy as np
import concourse.bass as bass
import concourse.mybir as mybir
from concourse.bass_utils import run_bass_kernel_spmd

F32 = mybir.dt.float32
BF16 = mybir.dt.bfloat16
AF = mybir.ActivationFunctionType
ALU = mybir.AluOpType
AX = mybir.AxisListType

NCORES = 8
D = 1024
T = 4096
NT = T // 128
NE = 32
DE = 256
ALPHA = 2.0 ** 0.25
LN_EPS = 1e-5
TP = 1024
NPASS = T // TP
TPT = TP // 128
GP = TP // 512

ENGS = ("pe", "act", "dve", "pool", "sp")


def _freeze(fn):
    if fn.__closure__ is None:
        return fn
    cells = []
    for c in fn.__closure__:
        try:
            cells.append(types.CellType(c.cell_contents))
        except ValueError:
            cells.append(c)
    return types.FunctionType(fn.__code__, fn.__globals__, fn.__name__, fn.__defaults__, tuple(cells))


class _Probe:
    def __init__(self):
        self.n = 512
        self.passes = 1

    def __getattr__(self, name):
        def f(*a, **k):
            if name == "then_inc":
                return self
            ap = k.get("in_") if name == "bn_stats" else k.get("out", a[0] if a else None)
            try:
                sz = 1
                for d in ap.shape[1:]:
                    sz *= d
                self.n = sz
            except Exception:
                self.n = 512
            if name == "matmul":
                lhs = k.get("lhsT", a[1] if len(a) > 1 else None)
                try:
                    if lhs.dtype == F32:
                        self.passes = 4
                except Exception:
                    pass
            return self
        return f


class Buf:
    __slots__ = ("name", "w", "r", "const")

    def __init__(self, name, const=False):
        self.name = name
        self.w = None
        self.r = []
        self.const = const


class Prog:
    LAT = 120.0

    def __init__(self, nc, stack):
        self.nc = nc
        self.stack = stack
        self.nodes = []
        self.fence = {}
        self.since_fence = []

    def _deps(self, engine, reads, writes, extra):
        deps = set()
        for d in extra:
            if d is not None:
                deps.add(d)
        for b in reads:
            if b.w is not None:
                deps.add(b.w)
        for b in writes:
            if b.w is not None:
                deps.add(b.w)
            deps.update(b.r)
        if engine in self.fence:
            deps.add(self.fence[engine])
        return deps

    def _mark(self, nid, reads, writes):
        for b in reads:
            if not b.const:
                b.r.append(nid)
        for b in writes:
            b.w = nid
            b.r = []

    def _add(self, node, reads, writes, extra):
        nid = len(self.nodes)
        node["id"] = nid
        node["deps"] = self._deps(node["engine"], reads, writes, extra)
        self.nodes.append(node)
        self.since_fence.append(nid)
        self._mark(nid, reads, writes)
        return nid

    def op(self, engine, fn, reads=(), writes=(), extra=(), n=None):
        fns = [_freeze(f) for f in (fn if isinstance(fn, (list, tuple)) else [fn])]
        dur = 0.0
        for f in fns:
            pr = _Probe()
            f(pr)
            sz = pr.n if n is None else n
            if engine == "pe":
                dur += 25.0 + sz * pr.passes / 2.35
            elif engine == "pool":
                dur += 300.0 + sz * 1.7
            elif engine == "act":
                dur += 200.0 + sz * 0.9
            else:
                dur += 150.0 + sz * 1.25
        return self._add(dict(engine=engine, kind="op", fns=fns, dur=dur), reads, writes, extra)

    def dma(self, engine, out, in_, reads=(), writes=(), key=None, extra=(), nbytes=1 << 20):
        if key is None:
            key = (writes[0].name if (writes and not writes[0].name.startswith("dram:")) else reads[0].name + "_st")
        return self._add(dict(engine=engine, kind="dma", out=out, in_=in_, key=key, dur=2000.0 + nbytes / 150.0), reads, writes, extra)

    def wait(self, engine, toks):
        return self._add(dict(engine=engine, kind="wait", dur=50.0), (), (), toks)

    def barrier(self):
        prev = list(self.since_fence)
        ids = {}
        for e in ENGS:
            ids[e] = self._add(dict(engine=e, kind="wait", dur=50.0), (), (), prev)
        self.fence = ids
        self.since_fence = list(ids.values())

    def _schedule(self):
        import heapq
        nodes = self.nodes
        N = len(nodes)
        children = [[] for _ in range(N)]
        indeg = [0] * N
        for nd in nodes:
            indeg[nd["id"]] = len(nd["deps"])
            for d in nd["deps"]:
                children[d].append(nd["id"])
        finish = [0.0] * N
        free = {e: 0.0 for e in ENGS}
        blevel = [0.0] * N
        for nid in range(N - 1, -1, -1):
            m = 0.0
            for c in children[nid]:
                v = blevel[c] + self.LAT
                if v > m:
                    m = v
            blevel[nid] = m + nodes[nid]["dur"]
        PRIO = "cp"
        key_of = (lambda nid: (-blevel[nid], nid)) if PRIO == "cp" else (lambda nid: (nid, nid))
        byt = {e: [] for e in ENGS}
        byi = {e: [] for e in ENGS}
        order = {e: [] for e in ENGS}

        def push(nid):
            nd = nodes[nid]
            e = nd["engine"]
            rt = 0.0
            for d in nd["deps"]:
                f = finish[d] + (0.0 if nodes[d]["engine"] == e and nodes[d]["kind"] != "dma" else self.LAT)
                if f > rt:
                    rt = f
            heapq.heappush(byt[e], (rt, nid))

        for nd in nodes:
            if indeg[nd["id"]] == 0:
                push(nd["id"])
        done = 0
        while done < N:
            best = None
            for e in ENGS:
                while byt[e] and byt[e][0][0] <= free[e]:
                    rt, nid = heapq.heappop(byt[e])
                    heapq.heappush(byi[e], key_of(nid))
                if byi[e]:
                    cand = (free[e], byi[e][0][1], e, True)
                elif byt[e]:
                    cand = (byt[e][0][0], byt[e][0][1], e, False)
                else:
                    continue
                if best is None or cand[:2] < best[:2]:
                    best = cand
            start, nid, e, from_i = best
            if from_i:
                heapq.heappop(byi[e])
            else:
                heapq.heappop(byt[e])
            nd = nodes[nid]
            if nd["kind"] == "dma":
                free[e] = start + 70.0
                finish[nid] = start + nd["dur"]
            else:
                free[e] = start + nd["dur"]
                finish[nid] = free[e]
            order[e].append(nid)
            done += 1
            for c in children[nid]:
                indeg[c] -= 1
                if indeg[c] == 0:
                    push(c)
        self.est_total = max(finish) if N else 0.0
        return order

    def run(self):
        nodes = self.nodes
        order = self._schedule()
        sem = {e: self.stack.enter_context(self.nc.semaphore("s_" + e)) for e in ENGS}
        dsem, dcnt, dq = {}, {}, {}
        tok = [None] * len(nodes)
        for e in ENGS:
            c = 0
            for nid in order[e]:
                nd = nodes[nid]
                if nd["kind"] == "op":
                    c += 1
                    tok[nid] = (e, c)
                elif nd["kind"] == "dma":
                    k = nd["key"]
                    if k not in dsem:
                        dsem[k] = self.stack.enter_context(self.nc.semaphore("d_" + k.replace(":", "_")))
                        dcnt[k] = 0
                        dq[k] = e
                    assert dq[k] == e, f"dma key {k} used from two queues"
                    dcnt[k] += 16
                    tok[nid] = (k, dcnt[k])
        def expand(nid, acc, seen):
            for d in nodes[nid]["deps"]:
                if d in seen:
                    continue
                seen.add(d)
                if nodes[d]["kind"] == "wait":
                    expand(d, acc, seen)
                else:
                    acc.append(d)
        wait_closure = {}
        for nd in nodes:
            if nd["kind"] == "wait":
                acc = []
                expand(nd["id"], acc, set())
                best = {}
                for d in acc:
                    k, c = tok[d]
                    if best.get(k, 0) < c:
                        best[k] = c
                wait_closure[nd["id"]] = best

        def semof(k):
            return sem[k] if k in sem else dsem[k]

        streams = {}
        for e in ENGS:
            waited = {}
            items = []
            for nid in order[e]:
                nd = nodes[nid]
                need = {}
                for d in nd["deps"]:
                    if nodes[d]["kind"] == "wait":
                        for k, c in wait_closure[d].items():
                            if need.get(k, 0) < c:
                                need[k] = c
                    else:
                        k, c = tok[d]
                        if need.get(k, 0) < c:
                            need[k] = c
                waits = []
                for k, c in need.items():
                    if k == "pe" and e == "pe":
                        continue
                    if waited.get(k, 0) >= c:
                        continue
                    waited[k] = c
                    waits.append((k, c))
                items.append((nd, waits))
            streams[e] = items

        def play(e, eng):
            for nd, waits in streams[e]:
                for (k, c) in waits:
                    eng.wait_ge(semof(k), c)
                if nd["kind"] == "op":
                    fns = nd["fns"]
                    for f in fns[:-1]:
                        f(eng)
                    fns[-1](eng).then_inc(sem[e], 1)
                elif nd["kind"] == "dma":
                    eng.dma_start(out=nd["out"], in_=nd["in_"]).then_inc(dsem[nd["key"]], 16)

        with self.nc.Block() as block:
            @block.tensor
            def _(eng):
                play("pe", eng)

            @block.scalar
            def _(eng):
                play("act", eng)

            @block.vector
            def _(eng):
                play("dve", eng)

            @block.gpsimd
            def _(eng):
                play("pool", eng)

            @block.sync
            def _(eng):
                play("sp", eng)


class Ctx:
    pass


def phase2(nc, P, C, st):
    sb = lambda n, s, d: st.enter_context(nc.sbuf_tensor("sb_" + n, s, d))
    NBUF = 2 if NPASS > 2 else 1
    x1T_2 = [sb(f"x1T{i}", [128, 8, TP], BF16) for i in range(NBUF)]
    acc_2 = [sb(f"acc{i}", [128, TPT, D], F32) for i in range(NBUF)]
    NW = 4 if NPASS > 2 else 3
    wg = [sb(f"wg{i}", [128, 8, DE], BF16) for i in range(NW)]
    wu = [sb(f"wu{i}", [128, 8, DE], BF16) for i in range(NW)]
    wd = [sb(f"wd{i}", [128, 2, D], BF16) for i in range(NW)]
    x1t = [sb(f"x1t{i}", [128, D], F32) for i in range(2)]
    x1b = [sb(f"x1b{i}", [128, D], BF16) for i in range(2)]
    hT = [[sb(f"hT{i}{j}", [128, 512], BF16) for j in range(2)] for i in range(2)]
    sg = [sb(f"sg{i}", [128, 512], BF16) for i in range(3)]
    comb_2 = [sb(f"comb{i}", [128, TPT, NE], F32) for i in range(NBUF)]
    ln2w = sb("ln2w", [128, D], F32)
    ln2b = sb("ln2b", [128, D], F32)
    wr = sb("wr", [128, 8, 36], BF16)
    rb = sb("rb", [128, 36], F32)
    ident = sb("ident2", [128, 128], BF16)
    rt4 = [sb(f"rtr{i}", [128, 528], F32) for i in range(2)]
    ot = [sb(f"ot{i}", [128, D], F32) for i in range(2)]
    stats_2 = [sb(f"stats2{i}", [128, 2, 6], F32) for i in range(2)]
    mv_2 = [sb(f"mv2{i}", [128, 4], F32) for i in range(2)]
    ps = [st.enter_context(nc.psum_tensor(f"ps2_{i}", [128, 512], F32)) for i in range(8)]
    B_ps = [Buf(f"ps2_{i}") for i in range(8)]

    B_x1T_2 = [[Buf(f"x1T_{i}_{t}") for t in range(TPT)] for i in range(NBUF)]
    B_acc_2 = [[Buf(f"acc_{i}_{t}") for t in range(TPT)] for i in range(NBUF)]
    B_w = [Buf(f"w2_{i}") for i in range(NW)]
    B_x1t = [Buf(f"x1t_{i}") for i in range(2)]
    B_x1b = [Buf(f"x1b_{i}") for i in range(2)]
    B_hT = [[Buf(f"hT_{i}{j}") for j in range(2)] for i in range(2)]
    B_sg = [Buf(f"sg_{i}") for i in range(3)]
    B_comb_2 = [[Buf(f"comb_{i}_{t}") for t in range(TPT)] for i in range(NBUF)]
    B_c = Buf("const2", const=True)
    B_rt4 = [Buf(f"rt_{i}") for i in range(2)]
    B_ot = [Buf(f"ot_{i}") for i in range(2)]
    B_st2 = [Buf(f"stats2_{i}") for i in range(2)]

    ctoks = []
    ctoks.append(P.dma("sp", ln2w[:], C.ln2w, writes=[B_c], key="const2"))
    ctoks.append(P.dma("sp", ln2b[:], C.ln2b, writes=[B_c], key="const2"))
    ctoks.append(P.dma("sp", rb[:], C.rb, writes=[B_c], key="const2"))
    ctoks.append(P.dma("pool", wr[:], C.wr.rearrange("(c p) n -> p c n", p=128), writes=[B_c], key="const2p"))
    ctoks.append(P.dma("pool", ident[:], C.ident, writes=[B_c], key="const2p"))
    c_all = [ctoks[2], ctoks[4]]

    def load_expert(q):
        s = q % NW
        e = q % NE
        P.dma("pool", wg[s][:], C.weg[e].rearrange("(c p) n -> p c n", p=128), writes=[B_w[s]])
        P.dma("pool", wu[s][:], C.weu[e].rearrange("(c p) n -> p c n", p=128), writes=[B_w[s]])
        P.dma("pool", wd[s][:], C.wed[e].rearrange("(c p) n -> p c n", p=128), writes=[B_w[s]])

    LG, OHG, EIN, E2, OH1, OH2, CG = 0, 36, 40, 48, 56, 64, 72
    EX4 = 80
    SC = 96


    load_expert(0)
    load_expert(1)
    for p in range(NPASS):
        x1T, acc, comb = x1T_2[p % NBUF], acc_2[p % NBUF], comb_2[p % NBUF]
        B_x1T, B_acc, B_comb = B_x1T_2[p % NBUF], B_acc_2[p % NBUF], B_comb_2[p % NBUF]
        for t in range(TPT):
            tg = p * TPT + t
            s2 = t % 2
            P.dma("sp", x1t[s2][:], C.x1s[tg * 128:(tg + 1) * 128, :], reads=[C.B_x1s[tg]], writes=[B_x1t[s2]])
            P.op("act", lambda e, s2=s2, t=t: e.activation(out=acc[:, t, :], in_=x1t[s2][:], func=AF.Copy, scale=ALPHA),
                 reads=[B_x1t[s2]], writes=[B_acc[t]])
            P.op("act", lambda e, s2=s2: e.activation(out=x1b[s2][:], in_=x1t[s2][:], func=AF.Copy),
                 reads=[B_x1t[s2]], writes=[B_x1b[s2]])
            bk = t % 2
            tpv = ps[bk][:].bitcast(BF16) if hasattr(ps[bk][:], "bitcast") else None
            P.op("pe", [lambda e, c=c, s2=s2, tpv=tpv: e.transpose(tpv[:, c * 128:(c + 1) * 128], x1b[s2][:, c * 128:(c + 1) * 128], ident[:])
                        for c in range(8)],
                 reads=[B_x1b[s2], B_c], writes=[B_ps[bk]], extra=c_all)
            P.op("dve", lambda e, t=t, tpv=tpv: e.tensor_copy(out=x1T[:, :, t * 128:(t + 1) * 128],
                                                             in_=tpv[:, 0:1024].rearrange("p (c n) -> p c n", c=8)),
                 reads=[B_ps[bk]], writes=[B_x1T[t]])
            rbk = 2 + ((t // 4) % 2)
            tb = t % 4
            P.op("pe", [lambda e, c=c, t=t, rbk=rbk, tb=tb: e.matmul(ps[rbk][:, tb * 36:(tb + 1) * 36], x1T[:, c, t * 128:(t + 1) * 128], wr[:, c, :],
                                                                    start=(c == 0), stop=(c == 7)) for c in range(8)],
                 reads=[B_x1T[t], B_c], writes=[B_ps[rbk]], extra=c_all)
            if tb != 3:
                continue
            t0 = t - 3
            rt = rt4[(t // 4) % 2]
            R = [B_rt4[(t // 4) % 2]]
            NBT = 4
            L3 = rt[:, 0:144].rearrange("p (b k) -> p b k", b=NBT)
            G4 = L3[:, :, 0:4]
            EL = L3[:, :, 4:36].rearrange("p b (g e) -> p b g e", g=4)
            OHG = rt[:, 144:160].rearrange("p (b k) -> p b k", b=NBT)
            D4 = rt[:, 160:176].rearrange("p (b k) -> p b k", b=NBT)
            TMP = rt[:, 176:304].rearrange("p (b g e) -> p b g e", b=NBT, g=4)
            EIN = rt[:, 304:336].rearrange("p (b k) -> p b k", b=NBT)
            E2 = rt[:, 336:368].rearrange("p (b k) -> p b k", b=NBT)
            OH1 = rt[:, 368:400].rearrange("p (b k) -> p b k", b=NBT)
            OH2 = rt[:, 400:432].rearrange("p (b k) -> p b k", b=NBT)
            CG = rt[:, 432:464].rearrange("p (b k) -> p b k", b=NBT)
            T1 = rt[:, 464:496].rearrange("p (b k) -> p b k", b=NBT)
            sc = lambda i, rt=rt: rt[:, 496 + 4 * i:500 + 4 * i]
            bc = lambda ap, k: ap.unsqueeze(2).to_broadcast([128, NBT, k])
            P.op("dve", lambda e, rbk=rbk: e.tensor_tensor(out=L3, in0=ps[rbk][:, 0:144].rearrange("p (b k) -> p b k", b=NBT),
                                                          in1=rb[:].unsqueeze(1).to_broadcast([128, NBT, 36]), op=ALU.add),
                 reads=[B_ps[rbk], B_c], writes=R)
            P.op("dve", lambda e: e.reduce_max(out=sc(0), in_=G4, axis=AX.X), reads=R, writes=R)
            P.op("dve", lambda e: e.tensor_tensor(out=OHG, in0=G4, in1=bc(sc(0), 4), op=ALU.is_equal), reads=R, writes=R)
            P.op("dve", lambda e: e.tensor_tensor(out=D4, in0=G4, in1=bc(sc(0), 4), op=ALU.subtract), reads=R, writes=R)
            P.op("act", lambda e: e.activation(out=D4, in_=D4, func=AF.Exp), reads=R, writes=R)
            P.op("dve", lambda e: e.reduce_sum(out=sc(1), in_=D4, axis=AX.X), reads=R, writes=R)
            P.op("dve", lambda e: e.reciprocal(out=sc(1), in_=sc(1)), reads=R, writes=R)
            P.op("dve", lambda e: e.tensor_tensor(out=TMP, in0=EL, in1=OHG.unsqueeze(3).to_broadcast([128, NBT, 4, 8]), op=ALU.mult), reads=R, writes=R)
            P.op("dve", lambda e: e.reduce_sum(out=EIN, in_=TMP.rearrange("p b g e -> p b e g"), axis=AX.X), reads=R, writes=R)
            P.op("dve", lambda e: e.reduce_max(out=sc(2), in_=EIN, axis=AX.X), reads=R, writes=R)
            P.op("dve", lambda e: e.tensor_tensor(out=OH1, in0=EIN, in1=bc(sc(2), 8), op=ALU.is_equal), reads=R, writes=R)
            P.op("dve", lambda e: e.scalar_tensor_tensor(out=E2, in0=OH1, scalar=-1e30, in1=EIN, op0=ALU.mult, op1=ALU.add), reads=R, writes=R)
            P.op("dve", lambda e: e.reduce_max(out=sc(3), in_=E2, axis=AX.X), reads=R, writes=R)
            P.op("dve", lambda e: e.tensor_tensor(out=OH2, in0=E2, in1=bc(sc(3), 8), op=ALU.is_equal), reads=R, writes=R)
            P.op("dve", lambda e: e.tensor_tensor(out=sc(4), in0=sc(3), in1=sc(2), op=ALU.subtract), reads=R, writes=R)
            P.op("act", lambda e: e.activation(out=sc(4), in_=sc(4), func=AF.Exp), reads=R, writes=R)
            P.op("dve", lambda e: e.tensor_scalar(out=sc(4), in0=sc(4), scalar1=1.0, scalar2=None, op0=ALU.add), reads=R, writes=R)
            P.op("dve", lambda e: e.reciprocal(out=sc(4), in_=sc(4)), reads=R, writes=R)
            P.op("dve", lambda e: e.tensor_tensor(out=sc(5), in0=sc(4), in1=sc(1), op=ALU.mult), reads=R, writes=R)
            P.op("dve", lambda e: e.tensor_tensor(out=sc(6), in0=sc(1), in1=sc(5), op=ALU.subtract), reads=R, writes=R)
            P.op("dve", lambda e: e.tensor_tensor(out=CG, in0=OH1, in1=bc(sc(5), 8), op=ALU.mult), reads=R, writes=R)
            P.op("dve", lambda e: e.tensor_tensor(out=T1, in0=OH2, in1=bc(sc(6), 8), op=ALU.mult), reads=R, writes=R)
            P.op("dve", lambda e: e.tensor_tensor(out=CG, in0=CG, in1=T1, op=ALU.add), reads=R, writes=R)
            P.op("dve", lambda e, t0=t0: e.tensor_tensor(out=comb[:, t0:t0 + NBT, :].rearrange("p b (g e) -> p b g e", g=4),
                                                        in0=OHG.unsqueeze(3).to_broadcast([128, NBT, 4, 8]),
                                                        in1=CG.unsqueeze(2).to_broadcast([128, NBT, 4, 8]), op=ALU.mult),
                 reads=R, writes=B_comb[t0:t0 + NBT])

        if C.dbg:
            for t in range(TPT):
                tg = p * TPT + t
                C.out_toks.append(P.dma("sp", C.dbgc[tg * 128:(tg + 1) * 128, :], comb[:, t, :], reads=[B_comb[t]], writes=[C.B_dbg], key="dbgc"))
        steps = [(e, g) for e in range(NE - 2) for g in range(GP)] + [(e, g) for g in range(GP) for e in (NE - 2, NE - 1)]

        def emit_gu(k):
            e, g = steps[k]
            s = (p * NE + e) % NW
            for j in range(2):
                pr = (2 * k + j) % 2
                bg, bu = 2 * pr, 2 * pr + 1
                P.op("pe", [lambda en, c=c, j=j, s=s, g=g, bg=bg: en.matmul(ps[bg][:], wg[s][:, c, j * 128:(j + 1) * 128],
                                                                          x1T[:, c, g * 512:(g + 1) * 512], start=(c == 0), stop=(c == 7))
                            for c in range(8)],
                     reads=[B_w[s]] + B_x1T[4 * g:4 * g + 4], writes=[B_ps[bg]])
                P.op("pe", [lambda en, c=c, j=j, s=s, g=g, bu=bu: en.matmul(ps[bu][:], wu[s][:, c, j * 128:(j + 1) * 128],
                                                                          x1T[:, c, g * 512:(g + 1) * 512], start=(c == 0), stop=(c == 7))
                            for c in range(8)],
                     reads=[B_w[s]] + B_x1T[4 * g:4 * g + 4], writes=[B_ps[bu]])
                si = (2 * k + j) % 3
                P.op("act", lambda en, bg=bg, si=si: en.activation(out=sg[si][:], in_=ps[bg][:], func=AF.Silu),
                     reads=[B_ps[bg]], writes=[B_sg[si]])
                P.op("dve", lambda en, bu=bu, si=si, k=k, j=j: en.tensor_tensor(out=hT[k % 2][j][:], in0=ps[bu][:], in1=sg[si][:], op=ALU.mult),
                     reads=[B_ps[bu], B_sg[si]], writes=[B_hT[k % 2][j]])

        def emit_down(k):
            e, g = steps[k]
            s = (p * NE + e) % NW
            for tt in range(4):
                t = 4 * g + tt
                pr = (4 * k + tt) % 2
                b0, b1 = 4 + 2 * pr, 5 + 2 * pr
                fns = []
                for n, bk in ((0, b0), (1, b1)):
                    for j in range(2):
                        fns.append(lambda en, n=n, bk=bk, j=j, tt=tt, k=k, s=s: en.matmul(
                            ps[bk][:], hT[k % 2][j][:, tt * 128:(tt + 1) * 128], wd[s][:, j, n * 512:(n + 1) * 512],
                            start=(j == 0), stop=(j == 1)))
                P.op("pe", fns, reads=[B_w[s], B_hT[k % 2][0], B_hT[k % 2][1]], writes=[B_ps[b0], B_ps[b1]])
                for n, bk in ((0, b0), (1, b1)):
                    P.op("dve", lambda en, bk=bk, n=n, t=t, e=e: en.scalar_tensor_tensor(
                        out=acc[:, t, n * 512:(n + 1) * 512], in0=ps[bk][:], scalar=comb[:, t, e:e + 1],
                        in1=acc[:, t, n * 512:(n + 1) * 512], op0=ALU.mult, op1=ALU.add),
                        reads=[B_ps[bk], B_comb[t], B_acc[t]], writes=[B_acc[t]])

        for k in range(len(steps) + 1):
            if k < len(steps):
                emit_gu(k)
            if k >= 1:
                emit_down(k - 1)
            if k < len(steps):
                e, g = steps[k]
                q = p * NE + e
                if g == 0 and q + 2 < NPASS * NE:
                    load_expert(q + 2)

        for t in range(TPT):
            tg = p * TPT + t
            o = t % 2
            stats, mv, B_st = stats_2[t % 2], mv_2[t % 2], B_st2[t % 2]
            for hh in range(2):
                P.op("dve", lambda e, hh=hh, t=t: e.bn_stats(out=stats[:, hh, :], in_=acc[:, t, hh * 512:(hh + 1) * 512]),
                     reads=[B_acc[t]], writes=[B_st])
            P.op("dve", lambda e: e.bn_aggr(out=mv[:, 0:2], in_=stats[:]), reads=[B_st], writes=[B_st])
            P.op("dve", lambda e: e.tensor_scalar(out=mv[:, 2:3], in0=mv[:, 1:2], scalar1=LN_EPS, scalar2=None, op0=ALU.add),
                 reads=[B_st], writes=[B_st])
            P.op("act", lambda e: e.activation(out=mv[:, 2:3], in_=mv[:, 2:3], func=AF.Sqrt), reads=[B_st], writes=[B_st])
            P.op("dve", lambda e: e.reciprocal(out=mv[:, 2:3], in_=mv[:, 2:3]), reads=[B_st], writes=[B_st])
            P.op("dve", lambda e: e.tensor_scalar(out=mv[:, 3:4], in0=mv[:, 0:1], scalar1=-1.0, scalar2=mv[:, 2:3], op0=ALU.mult, op1=ALU.mult),
                 reads=[B_st], writes=[B_st])
            P.op("act", lambda e, t=t, o=o: e.activation(out=ot[o][:], in_=acc[:, t, :], func=AF.Identity, bias=mv[:, 3:4], scale=mv[:, 2:3]),
                 reads=[B_acc[t], B_st], writes=[B_ot[o]])
            P.op("pool", lambda e, o=o: e.tensor_tensor(out=ot[o][:], in0=ot[o][:], in1=ln2w[:], op=ALU.mult),
                 reads=[B_ot[o], B_c], writes=[B_ot[o]], extra=c_all)
            P.op("pool", lambda e, o=o: e.tensor_tensor(out=ot[o][:], in0=ot[o][:], in1=ln2b[:], op=ALU.add),
                 reads=[B_ot[o], B_c], writes=[B_ot[o]], extra=c_all)
            C.out_toks.append(P.dma("sp", C.out[tg * 128:(tg + 1) * 128, :], ot[o][:], reads=[B_ot[o]], writes=[C.B_out]))


QK0, GI0, GF0, V0, O0, AQ0, AK0, AV0, NIN = 0, 1024, 1028, 1032, 1544, 2056, 2568, 2696, 2824
DSC = 128.0 ** -0.5
NG = 16


def phase1(nc, P, C, st):
    sb = lambda n, s, d: st.enter_context(nc.sbuf_tensor("sb_" + n, s, d))
    win = sb("win", [128, 8, NIN], BF16)
    wout = sb("wout", [128, 8, D], BF16)
    xTg = [sb(f"xTg{i}", [128, 8, 512], BF16) for i in range(2)]
    praw = [sb(f"praw{i}", [128, 515], F32) for i in range(2)]
    halo = sb("halo", [128, 8, 3], F32)
    cv = [sb(f"cv{i}", [128, 512], F32) for i in range(2)]
    onecol = sb("onecol", [128, 1], F32)
    qkT2 = [sb(f"qkT{i}", [128, 8, 512], BF16) for i in range(2)]
    ones4 = sb("ones4", [4, 512], F32)
    gA2 = [sb(f"gA{i}", [4, 512], F32) for i in range(2)]
    gF = sb("gF", [4, 512], F32)
    gT = sb("gT", [4, 512], F32)
    gB = sb("gB", [4, 512], F32)
    gM = sb("gM", [4, 512], F32)
    gN2 = [sb(f"gN{i}", [4, 512], F32) for i in range(2)]
    gst = sb("gst", [4, 2], F32)
    M_bc2 = [sb(f"M_bc{i}", [128, 4, 512], F32) for i in range(2)]
    gtm = [sb(f"gtm{i}", [128, 8], F32) for i in range(2)]
    vext = [sb(f"vext{i}", [128, 4, 129], BF16) for i in range(2)]
    vaext = [sb(f"vaext{i}", [128, 2, 65], BF16) for i in range(2)]
    aqs2 = [sb(f"aqs{i}", [128, 512], F32) for i in range(2)]
    aks2 = [sb(f"aks{i}", [128, 256], F32) for i in range(2)]
    rt1 = sb("rt1", [128, 256], F32)
    rt2 = sb("rt2", [128, 256], F32)
    qrot = sb("qrot", [128, 512], BF16)
    krot = sb("krot", [128, 128], BF16)
    qT = sb("qT", [64, 1024], BF16)
    kT = [sb(f"kT{i}", [64, 256], BF16) for i in range(2)]
    eo2 = [sb(f"eo{i}", [128, 512], F32) for i in range(2)]
    WT2 = [sb(f"WT{i}", [128, 512], F32) for i in range(2)]
    WI2 = [sb(f"WI{i}", [128, 512], F32) for i in range(2)]
    PT2 = [sb(f"PT{i}", [128, 512], BF16) for i in range(2)]
    qTw2 = [sb(f"qTw{i}", [128, 512], BF16) for i in range(2)]
    kw2 = [sb(f"kw{i}", [128, 512], BF16) for i in range(2)]
    CT = sb("CT", [128, 4, 129], F32)
    CTb = sb("CTb", [128, 4, 129], BF16)
    hh = sb("hh", [128, 512], F32)
    sm = [sb(f"sm{i}", [128, 16], F32) for i in range(2)]
    sm2 = [sb(f"sm2{i}", [128, 24], F32) for i in range(2)]
    sm3 = [sb(f"sm3{i}", [128, 16], F32) for i in range(2)]
    st4 = sb("st4", [128, 4, 6], F32)
    mv4 = sb("mv4", [128, 4, 2], F32)
    PTa = [sb(f"PTa{i}", [128, 512], BF16) for i in range(4)]
    concat2 = [sb(f"concat{i}", [128, D], BF16) for i in range(2)]
    concatT = sb("concatT", [128, D], BF16)
    r1 = [sb(f"r1{i}", [128, D], F32) for i in range(2)]
    lst = sb("lst", [128, 2, 6], F32)
    lmv = sb("lmv", [128, 4], F32)
    ident = sb("ident1", [128, 128], BF16)
    identf = sb("identf1", [128, 128], F32)
    m128 = sb("m128", [128, 128], BF16)
    mbcur = sb("mbcur", [128, 512], BF16)
    mbprev = sb("mbprev", [128, 512], BF16)
    mbprev0 = sb("mbprev0", [128, 512], BF16)
    sel4 = sb("sel4", [4, 512], F32)
    normw = sb("normw", [128, 512], F32)
    ln1w = sb("ln1w", [128, D], F32)
    ln1b = sb("ln1b", [128, D], F32)
    cw = sb("cw", [128, 8, 4], F32)
    cb = sb("cb", [128, 8], F32)
    esink = sb("esink", [128, 8], F32)
    gbi = sb("gbi", [4, 1], F32)
    gbf = sb("gbf", [4, 1], F32)
    preib = sb("preib", [4, 1], F32)
    prefs = sb("prefs", [4, 1], F32)
    flag128 = sb("flag128", [128, 1], F32)
    ropeb = [sb(f"rope{i}", [128, 64], F32) for i in range(2)]

    ps = [st.enter_context(nc.psum_tensor(f"ps1_{i}", [128, 512], F32)) for i in range(8)]
    B_ps = [Buf(f"ps1_{i}") for i in range(8)]
    import os
    alias = {}
    cfg = "F=0,1,4,5;P=2,3;T=6,7"
    bank_set = {}
    for part in cfg.split(";"):
        k, v = part.split("=")
        if v in ("F", "P", "T"):
            alias[k] = v
        else:
            bank_set[k] = tuple(int(x) for x in v.split(","))
    bank_i = {"F": 0, "P": 0, "T": 0}

    def nb(pool):
        pool = alias.get(pool, pool)
        b = bank_set[pool][bank_i[pool] % len(bank_set[pool])]
        bank_i[pool] += 1
        return b

    mroles = ""
    mrole_map = {kv.split("=")[0]: int(kv.split("=")[1]) for kv in mroles.split(",")} if mroles else None

    def nbm(role):
        if mrole_map is None:
            return nb("F")
        return mrole_map[role]

    B_c = Buf("const1", const=True)
    B_wk, B_wv, B_wq, B_wo, B_wout, B_cid, B_csm, B_ones, B_esink = (Buf(n, const=True) for n in
        ("c_wk", "c_wv", "c_wq", "c_wo", "c_wout", "c_cid", "c_csm", "c_ones", "c_esink"))
    B_xTg = [Buf(f"xTg_{i}") for i in range(2)]
    B_praw = [Buf(f"praw_{i}") for i in range(2)]
    B_halo = [Buf(f"halo_{j}") for j in range(8)]
    B_cv = [Buf(f"cv_{i}") for i in range(2)]
    B_qkT2 = [[Buf(f"qkT_{i}_{j}") for j in range(8)] for i in range(2)]
    B_gF, B_gT, B_gB, B_gM, B_gst = (Buf(n) for n in ("gF", "gT", "gB", "gM", "gst"))
    B_gA2 = [Buf(f"gA_{i}") for i in range(2)]
    B_gN2 = [Buf(f"gN_{i}") for i in range(2)]
    B_Mbc2 = [[Buf(f"Mbc_{i}_{h}") for h in range(4)] for i in range(2)]
    B_gtm = [Buf(f"gtm_{i}") for i in range(2)]
    B_vext = [Buf(f"vext_{i}") for i in range(2)]
    B_vaext = [Buf(f"vaext_{i}") for i in range(2)]
    B_rt1, B_rt2, B_qrot, B_krot, B_qT = (Buf(n) for n in ("rt1", "rt2", "qrot", "krot", "qT"))
    B_aqs2 = [Buf(f"aqs_{i}") for i in range(2)]
    B_aks2 = [Buf(f"aks_{i}") for i in range(2)]
    B_rope = [Buf(f"rope_{i}") for i in range(2)]
    B_kT = [Buf(f"kT_{i}") for i in range(2)]
    B_CT, B_CTb, B_hh = (Buf(n) for n in ("CT", "CTb", "hh"))
    B_WT2, B_WI2, B_PT2, B_qTw2, B_kw2 = ([Buf(f"{n}_{i}") for i in range(2)] for n in ("WT", "WI", "PT", "qTw", "kw"))
    B_eo2 = [Buf(f"eo_{i}") for i in range(2)]
    B_sm = [Buf(f"sm_{i}") for i in range(2)]
    B_sm2 = [Buf(f"sm2_{i}") for i in range(2)]
    B_sm3 = [Buf(f"sm3_{i}") for i in range(2)]
    B_st4, B_mv4, B_lst = Buf("st4"), Buf("mv4"), Buf("lst")
    B_PTa = [Buf(f"PTa_{i}") for i in range(4)]
    B_concat2 = [Buf(f"concat_{i}") for i in range(2)]
    B_concatT = Buf("concatT")
    B_r1 = [Buf(f"r1_{i}") for i in range(2)]

    ck = []
    for (dst, src) in ((identf[:], C.ident), (sel4[:], C.sel4),
                       (normw[:], C.normw), (ln1w[:], C.ln1w), (ln1b[:], C.ln1b), (cw[:], C.cw), (cb[:], C.cb), (esink[:], C.sinks),
                       (gbi[:], C.gb[0:4, :]), (gbf[:], C.gb[4:8, :]), (preib[:], C.preib), (prefs[:], C.prefs), (flag128[:], C.flag128)):
        ck.append(P.dma("sp", dst, src, writes=[B_csm], key="const1", nbytes=65536))
    wsrc = C.win.rearrange("(c p) n -> p c n", p=128)

    def load_x(G):
        s = G % 2
        P.dma("pool", xTg[s][:], C.xT[:, G * 512:(G + 1) * 512].rearrange("(c p) n -> p c n", p=128), writes=[B_xTg[s]])

    P.dma("pool", ident[:], C.ident, writes=[B_cid], key="const1i", nbytes=65536)
    P.dma("pool", win[:, :, 512:1032], wsrc[:, :, 512:1032], writes=[B_wk], key="const1k")
    load_x(0)
    P.dma("pool", win[:, :, V0:V0 + 512], wsrc[:, :, V0:V0 + 512], writes=[B_wv], key="const1v")
    for (dst, src) in ((m128[:], C.mcur[:, 0:128]), (mbcur[:], C.mbcur), (mbprev[:], C.mbprev), (mbprev0[:], C.mbprev0)):
        P.dma("pool", dst, src, writes=[B_cid], key="const1i", nbytes=65536)
    load_x(1)
    P.dma("pool", win[:, :, 0:512], wsrc[:, :, 0:512], writes=[B_wq], key="const1q")
    P.dma("pool", win[:, :, O0:NIN], wsrc[:, :, O0:NIN], writes=[B_wo], key="const1o")
    P.dma("pool", wout[:], C.wout.rearrange("(c p) n -> p c n", p=128), writes=[B_wout], key="const1w")
    call = []
    t_init = []
    t_init.append(P.op("pool", lambda e: e.memset(onecol[:], 1.0), writes=[B_ones]))
    t_init.append(P.op("pool", lambda e: e.memset(ones4[:], 1.0), writes=[B_ones]))
    t_init.append(P.op("pool", lambda e: e.memset(halo[:], 0.0), writes=B_halo))
    t_init.append(P.op("pool", lambda e: e.memset(gst[:], 0.0), writes=[B_gst]))
    for i in range(2):
        t_init.append(P.op("pool", lambda e, i=i: e.memset(M_bc2[i][:], 0.0), writes=B_Mbc2[i]))
    t_init.append(P.op("pool", lambda e: e.memset(CT[:], 0.0), writes=[B_CT]))
    for i in range(2):
        t_init.append(P.op("pool", lambda e, i=i: e.memset(vext[i][:], 1.0), writes=[B_vext[i]]))
        t_init.append(P.op("pool", lambda e, i=i: e.memset(vaext[i][:], 1.0), writes=[B_vaext[i]]))
    t_init.append(P.op("act", lambda e: e.activation(out=esink[:], in_=esink[:], func=AF.Exp), reads=[B_csm], writes=[B_esink]))

    def load_xtm(to):
        s = to % 2
        P.dma("sp", r1[s][:], C.xown[to * 128:(to + 1) * 128, :], writes=[B_r1[s]], nbytes=524288)

    def load_rope(to):
        s = (to + 1) % 2
        P.dma("sp", ropeb[s][:], C.rope[(to + 1) * 128:(to + 2) * 128, :], writes=[B_rope[s]], nbytes=32768)

    cnt = {"praw": 0, "cv": 0, "tile": 0, "E": 0}
    load_xtm(0)
    load_rope(-1)
    load_rope(0)

    def rstd_chain(var_ap, out_ap, Bs):
        P.op("dve", lambda e: e.tensor_scalar(out=out_ap, in0=var_ap, scalar1=LN_EPS, scalar2=None, op0=ALU.add), reads=Bs, writes=Bs)
        P.op("act", lambda e: e.activation(out=out_ap, in_=out_ap, func=AF.Ln), reads=Bs, writes=Bs)
        P.op("act", lambda e: e.activation(out=out_ap, in_=out_ap, func=AF.Exp, scale=-0.5), reads=Bs, writes=Bs)

    for G in range(NG):
        pre = G < 8
        xs = G % 2
        gs, gp = G % 2, (G + 1) % 2
        qkT, B_qkT = qkT2[gs], B_qkT2[gs]
        gA, gN, B_gA, B_gN = gA2[gs], gN2[gs], B_gA2[gs], B_gN2[gs]
        M_bc, B_Mbc = M_bc2[gs], B_Mbc2[gs]
        M_bcp, B_Mbcp = M_bc2[gp], B_Mbc2[gp]
        if 1 <= G and G + 1 < NG:
            load_x(G + 1)
        if G == 8:
            P.op("pool", lambda e: e.tensor_scalar(out=halo[:], in0=halo[:], scalar1=flag128[:, 0:1], scalar2=None, op0=ALU.mult),
                 reads=B_halo + [B_csm], writes=B_halo)
        for j in ((list(range(4, 8)) + ([0, 1, 2, 3] if G == 7 else [])) if pre else range(8)):
            halo_only = pre and j < 4
            bk = nb("F")
            P.op("pe", [lambda e, c=c, j=j, bk=bk: e.matmul(ps[bk][:], win[:, c, j * 128:(j + 1) * 128], xTg[xs][:, c, :],
                                                           start=(c == 0), stop=(c == 7)) for c in range(8)],
                 reads=[B_xTg[xs], B_wk if j >= 4 else B_wq], writes=[B_ps[bk]])
            r = cnt["praw"] % 2
            cnt["praw"] += 1
            P.op("pool", lambda e, r=r, j=j: e.tensor_copy(out=praw[r][:, 0:3], in_=halo[:, j, :]), reads=[B_halo[j]], writes=[B_praw[r]])
            P.op("act", lambda e, r=r, bk=bk: e.activation(out=praw[r][:, 3:515], in_=ps[bk][:], func=AF.Copy),
                 reads=[B_ps[bk]], writes=[B_praw[r]])
            P.op("pool", lambda e, r=r, j=j: e.tensor_copy(out=halo[:, j, :], in_=praw[r][:, 512:515]), reads=[B_praw[r]], writes=[B_halo[j]])
            if halo_only:
                continue
            c2 = cnt["cv"] % 2
            cnt["cv"] += 1
            P.op("act", lambda e, r=r, j=j, c2=c2: e.activation(out=cv[c2][:], in_=praw[r][:, 3:515], func=AF.Identity, scale=cw[:, j, 3:4], bias=cb[:, j:j + 1]),
                 reads=[B_praw[r], B_csm], writes=[B_cv[c2]])
            for tap in (2, 1, 0):
                P.op("dve", lambda e, r=r, j=j, c2=c2, tap=tap: e.scalar_tensor_tensor(out=cv[c2][:], in0=praw[r][:, tap:tap + 512], scalar=cw[:, j, tap:tap + 1],
                                                                                      in1=cv[c2][:], op0=ALU.mult, op1=ALU.add),
                     reads=[B_praw[r], B_cv[c2]], writes=[B_cv[c2]])
            P.op("act", lambda e, j=j, c2=c2: e.activation(out=qkT[:, j, :], in_=cv[c2][:], func=AF.Silu), reads=[B_cv[c2]], writes=[B_qkT[j]])
        bi = nb("F")
        P.op("pe", [lambda e, c=c: e.matmul(ps[bi][0:4, :], win[:, c, GI0:GI0 + 4], xTg[xs][:, c, :], start=(c == 0), stop=(c == 7)) for c in range(8)],
             reads=[B_xTg[xs], B_wk], writes=[B_ps[bi]])
        if pre:
            P.op("dve", lambda e: e.tensor_scalar(out=gA[:], in0=ps[bi][0:4, :], scalar1=gbi[:, 0:1], scalar2=preib[:, 0:1], op0=ALU.add, op1=ALU.add),
                 reads=[B_ps[bi], B_csm], writes=[B_gA])
        else:
            P.op("dve", lambda e: e.tensor_scalar(out=gA[:], in0=ps[bi][0:4, :], scalar1=gbi[:, 0:1], scalar2=None, op0=ALU.add),
                 reads=[B_ps[bi], B_csm], writes=[B_gA])
        bf = nb("F")
        P.op("pe", [lambda e, c=c: e.matmul(ps[bf][0:4, :], win[:, c, GF0:GF0 + 4], xTg[xs][:, c, :], start=(c == 0), stop=(c == 7)) for c in range(8)],
             reads=[B_xTg[xs], B_wk], writes=[B_ps[bf]])
        P.op("dve", lambda e: e.tensor_scalar(out=gF[:], in0=ps[bf][0:4, :], scalar1=gbf[:, 0:1], scalar2=None, op0=ALU.add),
             reads=[B_ps[bf], B_csm], writes=[B_gF])
        P.op("dve", lambda e: e.tensor_scalar(out=gT[:], in0=gF[:], scalar1=-1.0, scalar2=None, op0=ALU.mult), reads=[B_gF], writes=[B_gT])
        P.op("dve", lambda e: e.tensor_tensor(out=gT[:], in0=gT[:], in1=gF[:], op=ALU.max), reads=[B_gF, B_gT], writes=[B_gT])
        P.op("act", lambda e: e.activation(out=gT[:], in_=gT[:], func=AF.Exp, scale=-1.0), reads=[B_gT], writes=[B_gT])
        P.op("dve", lambda e: e.tensor_scalar(out=gT[:], in0=gT[:], scalar1=1.0, scalar2=None, op0=ALU.add), reads=[B_gT], writes=[B_gT])
        P.op("act", lambda e: e.activation(out=gT[:], in_=gT[:], func=AF.Ln), reads=[B_gT], writes=[B_gT])
        P.op("dve", lambda e: e.scalar_tensor_tensor(out=gF[:], in0=gF[:], scalar=0.0, in1=gT[:], op0=ALU.min, op1=ALU.subtract),
             reads=[B_gF, B_gT], writes=[B_gF])
        if pre:
            P.op("dve", lambda e: e.tensor_scalar(out=gF[:], in0=gF[:], scalar1=prefs[:, 0:1], scalar2=None, op0=ALU.mult), reads=[B_gF, B_csm], writes=[B_gF])
        P.op("dve", lambda e: e.tensor_tensor_scan(out=gB[:], data0=ones4[:], data1=gF[:], initial=gst[:, 0:1], op0=ALU.mult, op1=ALU.add),
             reads=[B_gF, B_gst, B_ones], writes=[B_gB])
        P.op("pool", lambda e: e.tensor_tensor(out=gA[:], in0=gA[:], in1=gB[:], op=ALU.subtract), reads=[B_gA, B_gB], writes=[B_gA])
        P.op("dve", lambda e: e.tensor_tensor_scan(out=gM[:], data0=ones4[:], data1=gA[:], initial=gst[:, 1:2], op0=ALU.mult, op1=ALU.max),
             reads=[B_gA, B_gst, B_ones], writes=[B_gM])
        P.op("pool", lambda e: e.tensor_copy(out=gst[:, 0:1], in_=gB[:, 511:512]), reads=[B_gB], writes=[B_gst])
        P.op("pool", lambda e: e.tensor_copy(out=gst[:, 1:2], in_=gM[:, 511:512]), reads=[B_gM], writes=[B_gst])
        P.op("dve", lambda e: e.scalar_tensor_tensor(out=gN[:], in0=gB[:], scalar=-1.0, in1=gM[:], op0=ALU.mult, op1=ALU.subtract),
             reads=[B_gB, B_gM], writes=[B_gN])
        for h in range(4):
            bk = nb("F")
            if pre:
                P.op("pe", lambda e, h=h, bk=bk: e.matmul(ps[bk][:, 0:4], sel4[:, h * 128:(h + 1) * 128], gM[:, 127:512:128], start=True, stop=True),
                     reads=[B_gM, B_csm], writes=[B_ps[bk]], n=16)
                P.op("act", lambda e, h=h, bk=bk: e.activation(out=M_bc[:, h, 127:512:128], in_=ps[bk][:, 0:4], func=AF.Copy), reads=[B_ps[bk]], writes=[B_Mbc[h]])
            else:
                P.op("pe", lambda e, h=h, bk=bk: e.matmul(ps[bk][:], sel4[:, h * 128:(h + 1) * 128], gM[:], start=True, stop=True),
                     reads=[B_gM, B_csm], writes=[B_ps[bk]], n=2048)
                P.op("act", lambda e, h=h, bk=bk: e.activation(out=M_bc[:, h, :], in_=ps[bk][:], func=AF.Copy), reads=[B_ps[bk]], writes=[B_Mbc[h]])

        for t in range(4):
            cols = slice(t * 128, (t + 1) * 128)
            to = (G - 8) * 4 + t
            halo_tile = (G == 7 and t == 3)
            own = not pre
            ti = cnt["tile"]
            cnt["tile"] += 1
            vs = ti % 2
            ts_ = ti % 2
            eo, B_eo = eo2[ti % 2], B_eo2[ti % 2]
            aqs, B_aqs = aqs2[ti % 2], B_aqs2[ti % 2]
            aks, B_aks = aks2[ti % 2], B_aks2[ti % 2]
            concat, B_concat = concat2[ti % 2], B_concat2[ti % 2]
            WT, WI, PT, qTw, kw = WT2[ti % 2], WI2[ti % 2], PT2[ti % 2], qTw2[ti % 2], kw2[ti % 2]
            B_WT, B_WI, B_PT, B_qTw, B_kw = B_WT2[ti % 2], B_WI2[ti % 2], B_PT2[ti % 2], B_qTw2[ti % 2], B_kw2[ti % 2]
            bv = nb("P")
            fns = []
            wr_ = [B_ps[bv]]
            if own:
                bo = nb("P")
                wr_.append(B_ps[bo])
            for c in range(8):
                fns.append(lambda e, c=c, bv=bv: e.matmul(ps[bv][:], xTg[xs][:, c, cols], win[:, c, V0:V0 + 512], start=(c == 0), stop=(c == 7)))
                if own:
                    fns.append(lambda e, c=c, bo=bo: e.matmul(ps[bo][:], xTg[xs][:, c, cols], win[:, c, O0:O0 + 512], start=(c == 0), stop=(c == 7)))
            P.op("pe", fns, reads=[B_xTg[xs], B_wv, B_wo], writes=wr_)
            if own:
                P.op("dve", lambda e, bv=bv, vs=vs: e.tensor_copy(out=vext[vs][:, :, 0:128], in_=ps[bv][:].rearrange("p (h n) -> p h n", h=4)),
                     reads=[B_ps[bv]], writes=[B_vext[vs]])
            else:
                P.op("act", lambda e, bv=bv, vs=vs: e.activation(out=vext[vs][:, :, 0:128], in_=ps[bv][:].rearrange("p (h n) -> p h n", h=4), func=AF.Copy),
                     reads=[B_ps[bv]], writes=[B_vext[vs]])
            if own:
                P.op("act", lambda e, bo=bo: e.activation(out=eo[:], in_=ps[bo][:], func=AF.Exp, scale=-1.0), reads=[B_ps[bo]], writes=[B_eo])
                P.op("act", lambda e: e.activation(out=eo[:], in_=eo[:], func=AF.Ln, bias=onecol[:, 0:1], scale=1.0), reads=[B_eo, B_ones], writes=[B_eo])
                P.op("act", lambda e: e.activation(out=eo[:], in_=eo[:], func=AF.Exp, scale=-1.0), reads=[B_eo], writes=[B_eo])
            if own or halo_tile:
                fns = []
                wr_ = []
                bkv = nb("P")
                wr_.append(B_ps[bkv])
                if own:
                    bq = nb("P")
                    wr_.append(B_ps[bq])
                for c in range(8):
                    if own:
                        fns.append(lambda e, c=c, bq=bq: e.matmul(ps[bq][:], xTg[xs][:, c, cols], win[:, c, AQ0:AQ0 + 512], start=(c == 0), stop=(c == 7)))
                    fns.append(lambda e, c=c, bkv=bkv: e.matmul(ps[bkv][:, 0:256], xTg[xs][:, c, cols], win[:, c, AK0:AK0 + 256], start=(c == 0), stop=(c == 7)))
                P.op("pe", fns, reads=[B_xTg[xs], B_wo], writes=wr_)
                if own:
                    P.op("act", lambda e, bq=bq: e.activation(out=aqs[:], in_=ps[bq][:], func=AF.Copy), reads=[B_ps[bq]], writes=[B_aqs])
                P.op("act", lambda e, bkv=bkv: e.activation(out=aks[:], in_=ps[bkv][:, 0:256], func=AF.Copy), reads=[B_ps[bkv]], writes=[B_aks], n=256)

            bg = nbm("B")
            P.op("pe", [lambda e, bg=bg: e.matmul(ps[bg][:, 0:4], gA[:, cols], identf[0:4, 0:4], start=True, stop=True),
                        lambda e, bg=bg: e.matmul(ps[bg][:, 4:8], gN[:, cols], identf[0:4, 0:4], start=True, stop=True)],
                 reads=[B_gA, B_gN, B_csm], writes=[B_ps[bg]], n=8)
            P.op("dve", lambda e, bg=bg: e.tensor_copy(out=gtm[ts_][:], in_=ps[bg][:, 0:8]), reads=[B_ps[bg]], writes=[B_gtm[ts_]], n=8)
            ccol = t * 128 + 127
            Mp = M_bcp[:, :, 511] if t == 0 else M_bc[:, :, t * 128 - 1]
            B_Mp = B_Mbcp if t == 0 else B_Mbc

            if own:
                bs = nbm("S")
                P.op("pe", [lambda e, h=h, bs=bs: e.matmul(ps[bs][:, h * 128:(h + 1) * 128], qkT[:, 4 + h, cols], qkT[:, h, cols], start=True, stop=True)
                            for h in range(4)], reads=B_qkT, writes=[B_ps[bs]])
                for h in range(4):
                    P.op("act", lambda e, h=h: e.activation(out=WT[:, h * 128:(h + 1) * 128], in_=M_bc[:, h, cols], func=AF.Exp, scale=-1.0,
                                                            bias=gtm[ts_][:, h:h + 1]),
                         reads=[B_Mbc[h], B_gtm[ts_]], writes=[B_WT])
                P.op("pool", lambda e: e.tensor_tensor(out=WT[:].rearrange("p (h n) -> p h n", h=4), in0=WT[:].rearrange("p (h n) -> p h n", h=4),
                                                      in1=m128[:].unsqueeze(1).to_broadcast([128, 4, 128]), op=ALU.mult), reads=[B_WT, B_cid], writes=[B_WT])
                P.op("dve", lambda e, bs=bs: e.scalar_tensor_tensor(out=PT[:], in0=ps[bs][:], scalar=DSC, in1=WT[:], op0=ALU.mult, op1=ALU.mult),
                     reads=[B_ps[bs], B_WT], writes=[B_PT])
                for h in range(4):
                    mp_h = M_bcp[:, h, 511:512] if t == 0 else M_bc[:, h, t * 128 - 1:t * 128]
                    P.op("act", lambda e, h=h, mp_h=mp_h: e.activation(out=WI[:, h * 128:(h + 1) * 128], in_=M_bc[:, h, cols], func=AF.Exp, scale=-1.0, bias=mp_h),
                         reads=[B_Mbc[h]] + B_Mp, writes=[B_WI])
                P.op("pool", lambda e: e.tensor_tensor(out=qTw[:].rearrange("p (h n) -> p h n", h=4), in0=qkT[:, 0:4, cols],
                                                      in1=WI[:].rearrange("p (h n) -> p h n", h=4), op=ALU.mult),
                     reads=B_qkT[0:4] + [B_WI], writes=[B_qTw])
                bn = [nbm("C"), nbm("D")]
                fns = []
                for h in range(4):
                    o_ = (h % 2) * 129
                    fns.append(lambda e, h=h, o_=o_: e.matmul(ps[bn[h // 2]][:, o_:o_ + 129], qTw[:, h * 128:(h + 1) * 128], CTb[:, h, :], start=True, stop=False))
                    fns.append(lambda e, h=h, o_=o_: e.matmul(ps[bn[h // 2]][:, o_:o_ + 129], PT[:, h * 128:(h + 1) * 128], vext[vs][:, h, :], start=False, stop=True))
                P.op("pe", fns, reads=[B_qTw, B_CTb, B_PT, B_vext[vs]], writes=[B_ps[bn[0]], B_ps[bn[1]]])
                s2 = sm2[ts_]
                S2 = [B_sm2[ts_]]
                P.op("act", lambda e, s2=s2: e.activation(out=s2[:, 0:4], in_=gtm[ts_][:, 4:8], func=AF.Exp), reads=[B_gtm[ts_]], writes=S2)
                for b in range(2):
                    P.op("dve", lambda e, b=b, s2=s2: e.tensor_scalar(out=s2[:, 20 + 2 * b:22 + 2 * b], in0=ps[bn[b]][:, 128:258:129], scalar1=-1.0, scalar2=None, op0=ALU.mult),
                         reads=[B_ps[bn[b]]] + S2, writes=S2)
                    P.op("dve", lambda e, b=b, s2=s2: e.tensor_tensor(out=s2[:, 20 + 2 * b:22 + 2 * b], in0=s2[:, 20 + 2 * b:22 + 2 * b], in1=ps[bn[b]][:, 128:258:129], op=ALU.max),
                         reads=[B_ps[bn[b]]] + S2, writes=S2)
                    P.op("dve", lambda e, b=b, s2=s2: e.tensor_tensor(out=s2[:, 4 + 2 * b:6 + 2 * b], in0=s2[:, 20 + 2 * b:22 + 2 * b], in1=s2[:, 2 * b:2 * b + 2], op=ALU.max),
                         reads=S2, writes=S2)
                P.op("dve", lambda e, s2=s2: e.reciprocal(out=s2[:, 8:12], in_=s2[:, 4:8]), reads=S2, writes=S2)
                for h in range(4):
                    o_ = (h % 2) * 129
                    P.op("act", lambda e, h=h, o_=o_, s2=s2: e.activation(out=hh[:, h * 128:(h + 1) * 128], in_=ps[bn[h // 2]][:, o_:o_ + 128], func=AF.Identity,
                                                                         scale=s2[:, 8 + h:9 + h]),
                         reads=[B_ps[bn[h // 2]]] + S2, writes=[B_hh])
                for h in range(4):
                    P.op("dve", lambda e, h=h: e.bn_stats(out=st4[:, h, :], in_=hh[:, h * 128:(h + 1) * 128]), reads=[B_hh], writes=[B_st4])
                for h in range(4):
                    P.op("dve", lambda e, h=h: e.bn_aggr(out=mv4[:, h, :], in_=st4[:, h, :]), reads=[B_st4], writes=[B_mv4])
                rstd_chain(mv4[:, :, 1], s2[:, 12:16], S2 + [B_mv4])
                P.op("dve", lambda e, s2=s2: e.scalar_tensor_tensor(out=s2[:, 16:20], in0=mv4[:, :, 0], scalar=-1.0, in1=s2[:, 12:16], op0=ALU.mult, op1=ALU.mult),
                     reads=S2 + [B_mv4], writes=S2)
                for h in range(4):
                    P.op("act", lambda e, h=h, s2=s2: e.activation(out=hh[:, h * 128:(h + 1) * 128], in_=hh[:, h * 128:(h + 1) * 128], func=AF.Identity,
                                                                 scale=s2[:, 12 + h:13 + h], bias=s2[:, 16 + h:17 + h]),
                         reads=[B_hh] + S2, writes=[B_hh])
                P.op("pool", lambda e: e.tensor_tensor(out=hh[:], in0=hh[:], in1=normw[:], op=ALU.mult), reads=[B_hh, B_csm], writes=[B_hh])
                P.op("dve", lambda e: e.tensor_tensor(out=concat[:, 0:512], in0=hh[:], in1=eo[:], op=ALU.mult), reads=[B_hh, B_eo], writes=[B_concat])

            btp = nbm("B")
            tpv = ps[btp][:].bitcast(BF16)[:, 512:1024]
            P.op("pe", [lambda e, h=h, tpv=tpv: e.transpose(tpv[:, h * 128:(h + 1) * 128], qkT[:, 4 + h, cols], ident[:]) for h in range(4)],
                 reads=B_qkT[4:8] + [B_cid], writes=[B_ps[btp]], n=128)
            s1 = sm[ts_]
            S1 = [B_sm[ts_]]
            P.op("dve", lambda e, s1=s1: e.tensor_tensor(out=s1[:, 0:4], in0=gtm[ts_][:, 0:4], in1=M_bc[:, :, ccol], op=ALU.subtract),
                 reads=[B_gtm[ts_]] + B_Mbc, writes=S1)
            P.op("dve", lambda e, s1=s1, Mp=Mp: e.tensor_tensor(out=s1[:, 4:8], in0=Mp, in1=M_bc[:, :, ccol], op=ALU.subtract),
                 reads=B_Mp + B_Mbc, writes=S1)
            P.op("act", lambda e, s1=s1: e.activation(out=s1[:, 8:16], in_=s1[:, 0:8], func=AF.Exp), reads=S1, writes=S1)
            for h in range(4):
                P.op("dve", lambda e, h=h, s1=s1, tpv=tpv: e.tensor_scalar(out=kw[:, h * 128:(h + 1) * 128], in0=tpv[:, h * 128:(h + 1) * 128],
                                                                          scalar1=s1[:, 8 + h:9 + h], scalar2=DSC, op0=ALU.mult, op1=ALU.mult),
                     reads=[B_ps[btp]] + S1, writes=[B_kw], n=128)
            bu = [nbm("C"), nbm("D")]
            fns = []
            for h in range(4):
                o_ = (h % 2) * 129
                fns.append(lambda e, h=h, o_=o_: e.matmul(ps[bu[h // 2]][:, o_:o_ + 129], kw[:, h * 128:(h + 1) * 128], vext[vs][:, h, :], start=True, stop=True))
            P.op("pe", fns, reads=[B_kw, B_vext[vs]], writes=[B_ps[bu[0]], B_ps[bu[1]]])
            for h in range(4):
                o_ = (h % 2) * 129
                P.op("dve", lambda e, h=h, o_=o_, s1=s1: e.scalar_tensor_tensor(out=CT[:, h, :], in0=CT[:, h, :], scalar=s1[:, 12 + h:13 + h],
                                                                               in1=ps[bu[h // 2]][:, o_:o_ + 129], op0=ALU.mult, op1=ALU.add),
                     reads=[B_CT, B_ps[bu[h // 2]]] + S1, writes=[B_CT])
            if own or halo_tile:
                P.op("pool", lambda e: e.tensor_copy(out=CTb[:], in_=CT[:]), reads=[B_CT], writes=[B_CTb])

            if own or halo_tile:
                ks = (to + 1) % 2
                kp = to % 2
                rsl = (to + 1) % 2
                cosb = lambda n: ropeb[rsl][:, 0:32].unsqueeze(1).to_broadcast([128, n, 32])
                sinb = lambda n: ropeb[rsl][:, 32:64].unsqueeze(1).to_broadcast([128, n, 32])

                def do_rope(src, dst, n, Bsrc, Bdst):
                    v = src.rearrange("p (h t d) -> p h t d", h=n, t=2)
                    o = dst.rearrange("p (h t d) -> p h t d", h=n, t=2)
                    a = rt1[:, 0:n * 32].rearrange("p (h d) -> p h d", h=n)
                    b = rt2[:, 0:n * 32].rearrange("p (h d) -> p h d", h=n)
                    cn, sn = cosb(n), sinb(n)
                    P.op("pool", lambda e: e.tensor_tensor(out=a, in0=v[:, :, 0, :], in1=cn, op=ALU.mult), reads=[Bsrc, B_rope[rsl]], writes=[B_rt1])
                    P.op("pool", lambda e: e.tensor_tensor(out=b, in0=v[:, :, 1, :], in1=sn, op=ALU.mult), reads=[Bsrc, B_rope[rsl]], writes=[B_rt2])
                    P.op("pool", lambda e: e.tensor_tensor(out=o[:, :, 0, :], in0=a, in1=b, op=ALU.subtract), reads=[B_rt1, B_rt2], writes=[Bdst])
                    P.op("pool", lambda e: e.tensor_tensor(out=a, in0=v[:, :, 0, :], in1=sn, op=ALU.mult), reads=[Bsrc, B_rope[rsl]], writes=[B_rt1])
                    P.op("pool", lambda e: e.tensor_tensor(out=b, in0=v[:, :, 1, :], in1=cn, op=ALU.mult), reads=[Bsrc, B_rope[rsl]], writes=[B_rt2])
                    P.op("pool", lambda e: e.tensor_tensor(out=o[:, :, 1, :], in0=a, in1=b, op=ALU.add), reads=[B_rt1, B_rt2], writes=[Bdst])

                do_rope(aks[:, 0:128], krot[:], 2, B_aks, B_krot)
                bk2 = nb("T")
                tk = ps[bk2][:].bitcast(BF16)
                P.op("pe", [lambda e, j=j, tk=tk: e.transpose(tk[0:64, j * 128:(j + 1) * 128], krot[:, j * 64:(j + 1) * 64], ident[:]) for j in range(2)],
                     reads=[B_krot, B_cid], writes=[B_ps[bk2]])
                P.op("act", lambda e, tk=tk, ks=ks: e.activation(out=kT[ks][:], in_=tk[0:64, 0:256], func=AF.Copy), reads=[B_ps[bk2]], writes=[B_kT[ks]])
                P.op("pool", lambda e, ks=ks: e.tensor_copy(out=vaext[ks][:, :, 0:64], in_=aks[:, 128:256].rearrange("p (h d) -> p h d", h=2)),
                     reads=[B_aks], writes=[B_vaext[ks]])
            if own:
                do_rope(aqs[:], qrot[:], 8, B_aqs, B_qrot)
                bq2 = nb("T")
                tq = ps[bq2][:].bitcast(BF16)
                P.op("pe", [lambda e, hq=hq, tq=tq: e.transpose(tq[0:64, hq * 128:(hq + 1) * 128], qrot[:, hq * 64:(hq + 1) * 64], ident[:]) for hq in range(8)],
                     reads=[B_qrot, B_cid], writes=[B_ps[bq2]])
                P.op("act", lambda e, tq=tq: e.activation(out=qT[:], in_=tq[0:64, 0:1024], func=AF.Copy), reads=[B_ps[bq2]], writes=[B_qT])
                for j in range(2):
                    for kb in range(2):
                        slot = kp if kb == 0 else ks
                        bsc = nb("T")
                        msk = mbcur if kb == 1 else (mbprev0 if to == 0 else mbprev)
                        P.op("pe", [lambda e, j=j, slot=slot, bsc=bsc: e.matmul(ps[bsc][:], kT[slot][:, j * 128:(j + 1) * 128], qT[:, j * 512:(j + 1) * 512], start=True, stop=False),
                                    lambda e, bsc=bsc, msk=msk: e.matmul(ps[bsc][:], ident[:], msk[:], start=False, stop=True)],
                             reads=[B_kT[slot], B_qT, B_cid], writes=[B_ps[bsc]])
                        P.op("act", lambda e, bsc=bsc, j=j, kb=kb: e.activation(out=PTa[2 * j + kb][:], in_=ps[bsc][:], func=AF.Exp, scale=0.125),
                             reads=[B_ps[bsc]], writes=[B_PTa[2 * j + kb]])
                bpv = [nb("T"), nb("T")]
                for j in range(2):
                    fns = []
                    for g in range(4):
                        fns.append(lambda e, j=j, g=g: e.matmul(ps[bpv[j]][:, g * 65:(g + 1) * 65], PTa[2 * j][:, g * 128:(g + 1) * 128], vaext[kp][:, j, :], start=True, stop=False))
                        fns.append(lambda e, j=j, g=g: e.matmul(ps[bpv[j]][:, g * 65:(g + 1) * 65], PTa[2 * j + 1][:, g * 128:(g + 1) * 128], vaext[ks][:, j, :], start=False, stop=True))
                    P.op("pe", fns, reads=[B_PTa[2 * j], B_PTa[2 * j + 1], B_vaext[kp], B_vaext[ks]], writes=[B_ps[bpv[j]]])
                s3 = sm3[ts_]
                S3 = [B_sm3[ts_]]
                for j in range(2):
                    P.op("dve", lambda e, j=j, s3=s3: e.tensor_tensor(out=s3[:, 4 * j:4 * j + 4], in0=ps[bpv[j]][:, 64:260:65], in1=esink[:, 4 * j:4 * j + 4], op=ALU.add),
                         reads=[B_ps[bpv[j]], B_esink], writes=S3)
                P.op("dve", lambda e, s3=s3: e.reciprocal(out=s3[:, 8:16], in_=s3[:, 0:8]), reads=S3, writes=S3)
                for j in range(2):
                    P.op("dve", lambda e, j=j, s3=s3: e.tensor_tensor(out=concat[:, 512 + 256 * j:768 + 256 * j].rearrange("p (g d) -> p g d", g=4),
                                                                     in0=ps[bpv[j]][:, 0:260].rearrange("p (g d) -> p g d", g=4)[:, :, 0:64],
                                                                     in1=s3[:, 8 + 4 * j:12 + 4 * j].unsqueeze(2).to_broadcast([128, 4, 64]), op=ALU.mult),
                         reads=[B_ps[bpv[j]]] + S3, writes=[B_concat])

                bct = nb("T")
                tcv = ps[bct][:].bitcast(BF16)
                P.op("pe", [lambda e, c=c, tcv=tcv: e.transpose(tcv[:, c * 128:(c + 1) * 128], concat[:, c * 128:(c + 1) * 128], ident[:]) for c in range(8)],
                     reads=[B_concat, B_cid], writes=[B_ps[bct]])
                P.op("act", lambda e, tcv=tcv: e.activation(out=concatT[:], in_=tcv[:, 0:1024], func=AF.Copy), reads=[B_ps[bct]], writes=[B_concatT])
                by = [nb("T"), nb("T")]
                fns = []
                for n in range(2):
                    for c in range(8):
                        fns.append(lambda e, n=n, c=c: e.matmul(ps[by[n]][:], concatT[:, c * 128:(c + 1) * 128], wout[:, c, n * 512:(n + 1) * 512], start=(c == 0), stop=(c == 7)))
                P.op("pe", fns, reads=[B_concatT, B_wout], writes=[B_ps[by[0]], B_ps[by[1]]])
                rs = to % 2
                for n in range(2):
                    P.op("dve", lambda e, n=n, rs=rs: e.scalar_tensor_tensor(out=r1[rs][:, n * 512:(n + 1) * 512], in0=r1[rs][:, n * 512:(n + 1) * 512], scalar=ALPHA,
                                                                            in1=ps[by[n]][:], op0=ALU.mult, op1=ALU.add),
                         reads=[B_r1[rs], B_ps[by[n]]], writes=[B_r1[rs]])
                for n in range(2):
                    P.op("dve", lambda e, n=n, rs=rs: e.bn_stats(out=lst[:, n, :], in_=r1[rs][:, n * 512:(n + 1) * 512]), reads=[B_r1[rs]], writes=[B_lst])
                P.op("dve", lambda e: e.bn_aggr(out=lmv[:, 0:2], in_=lst[:]), reads=[B_lst], writes=[B_lst])
                rstd_chain(lmv[:, 1:2], lmv[:, 2:3], [B_lst])
                P.op("dve", lambda e: e.tensor_scalar(out=lmv[:, 3:4], in0=lmv[:, 0:1], scalar1=-1.0, scalar2=lmv[:, 2:3], op0=ALU.mult, op1=ALU.mult),
                     reads=[B_lst], writes=[B_lst])
                P.op("act", lambda e, rs=rs: e.activation(out=r1[rs][:], in_=r1[rs][:], func=AF.Identity, bias=lmv[:, 3:4], scale=lmv[:, 2:3]),
                     reads=[B_r1[rs], B_lst], writes=[B_r1[rs]])
                P.op("pool", lambda e, rs=rs: e.tensor_tensor(out=r1[rs][:], in0=r1[rs][:], in1=ln1w[:], op=ALU.mult), reads=[B_r1[rs], B_csm], writes=[B_r1[rs]])
                P.op("pool", lambda e, rs=rs: e.tensor_tensor(out=r1[rs][:], in0=r1[rs][:], in1=ln1b[:], op=ALU.add), reads=[B_r1[rs], B_csm], writes=[B_r1[rs]])
                if to + 1 < NT:
                    load_xtm(to + 1)
                    load_rope(to + 1)
                tok = P.dma("sp", C.x1s[to * 128:(to + 1) * 128, :], r1[rs][:], reads=[B_r1[rs]], writes=[C.B_x1s[to]], key=f"x1st{rs}")
                C.x1_toks.append(tok)


def build(mode="full"):
    nc = bass.Bass("TRN2", target_bir_lowering=False)
    C = Ctx()
    dt = lambda n, s, d=F32, kind="ExternalInput": nc.dram_tensor(n, s, d, kind=kind).ap()
    C.dbg = False
    if mode != "p2":
        C.xT = dt("xT", [D, 2 * T])
        C.xown = dt("xown", [T, D])
        C.win = dt("win", [D, NIN])
        C.wout = dt("wout", [D, D])
        C.mcur = dt("mcur", [128, 512])
        C.mbcur = dt("mbcur", [128, 512])
        C.mbprev = dt("mbprev", [128, 512])
        C.mbprev0 = dt("mbprev0", [128, 512])
        C.sel4 = dt("sel4", [4, 512])
        C.normw = dt("normw", [128, 512])
        C.ln1w = dt("ln1w", [128, D])
        C.ln1b = dt("ln1b", [128, D])
        C.cw = dt("cw", [128, 8, 4])
        C.cb = dt("cb", [128, 8])
        C.sinks = dt("sinks", [128, 8])
        C.gb = dt("gb", [8, 1])
        C.preib = dt("preib", [4, 1])
        C.prefs = dt("prefs", [4, 1])
        C.flag128 = dt("flag128", [128, 1])
        C.rope = dt("rope", [(NT + 1) * 128, 64])
    C.ident = dt("ident", [128, 128])
    if mode != "p1":
        C.ln2w = dt("ln2w", [128, D])
        C.ln2b = dt("ln2b", [128, D])
        C.rb = dt("rb", [128, 36])
        C.wr = dt("wr", [D, 36])
        C.weg = dt("weg", [NE, D, DE])
        C.weu = dt("weu", [NE, D, DE])
        C.wed = dt("wed", [NE, DE, D])
        C.out = dt("out", [T, D], kind="ExternalOutput")
    C.x1s = dt("x1s", [T, D], kind={"full": "Internal", "p1": "ExternalOutput", "p2": "ExternalInput"}[mode])
    C.B_x1s = [Buf(f"dram:x1s_{t}") for t in range(NT)]
    C.B_out = Buf("dram:out")
    C.out_toks = []
    C.x1_toks = []
    with ExitStack() as st:
        P = Prog(nc, st)
        if mode != "p2":
            with ExitStack() as st1:
                phase1(nc, P, C, st1)
                P.barrier()
        if mode != "p1":
            with ExitStack() as st2:
                phase2(nc, P, C, st2)
        P.wait("sp", C.out_toks + C.x1_toks)
        P.run()
    return nc


def _rep(v, n=128):
    return np.ascontiguousarray(np.broadcast_to(np.asarray(v, np.float32)[None, :], (n, v.shape[0])))


def host_inputs(inp, mode="full"):
    f32 = np.float32
    x = np.asarray(inp["x"], f32)
    w_in = np.asarray(inp["w_in"], f32)[0]
    qk, v, o, gi, gf, aq, ak, av = np.split(w_in, [1024, 1536, 2048, 2052, 2056, 2568, 2696], axis=1)
    win = np.ascontiguousarray(np.concatenate([qk, gi, gf, v, o, aq, ak, av], axis=1))
    conv_w = np.asarray(inp["conv_w"], f32)[0]
    cw = np.ascontiguousarray(conv_w.T.reshape(8, 128, 4).transpose(1, 0, 2))
    cb = np.ascontiguousarray(np.asarray(inp["conv_b"], f32)[0].reshape(8, 128).T)
    gb = np.ascontiguousarray(np.asarray(inp["mlstm_gate_bias"], f32)[0].reshape(8, 1))
    kq = np.arange(128)
    m = (kq[:, None] <= kq[None, :]).astype(f32)
    mcur = np.ascontiguousarray(np.tile(m, (1, 4)))
    NEG = -30000.0
    mbcur = np.ascontiguousarray((1.0 - mcur) * NEG)
    mbprev = np.ascontiguousarray(mcur * NEG)
    sel4 = np.zeros((4, 4, 128), f32)
    for h in range(4):
        sel4[h, h, :] = 1.0
    sel4 = sel4.reshape(4, 512)
    inv_freq = (10000.0 ** (-np.arange(32, dtype=f32) / 32.0)).astype(f32)
    wr = np.concatenate([np.asarray(inp["w_group_router"], f32)[0], np.asarray(inp["w_expert_router"], f32)[0]], axis=1)
    rbv = np.concatenate([np.asarray(inp["b_group_router"], f32)[0], np.asarray(inp["b_expert_router"], f32)[0]], axis=0)
    common = dict(ident=np.eye(128, dtype=f32))
    if mode != "p2":
        common.update(win=win, wout=np.ascontiguousarray(np.asarray(inp["w_out"], f32)[0]), mcur=mcur, mbcur=mbcur, mbprev=mbprev, sel4=sel4,
                      normw=_rep(np.asarray(inp["mlstm_norm_w"], f32)[0]), ln1w=_rep(np.asarray(inp["ln1_w"], f32)[0]),
                      ln1b=_rep(np.asarray(inp["ln1_b"], f32)[0]), cw=cw, cb=cb, sinks=_rep(np.asarray(inp["attn_sinks"], f32)[0]), gb=gb)
    if mode != "p1":
        common.update(ln2w=_rep(np.asarray(inp["ln2_w"], f32)[0]), ln2b=_rep(np.asarray(inp["ln2_b"], f32)[0]), rb=_rep(rbv),
                      wr=np.ascontiguousarray(wr), weg=np.asarray(inp["w_exp_gate"], f32)[0], weu=np.asarray(inp["w_exp_up"], f32)[0],
                      wed=np.asarray(inp["w_exp_down"], f32)[0])
    maps = []
    for c in range(NCORES):
        b, h = c // 2, c % 2
        d = dict(common)
        if mode != "p2":
            own = x[b, h * T:(h + 1) * T]
            pre = x[b, 0:T]
            d["xT"] = np.ascontiguousarray(np.concatenate([pre, own], axis=0).T)
            d["xown"] = np.ascontiguousarray(own)
            d["mbprev0"] = mbprev if h == 1 else np.full_like(mbprev, NEG)
            d["preib"] = np.full((4, 1), 0.0 if h == 1 else -1e30, f32)
            d["prefs"] = np.full((4, 1), 1.0 if h == 1 else 0.0, f32)
            d["flag128"] = np.full((128, 1), 1.0 if h == 1 else 0.0, f32)
            pos = (np.arange(-128, T) + h * T).astype(f32)
            ang = pos[:, None] * inv_freq[None, :]
            d["rope"] = np.ascontiguousarray(np.concatenate([np.cos(ang), np.sin(ang)], axis=1).astype(f32))
        maps.append(d)
    return maps


_NC_CACHE = {}


def kernel(**inputs):
    if "full" not in _NC_CACHE:
        _NC_CACHE["full"] = build("full")
    nc = _NC_CACHE["full"]
    maps = host_inputs(inputs, "full")
    res = run_bass_kernel_spmd(nc, maps, core_ids=list(range(NCORES)))
    out = np.stack([np.asarray(r["out"], np.float32) for r in res.results])
    return np.ascontiguousarray(out.reshape(4, 2 * T, D))
```

```python
import os
import types
from contextlib import ExitStack

import numpy as np
import concourse.bass as bass
import concourse.mybir as mybir
from concourse.bass_utils import run_bass_kernel_spmd

F32 = mybir.dt.float32
BF16 = mybir.dt.bfloat16
AF = mybir.ActivationFunctionType
ALU = mybir.AluOpType
AX = mybir.AxisListType

NCORES = 8
D = 1024
T = 4096
NT = T // 128
NE = 32
DE = 256
ALPHA = 2.0 ** 0.25
LN_EPS = 1e-5
TP = 1024
NPASS = T // TP
TPT = TP // 128
GP = TP // 512

ENGS = ("pe", "act", "dve", "pool", "sp")


def _freeze(fn):
    if fn.__closure__ is None:
        return fn
    cells = []
    for c in fn.__closure__:
        try:
            cells.append(types.CellType(c.cell_contents))
        except ValueError:
            cells.append(c)
    return types.FunctionType(fn.__code__, fn.__globals__, fn.__name__, fn.__defaults__, tuple(cells))


class _Probe:
    def __init__(self):
        self.n = 512
        self.passes = 1

    def __getattr__(self, name):
        def f(*a, **k):
            if name == "then_inc":
                return self
            ap = k.get("in_") if name == "bn_stats" else k.get("out", a[0] if a else None)
            try:
                sz = 1
                for d in ap.shape[1:]:
                    sz *= d
                self.n = sz
            except Exception:
                self.n = 512
            if name == "matmul":
                lhs = k.get("lhsT", a[1] if len(a) > 1 else None)
                try:
                    if lhs.dtype == F32:
                        self.passes = 4
                except Exception:
                    pass
            return self
        return f


class Buf:
    __slots__ = ("name", "w", "r", "const")

    def __init__(self, name, const=False):
        self.name = name
        self.w = None
        self.r = []
        self.const = const


class Prog:
    LAT = 120.0

    def __init__(self, nc, stack):
        self.nc = nc
        self.stack = stack
        self.nodes = []
        self.fence = {}
        self.since_fence = []

    def _deps(self, engine, reads, writes, extra):
        deps = set()
        for d in extra:
            if d is not None:
                deps.add(d)
        for b in reads:
            if b.w is not None:
                deps.add(b.w)
        for b in writes:
            if b.w is not None:
                deps.add(b.w)
            deps.update(b.r)
        if engine in self.fence:
            deps.add(self.fence[engine])
        return deps

    def _mark(self, nid, reads, writes):
        for b in reads:
            if not b.const:
                b.r.append(nid)
        for b in writes:
            b.w = nid
            b.r = []

    def _add(self, node, reads, writes, extra):
        nid = len(self.nodes)
        node["id"] = nid
        node["deps"] = self._deps(node["engine"], reads, writes, extra)
        self.nodes.append(node)
        self.since_fence.append(nid)
        self._mark(nid, reads, writes)
        return nid

    def op(self, engine, fn, reads=(), writes=(), extra=(), n=None):
        fns = [_freeze(f) for f in (fn if isinstance(fn, (list, tuple)) else [fn])]
        dur = 0.0
        for f in fns:
            pr = _Probe()
            f(pr)
            sz = pr.n if n is None else n
            if engine == "pe":
                dur += 25.0 + sz * pr.passes / 2.35
            elif engine == "pool":
                dur += 300.0 + sz * 1.7
            elif engine == "act":
                dur += 200.0 + sz * 0.9
            else:
                dur += 150.0 + sz * 1.25
        return self._add(dict(engine=engine, kind="op", fns=fns, dur=dur), reads, writes, extra)

    def dma(self, engine, out, in_, reads=(), writes=(), key=None, extra=(), nbytes=1 << 20):
        if key is None:
            key = (writes[0].name if (writes and not writes[0].name.startswith("dram:")) else reads[0].name + "_st")
        return self._add(dict(engine=engine, kind="dma", out=out, in_=in_, key=key, dur=2000.0 + nbytes / 150.0), reads, writes, extra)

    def wait(self, engine, toks):
        return self._add(dict(engine=engine, kind="wait", dur=50.0), (), (), toks)

    def barrier(self):
        prev = list(self.since_fence)
        ids = {}
        for e in ENGS:
            ids[e] = self._add(dict(engine=e, kind="wait", dur=50.0), (), (), prev)
        self.fence = ids
        self.since_fence = list(ids.values())

    def _schedule(self):
        import heapq
        nodes = self.nodes
        N = len(nodes)
        children = [[] for _ in range(N)]
        indeg = [0] * N
        for nd in nodes:
            indeg[nd["id"]] = len(nd["deps"])
            for d in nd["deps"]:
                children[d].append(nd["id"])
        finish = [0.0] * N
        free = {e: 0.0 for e in ENGS}
        blevel = [0.0] * N
        for nid in range(N - 1, -1, -1):
            m = 0.0
            for c in children[nid]:
                v = blevel[c] + self.LAT
                if v > m:
                    m = v
            blevel[nid] = m + nodes[nid]["dur"]
        PRIO = "cp"
        key_of = (lambda nid: (-blevel[nid], nid)) if PRIO == "cp" else (lambda nid: (nid, nid))
        byt = {e: [] for e in ENGS}
        byi = {e: [] for e in ENGS}
        order = {e: [] for e in ENGS}

        def push(nid):
            nd = nodes[nid]
            e = nd["engine"]
            rt = 0.0
            for d in nd["deps"]:
                f = finish[d] + (0.0 if nodes[d]["engine"] == e and nodes[d]["kind"] != "dma" else self.LAT)
                if f > rt:
                    rt = f
            heapq.heappush(byt[e], (rt, nid))

        for nd in nodes:
            if indeg[nd["id"]] == 0:
                push(nd["id"])
        done = 0
        while done < N:
            best = None
            for e in ENGS:
                while byt[e] and byt[e][0][0] <= free[e]:
                    rt, nid = heapq.heappop(byt[e])
                    heapq.heappush(byi[e], key_of(nid))
                if byi[e]:
                    cand = (free[e], byi[e][0][1], e, True)
                elif byt[e]:
                    cand = (byt[e][0][0], byt[e][0][1], e, False)
                else:
                    continue
                if best is None or cand[:2] < best[:2]:
                    best = cand
            start, nid, e, from_i = best
            if from_i:
                heapq.heappop(byi[e])
            else:
                heapq.heappop(byt[e])
            nd = nodes[nid]
            if nd["kind"] == "dma":
                free[e] = start + 70.0
                finish[nid] = start + nd["dur"]
            else:
                free[e] = start + nd["dur"]
                finish[nid] = free[e]
            order[e].append(nid)
            done += 1
            for c in children[nid]:
                indeg[c] -= 1
                if indeg[c] == 0:
                    push(c)
        self.est_total = max(finish) if N else 0.0
        return order

    def run(self):
        nodes = self.nodes
        order = self._schedule()
        sem = {e: self.stack.enter_context(self.nc.semaphore("s_" + e)) for e in ENGS}
        dsem, dcnt, dq = {}, {}, {}
        tok = [None] * len(nodes)
        for e in ENGS:
            c = 0
            for nid in order[e]:
                nd = nodes[nid]
                if nd["kind"] == "op":
                    c += 1
                    tok[nid] = (e, c)
                elif nd["kind"] == "dma":
                    k = nd["key"]
                    if k not in dsem:
                        dsem[k] = self.stack.enter_context(self.nc.semaphore("d_" + k.replace(":", "_")))
                        dcnt[k] = 0
                        dq[k] = e
                    assert dq[k] == e, f"dma key {k} used from two queues"
                    dcnt[k] += 16
                    tok[nid] = (k, dcnt[k])
        def expand(nid, acc, seen):
            for d in nodes[nid]["deps"]:
                if d in seen:
                    continue
                seen.add(d)
                if nodes[d]["kind"] == "wait":
                    expand(d, acc, seen)
                else:
                    acc.append(d)
        wait_closure = {}
        for nd in nodes:
            if nd["kind"] == "wait":
                acc = []
                expand(nd["id"], acc, set())
                best = {}
                for d in acc:
                    k, c = tok[d]
                    if best.get(k, 0) < c:
                        best[k] = c
                wait_closure[nd["id"]] = best

        def semof(k):
            return sem[k] if k in sem else dsem[k]

        streams = {}
        for e in ENGS:
            waited = {}
            items = []
            for nid in order[e]:
                nd = nodes[nid]
                need = {}
                for d in nd["deps"]:
                    if nodes[d]["kind"] == "wait":
                        for k, c in wait_closure[d].items():
                            if need.get(k, 0) < c:
                                need[k] = c
                    else:
                        k, c = tok[d]
                        if need.get(k, 0) < c:
                            need[k] = c
                waits = []
                for k, c in need.items():
                    if k == "pe" and e == "pe":
                        continue
                    if waited.get(k, 0) >= c:
                        continue
                    waited[k] = c
                    waits.append((k, c))
                items.append((nd, waits))
            streams[e] = items

        def play(e, eng):
            for nd, waits in streams[e]:
                for (k, c) in waits:
                    eng.wait_ge(semof(k), c)
                if nd["kind"] == "op":
                    fns = nd["fns"]
                    for f in fns[:-1]:
                        f(eng)
                    fns[-1](eng).then_inc(sem[e], 1)
                elif nd["kind"] == "dma":
                    eng.dma_start(out=nd["out"], in_=nd["in_"]).then_inc(dsem[nd["key"]], 16)

        with self.nc.Block() as block:
            @block.tensor
            def _(eng):
                play("pe", eng)

            @block.scalar
            def _(eng):
                play("act", eng)

            @block.vector
            def _(eng):
                play("dve", eng)

            @block.gpsimd
            def _(eng):
                play("pool", eng)

            @block.sync
            def _(eng):
                play("sp", eng)


class Ctx:
    pass


def phase2(nc, P, C, st):
    sb = lambda n, s, d: st.enter_context(nc.sbuf_tensor("sb_" + n, s, d))
    NBUF = 2 if NPASS > 2 else 1
    x1T_2 = [sb(f"x1T{i}", [128, 8, TP], BF16) for i in range(NBUF)]
    acc_2 = [sb(f"acc{i}", [128, TPT, D], F32) for i in range(NBUF)]
    NW = 4 if NPASS > 2 else 3
    wg = [sb(f"wg{i}", [128, 8, DE], BF16) for i in range(NW)]
    wu = [sb(f"wu{i}", [128, 8, DE], BF16) for i in range(NW)]
    wd = [sb(f"wd{i}", [128, 2, D], BF16) for i in range(NW)]
    x1t = [sb(f"x1t{i}", [128, D], F32) for i in range(2)]
    x1b = [sb(f"x1b{i}", [128, D], BF16) for i in range(2)]
    hT = [[sb(f"hT{i}{j}", [128, 512], BF16) for j in range(2)] for i in range(2)]
    sg = [sb(f"sg{i}", [128, 512], BF16) for i in range(3)]
    comb_2 = [sb(f"comb{i}", [128, TPT, NE], F32) for i in range(NBUF)]
    ln2w = sb("ln2w", [128, D], F32)
    ln2b = sb("ln2b", [128, D], F32)
    wr = sb("wr", [128, 8, 36], BF16)
    rb = sb("rb", [128, 36], F32)
    ident = sb("ident2", [128, 128], BF16)
    rt4 = [sb(f"rtr{i}", [128, 528], F32) for i in range(2)]
    ot = [sb(f"ot{i}", [128, D], F32) for i in range(2)]
    stats_2 = [sb(f"stats2{i}", [128, 2, 6], F32) for i in range(2)]
    mv_2 = [sb(f"mv2{i}", [128, 4], F32) for i in range(2)]
    ps = [st.enter_context(nc.psum_tensor(f"ps2_{i}", [128, 512], F32)) for i in range(8)]
    B_ps = [Buf(f"ps2_{i}") for i in range(8)]

    B_x1T_2 = [[Buf(f"x1T_{i}_{t}") for t in range(TPT)] for i in range(NBUF)]
    B_acc_2 = [[Buf(f"acc_{i}_{t}") for t in range(TPT)] for i in range(NBUF)]
    B_w = [Buf(f"w2_{i}") for i in range(NW)]
    B_x1t = [Buf(f"x1t_{i}") for i in range(2)]
    B_x1b = [Buf(f"x1b_{i}") for i in range(2)]
    B_hT = [[Buf(f"hT_{i}{j}") for j in range(2)] for i in range(2)]
    B_sg = [Buf(f"sg_{i}") for i in range(3)]
    B_comb_2 = [[Buf(f"comb_{i}_{t}") for t in range(TPT)] for i in range(NBUF)]
    B_c = Buf("const2", const=True)
    B_rt4 = [Buf(f"rt_{i}") for i in range(2)]
    B_ot = [Buf(f"ot_{i}") for i in range(2)]
    B_st2 = [Buf(f"stats2_{i}") for i in range(2)]

    ctoks = []
    ctoks.append(P.dma("sp", ln2w[:], C.ln2w, writes=[B_c], key="const2"))
    ctoks.append(P.dma("sp", ln2b[:], C.ln2b, writes=[B_c], key="const2"))
    ctoks.append(P.dma("sp", rb[:], C.rb, writes=[B_c], key="const2"))
    ctoks.append(P.dma("pool", wr[:], C.wr.rearrange("(c p) n -> p c n", p=128), writes=[B_c], key="const2p"))
    ctoks.append(P.dma("pool", ident[:], C.ident, writes=[B_c], key="const2p"))
    c_all = [ctoks[2], ctoks[4]]

    def load_expert(q):
        s = q % NW
        e = q % NE
        P.dma("pool", wg[s][:], C.weg[e].rearrange("(c p) n -> p c n", p=128), writes=[B_w[s]])
        P.dma("pool", wu[s][:], C.weu[e].rearrange("(c p) n -> p c n", p=128), writes=[B_w[s]])
        P.dma("pool", wd[s][:], C.wed[e].rearrange("(c p) n -> p c n", p=128), writes=[B_w[s]])

    LG, OHG, EIN, E2, OH1, OH2, CG = 0, 36, 40, 48, 56, 64, 72
    EX4 = 80
    SC = 96


    load_expert(0)
    load_expert(1)
    for p in range(NPASS):
        x1T, acc, comb = x1T_2[p % NBUF], acc_2[p % NBUF], comb_2[p % NBUF]
        B_x1T, B_acc, B_comb = B_x1T_2[p % NBUF], B_acc_2[p % NBUF], B_comb_2[p % NBUF]
        for t in range(TPT):
            tg = p * TPT + t
            s2 = t % 2
            P.dma("sp", x1t[s2][:], C.x1s[tg * 128:(tg + 1) * 128, :], reads=[C.B_x1s[tg]], writes=[B_x1t[s2]])
            P.op("act", lambda e, s2=s2, t=t: e.activation(out=acc[:, t, :], in_=x1t[s2][:], func=AF.Copy, scale=ALPHA),
                 reads=[B_x1t[s2]], writes=[B_acc[t]])
            P.op("act", lambda e, s2=s2: e.activation(out=x1b[s2][:], in_=x1t[s2][:], func=AF.Copy),
                 reads=[B_x1t[s2]], writes=[B_x1b[s2]])
            bk = t % 2
            tpv = ps[bk][:].bitcast(BF16) if hasattr(ps[bk][:], "bitcast") else None
            P.op("pe", [lambda e, c=c, s2=s2, tpv=tpv: e.transpose(tpv[:, c * 128:(c + 1) * 128], x1b[s2][:, c * 128:(c + 1) * 128], ident[:])
                        for c in range(8)],
                 reads=[B_x1b[s2], B_c], writes=[B_ps[bk]], extra=c_all)
            P.op("dve", lambda e, t=t, tpv=tpv: e.tensor_copy(out=x1T[:, :, t * 128:(t + 1) * 128],
                                                             in_=tpv[:, 0:1024].rearrange("p (c n) -> p c n", c=8)),
                 reads=[B_ps[bk]], writes=[B_x1T[t]])
            rbk = 2 + ((t // 4) % 2)
            tb = t % 4
            P.op("pe", [lambda e, c=c, t=t, rbk=rbk, tb=tb: e.matmul(ps[rbk][:, tb * 36:(tb + 1) * 36], x1T[:, c, t * 128:(t + 1) * 128], wr[:, c, :],
                                                                    start=(c == 0), stop=(c == 7)) for c in range(8)],
                 reads=[B_x1T[t], B_c], writes=[B_ps[rbk]], extra=c_all)
            if tb != 3:
                continue
            t0 = t - 3
            rt = rt4[(t // 4) % 2]
            R = [B_rt4[(t // 4) % 2]]
            NBT = 4
            L3 = rt[:, 0:144].rearrange("p (b k) -> p b k", b=NBT)
            G4 = L3[:, :, 0:4]
            EL = L3[:, :, 4:36].rearrange("p b (g e) -> p b g e", g=4)
            OHG = rt[:, 144:160].rearrange("p (b k) -> p b k", b=NBT)
            D4 = rt[:, 160:176].rearrange("p (b k) -> p b k", b=NBT)
            TMP = rt[:, 176:304].rearrange("p (b g e) -> p b g e", b=NBT, g=4)
            EIN = rt[:, 304:336].rearrange("p (b k) -> p b k", b=NBT)
            E2 = rt[:, 336:368].rearrange("p (b k) -> p b k", b=NBT)
            OH1 = rt[:, 368:400].rearrange("p (b k) -> p b k", b=NBT)
            OH2 = rt[:, 400:432].rearrange("p (b k) -> p b k", b=NBT)
            CG = rt[:, 432:464].rearrange("p (b k) -> p b k", b=NBT)
            T1 = rt[:, 464:496].rearrange("p (b k) -> p b k", b=NBT)
            sc = lambda i, rt=rt: rt[:, 496 + 4 * i:500 + 4 * i]
            bc = lambda ap, k: ap.unsqueeze(2).to_broadcast([128, NBT, k])
            P.op("dve", lambda e, rbk=rbk: e.tensor_tensor(out=L3, in0=ps[rbk][:, 0:144].rearrange("p (b k) -> p b k", b=NBT),
                                                          in1=rb[:].unsqueeze(1).to_broadcast([128, NBT, 36]), op=ALU.add),
                 reads=[B_ps[rbk], B_c], writes=R)
            P.op("dve", lambda e: e.reduce_max(out=sc(0), in_=G4, axis=AX.X), reads=R, writes=R)
            P.op("dve", lambda e: e.tensor_tensor(out=OHG, in0=G4, in1=bc(sc(0), 4), op=ALU.is_equal), reads=R, writes=R)
            P.op("dve", lambda e: e.tensor_tensor(out=D4, in0=G4, in1=bc(sc(0), 4), op=ALU.subtract), reads=R, writes=R)
            P.op("act", lambda e: e.activation(out=D4, in_=D4, func=AF.Exp), reads=R, writes=R)
            P.op("dve", lambda e: e.reduce_sum(out=sc(1), in_=D4, axis=AX.X), reads=R, writes=R)
            P.op("dve", lambda e: e.reciprocal(out=sc(1), in_=sc(1)), reads=R, writes=R)
            P.op("dve", lambda e: e.tensor_tensor(out=TMP, in0=EL, in1=OHG.unsqueeze(3).to_broadcast([128, NBT, 4, 8]), op=ALU.mult), reads=R, writes=R)
            P.op("dve", lambda e: e.reduce_sum(out=EIN, in_=TMP.rearrange("p b g e -> p b e g"), axis=AX.X), reads=R, writes=R)
            P.op("dve", lambda e: e.reduce_max(out=sc(2), in_=EIN, axis=AX.X), reads=R, writes=R)
            P.op("dve", lambda e: e.tensor_tensor(out=OH1, in0=EIN, in1=bc(sc(2), 8), op=ALU.is_equal), reads=R, writes=R)
            P.op("dve", lambda e: e.scalar_tensor_tensor(out=E2, in0=OH1, scalar=-1e30, in1=EIN, op0=ALU.mult, op1=ALU.add), reads=R, writes=R)
            P.op("dve", lambda e: e.reduce_max(out=sc(3), in_=E2, axis=AX.X), reads=R, writes=R)
            P.op("dve", lambda e: e.tensor_tensor(out=OH2, in0=E2, in1=bc(sc(3), 8), op=ALU.is_equal), reads=R, writes=R)
            P.op("dve", lambda e: e.tensor_tensor(out=sc(4), in0=sc(3), in1=sc(2), op=ALU.subtract), reads=R, writes=R)
            P.op("act", lambda e: e.activation(out=sc(4), in_=sc(4), func=AF.Exp), reads=R, writes=R)
            P.op("dve", lambda e: e.tensor_scalar(out=sc(4), in0=sc(4), scalar1=1.0, scalar2=None, op0=ALU.add), reads=R, writes=R)
            P.op("dve", lambda e: e.reciprocal(out=sc(4), in_=sc(4)), reads=R, writes=R)
            P.op("dve", lambda e: e.tensor_tensor(out=sc(5), in0=sc(4), in1=sc(1), op=ALU.mult), reads=R, writes=R)
            P.op("dve", lambda e: e.tensor_tensor(out=sc(6), in0=sc(1), in1=sc(5), op=ALU.subtract), reads=R, writes=R)
            P.op("dve", lambda e: e.tensor_tensor(out=CG, in0=OH1, in1=bc(sc(5), 8), op=ALU.mult), reads=R, writes=R)
            P.op("dve", lambda e: e.tensor_tensor(out=T1, in0=OH2, in1=bc(sc(6), 8), op=ALU.mult), reads=R, writes=R)
            P.op("dve", lambda e: e.tensor_tensor(out=CG, in0=CG, in1=T1, op=ALU.add), reads=R, writes=R)
            P.op("dve", lambda e, t0=t0: e.tensor_tensor(out=comb[:, t0:t0 + NBT, :].rearrange("p b (g e) -> p b g e", g=4),
                                                        in0=OHG.unsqueeze(3).to_broadcast([128, NBT, 4, 8]),
                                                        in1=CG.unsqueeze(2).to_broadcast([128, NBT, 4, 8]), op=ALU.mult),
                 reads=R, writes=B_comb[t0:t0 + NBT])

        if C.dbg:
            for t in range(TPT):
                tg = p * TPT + t
                C.out_toks.append(P.dma("sp", C.dbgc[tg * 128:(tg + 1) * 128, :], comb[:, t, :], reads=[B_comb[t]], writes=[C.B_dbg], key="dbgc"))
        steps = [(e, g) for e in range(NE - 2) for g in range(GP)] + [(e, g) for g in range(GP) for e in (NE - 2, NE - 1)]

        def emit_gu(k):
            e, g = steps[k]
            s = (p * NE + e) % NW
            for j in range(2):
                pr = (2 * k + j) % 2
                bg, bu = 2 * pr, 2 * pr + 1
                P.op("pe", [lambda en, c=c, j=j, s=s, g=g, bg=bg: en.matmul(ps[bg][:], wg[s][:, c, j * 128:(j + 1) * 128],
                                                                          x1T[:, c, g * 512:(g + 1) * 512], start=(c == 0), stop=(c == 7))
                            for c in range(8)],
                     reads=[B_w[s]] + B_x1T[4 * g:4 * g + 4], writes=[B_ps[bg]])
                P.op("pe", [lambda en, c=c, j=j, s=s, g=g, bu=bu: en.matmul(ps[bu][:], wu[s][:, c, j * 128:(j + 1) * 128],
                                                                          x1T[:, c, g * 512:(g + 1) * 512], start=(c == 0), stop=(c == 7))
                            for c in range(8)],
                     reads=[B_w[s]] + B_x1T[4 * g:4 * g + 4], writes=[B_ps[bu]])
                si = (2 * k + j) % 3
                P.op("act", lambda en, bg=bg, si=si: en.activation(out=sg[si][:], in_=ps[bg][:], func=AF.Silu),
                     reads=[B_ps[bg]], writes=[B_sg[si]])
                P.op("dve", lambda en, bu=bu, si=si, k=k, j=j: en.tensor_tensor(out=hT[k % 2][j][:], in0=ps[bu][:], in1=sg[si][:], op=ALU.mult),
                     reads=[B_ps[bu], B_sg[si]], writes=[B_hT[k % 2][j]])

        def emit_down(k):
            e, g = steps[k]
            s = (p * NE + e) % NW
            for tt in range(4):
                t = 4 * g + tt
                pr = (4 * k + tt) % 2
                b0, b1 = 4 + 2 * pr, 5 + 2 * pr
                fns = []
                for n, bk in ((0, b0), (1, b1)):
                    for j in range(2):
                        fns.append(lambda en, n=n, bk=bk, j=j, tt=tt, k=k, s=s: en.matmul(
                            ps[bk][:], hT[k % 2][j][:, tt * 128:(tt + 1) * 128], wd[s][:, j, n * 512:(n + 1) * 512],
                            start=(j == 0), stop=(j == 1)))
                P.op("pe", fns, reads=[B_w[s], B_hT[k % 2][0], B_hT[k % 2][1]], writes=[B_ps[b0], B_ps[b1]])
                for n, bk in ((0, b0), (1, b1)):
                    P.op("dve", lambda en, bk=bk, n=n, t=t, e=e: en.scalar_tensor_tensor(
                        out=acc[:, t, n * 512:(n + 1) * 512], in0=ps[bk][:], scalar=comb[:, t, e:e + 1],
                        in1=acc[:, t, n * 512:(n + 1) * 512], op0=ALU.mult, op1=ALU.add),
                        reads=[B_ps[bk], B_comb[t], B_acc[t]], writes=[B_acc[t]])

        for k in range(len(steps) + 1):
            if k < len(steps):
                emit_gu(k)
            if k >= 1:
                emit_down(k - 1)
            if k < len(steps):
                e, g = steps[k]
                q = p * NE + e
                if g == 0 and q + 2 < NPASS * NE:
                    load_expert(q + 2)

        for t in range(TPT):
            tg = p * TPT + t
            o = t % 2
            stats, mv, B_st = stats_2[t % 2], mv_2[t % 2], B_st2[t % 2]
            for hh in range(2):
                P.op("dve", lambda e, hh=hh, t=t: e.bn_stats(out=stats[:, hh, :], in_=acc[:, t, hh * 512:(hh + 1) * 512]),
                     reads=[B_acc[t]], writes=[B_st])
            P.op("dve", lambda e: e.bn_aggr(out=mv[:, 0:2], in_=stats[:]), reads=[B_st], writes=[B_st])
            P.op("dve", lambda e: e.tensor_scalar(out=mv[:, 2:3], in0=mv[:, 1:2], scalar1=LN_EPS, scalar2=None, op0=ALU.add),
                 reads=[B_st], writes=[B_st])
            P.op("act", lambda e: e.activation(out=mv[:, 2:3], in_=mv[:, 2:3], func=AF.Sqrt), reads=[B_st], writes=[B_st])
            P.op("dve", lambda e: e.reciprocal(out=mv[:, 2:3], in_=mv[:, 2:3]), reads=[B_st], writes=[B_st])
            P.op("dve", lambda e: e.tensor_scalar(out=mv[:, 3:4], in0=mv[:, 0:1], scalar1=-1.0, scalar2=mv[:, 2:3], op0=ALU.mult, op1=ALU.mult),
                 reads=[B_st], writes=[B_st])
            P.op("act", lambda e, t=t, o=o: e.activation(out=ot[o][:], in_=acc[:, t, :], func=AF.Identity, bias=mv[:, 3:4], scale=mv[:, 2:3]),
                 reads=[B_acc[t], B_st], writes=[B_ot[o]])
            P.op("pool", lambda e, o=o: e.tensor_tensor(out=ot[o][:], in0=ot[o][:], in1=ln2w[:], op=ALU.mult),
                 reads=[B_ot[o], B_c], writes=[B_ot[o]], extra=c_all)
            P.op("pool", lambda e, o=o: e.tensor_tensor(out=ot[o][:], in0=ot[o][:], in1=ln2b[:], op=ALU.add),
                 reads=[B_ot[o], B_c], writes=[B_ot[o]], extra=c_all)
            C.out_toks.append(P.dma("sp", C.out[tg * 128:(tg + 1) * 128, :], ot[o][:], reads=[B_ot[o]], writes=[C.B_out]))


QK0, GI0, GF0, V0, O0, AQ0, AK0, AV0, NIN = 0, 1024, 1028, 1032, 1544, 2056, 2568, 2696, 2824
DSC = 128.0 ** -0.5
NG = 16


def phase1(nc, P, C, st):
    sb = lambda n, s, d: st.enter_context(nc.sbuf_tensor("sb_" + n, s, d))
    win = sb("win", [128, 8, NIN], BF16)
    wout = sb("wout", [128, 8, D], BF16)
    xTg = [sb(f"xTg{i}", [128, 8, 512], BF16) for i in range(2)]
    praw = [sb(f"praw{i}", [128, 515], F32) for i in range(2)]
    halo = sb("halo", [128, 8, 3], F32)
    cv = [sb(f"cv{i}", [128, 512], F32) for i in range(2)]
    onecol = sb("onecol", [128, 1], F32)
    qkT2 = [sb(f"qkT{i}", [128, 8, 512], BF16) for i in range(2)]
    ones4 = sb("ones4", [4, 512], F32)
    gA2 = [sb(f"gA{i}", [4, 512], F32) for i in range(2)]
    gF = sb("gF", [4, 512], F32)
    gT = sb("gT", [4, 512], F32)
    gB = sb("gB", [4, 512], F32)
    gM = sb("gM", [4, 512], F32)
    gN2 = [sb(f"gN{i}", [4, 512], F32) for i in range(2)]
    gst = sb("gst", [4, 2], F32)
    M_bc2 = [sb(f"M_bc{i}", [128, 4, 512], F32) for i in range(2)]
    gtm = [sb(f"gtm{i}", [128, 8], F32) for i in range(2)]
    vext = [sb(f"vext{i}", [128, 4, 129], BF16) for i in range(2)]
    vaext = [sb(f"vaext{i}", [128, 2, 65], BF16) for i in range(2)]
    aqs2 = [sb(f"aqs{i}", [128, 512], F32) for i in range(2)]
    aks2 = [sb(f"aks{i}", [128, 256], F32) for i in range(2)]
    rt1 = sb("rt1", [128, 256], F32)
    rt2 = sb("rt2", [128, 256], F32)
    qrot = sb("qrot", [128, 512], BF16)
    krot = sb("krot", [128, 128], BF16)
    qT = sb("qT", [64, 1024], BF16)
    kT = [sb(f"kT{i}", [64, 256], BF16) for i in range(2)]
    eo2 = [sb(f"eo{i}", [128, 512], F32) for i in range(2)]
    WT2 = [sb(f"WT{i}", [128, 512], F32) for i in range(2)]
    WI2 = [sb(f"WI{i}", [128, 512], F32) for i in range(2)]
    PT2 = [sb(f"PT{i}", [128, 512], BF16) for i in range(2)]
    qTw2 = [sb(f"qTw{i}", [128, 512], BF16) for i in range(2)]
    kw2 = [sb(f"kw{i}", [128, 512], BF16) for i in range(2)]
    CT = sb("CT", [128, 4, 129], F32)
    CTb = sb("CTb", [128, 4, 129], BF16)
    hh = sb("hh", [128, 512], F32)
    sm = [sb(f"sm{i}", [128, 16], F32) for i in range(2)]
    sm2 = [sb(f"sm2{i}", [128, 24], F32) for i in range(2)]
    sm3 = [sb(f"sm3{i}", [128, 16], F32) for i in range(2)]
    st4 = sb("st4", [128, 4, 6], F32)
    mv4 = sb("mv4", [128, 4, 2], F32)
    PTa = [sb(f"PTa{i}", [128, 512], BF16) for i in range(4)]
    concat2 = [sb(f"concat{i}", [128, D], BF16) for i in range(2)]
    concatT = sb("concatT", [128, D], BF16)
    r1 = [sb(f"r1{i}", [128, D], F32) for i in range(2)]
    lst = sb("lst", [128, 2, 6], F32)
    lmv = sb("lmv", [128, 4], F32)
    ident = sb("ident1", [128, 128], BF16)
    identf = sb("identf1", [128, 128], F32)
    m128 = sb("m128", [128, 128], BF16)
    mbcur = sb("mbcur", [128, 512], BF16)
    mbprev = sb("mbprev", [128, 512], BF16)
    mbprev0 = sb("mbprev0", [128, 512], BF16)
    sel4 = sb("sel4", [4, 512], F32)
    normw = sb("normw", [128, 512], F32)
    ln1w = sb("ln1w", [128, D], F32)
    ln1b = sb("ln1b", [128, D], F32)
    cw = sb("cw", [128, 8, 4], F32)
    cb = sb("cb", [128, 8], F32)
    esink = sb("esink", [128, 8], F32)
    gbi = sb("gbi", [4, 1], F32)
    gbf = sb("gbf", [4, 1], F32)
    preib = sb("preib", [4, 1], F32)
    prefs = sb("prefs", [4, 1], F32)
    flag128 = sb("flag128", [128, 1], F32)
    ropeb = [sb(f"rope{i}", [128, 64], F32) for i in range(2)]

    ps = [st.enter_context(nc.psum_tensor(f"ps1_{i}", [128, 512], F32)) for i in range(8)]
    B_ps = [Buf(f"ps1_{i}") for i in range(8)]
    import os
    alias = {}
    cfg = "F=0,1,4,5;P=2,3;T=6,7"
    bank_set = {}
    for part in cfg.split(";"):
        k, v = part.split("=")
        if v in ("F", "P", "T"):
            alias[k] = v
        else:
            bank_set[k] = tuple(int(x) for x in v.split(","))
    bank_i = {"F": 0, "P": 0, "T": 0}

    def nb(pool):
        pool = alias.get(pool, pool)
        b = bank_set[pool][bank_i[pool] % len(bank_set[pool])]
        bank_i[pool] += 1
        return b

    mroles = ""
    mrole_map = {kv.split("=")[0]: int(kv.split("=")[1]) for kv in mroles.split(",")} if mroles else None

    def nbm(role):
        if mrole_map is None:
            return nb("F")
        return mrole_map[role]

    B_c = Buf("const1", const=True)
    B_wk, B_wv, B_wq, B_wo, B_wout, B_cid, B_csm, B_ones, B_esink = (Buf(n, const=True) for n in
        ("c_wk", "c_wv", "c_wq", "c_wo", "c_wout", "c_cid", "c_csm", "c_ones", "c_esink"))
    B_xTg = [Buf(f"xTg_{i}") for i in range(2)]
    B_praw = [Buf(f"praw_{i}") for i in range(2)]
    B_halo = [Buf(f"halo_{j}") for j in range(8)]
    B_cv = [Buf(f"cv_{i}") for i in range(2)]
    B_qkT2 = [[Buf(f"qkT_{i}_{j}") for j in range(8)] for i in range(2)]
    B_gF, B_gT, B_gB, B_gM, B_gst = (Buf(n) for n in ("gF", "gT", "gB", "gM", "gst"))
    B_gA2 = [Buf(f"gA_{i}") for i in range(2)]
    B_gN2 = [Buf(f"gN_{i}") for i in range(2)]
    B_Mbc2 = [[Buf(f"Mbc_{i}_{h}") for h in range(4)] for i in range(2)]
    B_gtm = [Buf(f"gtm_{i}") for i in range(2)]
    B_vext = [Buf(f"vext_{i}") for i in range(2)]
    B_vaext = [Buf(f"vaext_{i}") for i in range(2)]
    B_rt1, B_rt2, B_qrot, B_krot, B_qT = (Buf(n) for n in ("rt1", "rt2", "qrot", "krot", "qT"))
    B_aqs2 = [Buf(f"aqs_{i}") for i in range(2)]
    B_aks2 = [Buf(f"aks_{i}") for i in range(2)]
    B_rope = [Buf(f"rope_{i}") for i in range(2)]
    B_kT = [Buf(f"kT_{i}") for i in range(2)]
    B_CT, B_CTb, B_hh = (Buf(n) for n in ("CT", "CTb", "hh"))
    B_WT2, B_WI2, B_PT2, B_qTw2, B_kw2 = ([Buf(f"{n}_{i}") for i in range(2)] for n in ("WT", "WI", "PT", "qTw", "kw"))
    B_eo2 = [Buf(f"eo_{i}") for i in range(2)]
    B_sm = [Buf(f"sm_{i}") for i in range(2)]
    B_sm2 = [Buf(f"sm2_{i}") for i in range(2)]
    B_sm3 = [Buf(f"sm3_{i}") for i in range(2)]
    B_st4, B_mv4, B_lst = Buf("st4"), Buf("mv4"), Buf("lst")
    B_PTa = [Buf(f"PTa_{i}") for i in range(4)]
    B_concat2 = [Buf(f"concat_{i}") for i in range(2)]
    B_concatT = Buf("concatT")
    B_r1 = [Buf(f"r1_{i}") for i in range(2)]

    ck = []
    for (dst, src) in ((identf[:], C.ident), (sel4[:], C.sel4),
                       (normw[:], C.normw), (ln1w[:], C.ln1w), (ln1b[:], C.ln1b), (cw[:], C.cw), (cb[:], C.cb), (esink[:], C.sinks),
                       (gbi[:], C.gb[0:4, :]), (gbf[:], C.gb[4:8, :]), (preib[:], C.preib), (prefs[:], C.prefs), (flag128[:], C.flag128)):
        ck.append(P.dma("sp", dst, src, writes=[B_csm], key="const1", nbytes=65536))
    wsrc = C.win.rearrange("(c p) n -> p c n", p=128)

    def load_x(G):
        s = G % 2
        P.dma("pool", xTg[s][:], C.xT[:, G * 512:(G + 1) * 512].rearrange("(c p) n -> p c n", p=128), writes=[B_xTg[s]])

    P.dma("pool", ident[:], C.ident, writes=[B_cid], key="const1i", nbytes=65536)
    P.dma("pool", win[:, :, 512:1032], wsrc[:, :, 512:1032], writes=[B_wk], key="const1k")
    load_x(0)
    P.dma("pool", win[:, :, V0:V0 + 512], wsrc[:, :, V0:V0 + 512], writes=[B_wv], key="const1v")
    for (dst, src) in ((m128[:], C.mcur[:, 0:128]), (mbcur[:], C.mbcur), (mbprev[:], C.mbprev), (mbprev0[:], C.mbprev0)):
        P.dma("pool", dst, src, writes=[B_cid], key="const1i", nbytes=65536)
    load_x(1)
    P.dma("pool", win[:, :, 0:512], wsrc[:, :, 0:512], writes=[B_wq], key="const1q")
    P.dma("pool", win[:, :, O0:NIN], wsrc[:, :, O0:NIN], writes=[B_wo], key="const1o")
    P.dma("pool", wout[:], C.wout.rearrange("(c p) n -> p c n", p=128), writes=[B_wout], key="const1w")
    call = []
    t_init = []
    t_init.append(P.op("pool", lambda e: e.memset(onecol[:], 1.0), writes=[B_ones]))
    t_init.append(P.op("pool", lambda e: e.memset(ones4[:], 1.0), writes=[B_ones]))
    t_init.append(P.op("pool", lambda e: e.memset(halo[:], 0.0), writes=B_halo))
    t_init.append(P.op("pool", lambda e: e.memset(gst[:], 0.0), writes=[B_gst]))
    for i in range(2):
        t_init.append(P.op("pool", lambda e, i=i: e.memset(M_bc2[i][:], 0.0), writes=B_Mbc2[i]))
    t_init.append(P.op("pool", lambda e: e.memset(CT[:], 0.0), writes=[B_CT]))
    for i in range(2):
        t_init.append(P.op("pool", lambda e, i=i: e.memset(vext[i][:], 1.0), writes=[B_vext[i]]))
        t_init.append(P.op("pool", lambda e, i=i: e.memset(vaext[i][:], 1.0), writes=[B_vaext[i]]))
    t_init.append(P.op("act", lambda e: e.activation(out=esink[:], in_=esink[:], func=AF.Exp), reads=[B_csm], writes=[B_esink]))

    def load_xtm(to):
        s = to % 2
        P.dma("sp", r1[s][:], C.xown[to * 128:(to + 1) * 128, :], writes=[B_r1[s]], nbytes=524288)

    def load_rope(to):
        s = (to + 1) % 2
        P.dma("sp", ropeb[s][:], C.rope[(to + 1) * 128:(to + 2) * 128, :], writes=[B_rope[s]], nbytes=32768)

    cnt = {"praw": 0, "cv": 0, "tile": 0, "E": 0}
    load_xtm(0)
    load_rope(-1)
    load_rope(0)

    def rstd_chain(var_ap, out_ap, Bs):
        P.op("dve", lambda e: e.tensor_scalar(out=out_ap, in0=var_ap, scalar1=LN_EPS, scalar2=None, op0=ALU.add), reads=Bs, writes=Bs)
        P.op("act", lambda e: e.activation(out=out_ap, in_=out_ap, func=AF.Ln), reads=Bs, writes=Bs)
        P.op("act", lambda e: e.activation(out=out_ap, in_=out_ap, func=AF.Exp, scale=-0.5), reads=Bs, writes=Bs)

    for G in range(NG):
        pre = G < 8
        xs = G % 2
        gs, gp = G % 2, (G + 1) % 2
        qkT, B_qkT = qkT2[gs], B_qkT2[gs]
        gA, gN, B_gA, B_gN = gA2[gs], gN2[gs], B_gA2[gs], B_gN2[gs]
        M_bc, B_Mbc = M_bc2[gs], B_Mbc2[gs]
        M_bcp, B_Mbcp = M_bc2[gp], B_Mbc2[gp]
        if 1 <= G and G + 1 < NG:
            load_x(G + 1)
        if G == 8:
            P.op("pool", lambda e: e.tensor_scalar(out=halo[:], in0=halo[:], scalar1=flag128[:, 0:1], scalar2=None, op0=ALU.mult),
                 reads=B_halo + [B_csm], writes=B_halo)
        for j in ((list(range(4, 8)) + ([0, 1, 2, 3] if G == 7 else [])) if pre else range(8)):
            halo_only = pre and j < 4
            bk = nb("F")
            P.op("pe", [lambda e, c=c, j=j, bk=bk: e.matmul(ps[bk][:], win[:, c, j * 128:(j + 1) * 128], xTg[xs][:, c, :],
                                                           start=(c == 0), stop=(c == 7)) for c in range(8)],
                 reads=[B_xTg[xs], B_wk if j >= 4 else B_wq], writes=[B_ps[bk]])
            r = cnt["praw"] % 2
            cnt["praw"] += 1
            P.op("pool", lambda e, r=r, j=j: e.tensor_copy(out=praw[r][:, 0:3], in_=halo[:, j, :]), reads=[B_halo[j]], writes=[B_praw[r]])
            P.op("act", lambda e, r=r, bk=bk: e.activation(out=praw[r][:, 3:515], in_=ps[bk][:], func=AF.Copy),
                 reads=[B_ps[bk]], writes=[B_praw[r]])
            P.op("pool", lambda e, r=r, j=j: e.tensor_copy(out=halo[:, j, :], in_=praw[r][:, 512:515]), reads=[B_praw[r]], writes=[B_halo[j]])
            if halo_only:
                continue
            c2 = cnt["cv"] % 2
            cnt["cv"] += 1
            P.op("act", lambda e, r=r, j=j, c2=c2: e.activation(out=cv[c2][:], in_=praw[r][:, 3:515], func=AF.Identity, scale=cw[:, j, 3:4], bias=cb[:, j:j + 1]),
                 reads=[B_praw[r], B_csm], writes=[B_cv[c2]])
            for tap in (2, 1, 0):
                P.op("dve", lambda e, r=r, j=j, c2=c2, tap=tap: e.scalar_tensor_tensor(out=cv[c2][:], in0=praw[r][:, tap:tap + 512], scalar=cw[:, j, tap:tap + 1],
                                                                                      in1=cv[c2][:], op0=ALU.mult, op1=ALU.add),
                     reads=[B_praw[r], B_cv[c2]], writes=[B_cv[c2]])
            P.op("act", lambda e, j=j, c2=c2: e.activation(out=qkT[:, j, :], in_=cv[c2][:], func=AF.Silu), reads=[B_cv[c2]], writes=[B_qkT[j]])
        bi = nb("F")
        P.op("pe", [lambda e, c=c: e.matmul(ps[bi][0:4, :], win[:, c, GI0:GI0 + 4], xTg[xs][:, c, :], start=(c == 0), stop=(c == 7)) for c in range(8)],
             reads=[B_xTg[xs], B_wk], writes=[B_ps[bi]])
        if pre:
            P.op("dve", lambda e: e.tensor_scalar(out=gA[:], in0=ps[bi][0:4, :], scalar1=gbi[:, 0:1], scalar2=preib[:, 0:1], op0=ALU.add, op1=ALU.add),
                 reads=[B_ps[bi], B_csm], writes=[B_gA])
        else:
            P.op("dve", lambda e: e.tensor_scalar(out=gA[:], in0=ps[bi][0:4, :], scalar1=gbi[:, 0:1], scalar2=None, op0=ALU.add),
                 reads=[B_ps[bi], B_csm], writes=[B_gA])
        bf = nb("F")
        P.op("pe", [lambda e, c=c: e.matmul(ps[bf][0:4, :], win[:, c, GF0:GF0 + 4], xTg[xs][:, c, :], start=(c == 0), stop=(c == 7)) for c in range(8)],
             reads=[B_xTg[xs], B_wk], writes=[B_ps[bf]])
        P.op("dve", lambda e: e.tensor_scalar(out=gF[:], in0=ps[bf][0:4, :], scalar1=gbf[:, 0:1], scalar2=None, op0=ALU.add),
             reads=[B_ps[bf], B_csm], writes=[B_gF])
        P.op("dve", lambda e: e.tensor_scalar(out=gT[:], in0=gF[:], scalar1=-1.0, scalar2=None, op0=ALU.mult), reads=[B_gF], writes=[B_gT])
        P.op("dve", lambda e: e.tensor_tensor(out=gT[:], in0=gT[:], in1=gF[:], op=ALU.max), reads=[B_gF, B_gT], writes=[B_gT])
        P.op("act", lambda e: e.activation(out=gT[:], in_=gT[:], func=AF.Exp, scale=-1.0), reads=[B_gT], writes=[B_gT])
        P.op("dve", lambda e: e.tensor_scalar(out=gT[:], in0=gT[:], scalar1=1.0, scalar2=None, op0=ALU.add), reads=[B_gT], writes=[B_gT])
        P.op("act", lambda e: e.activation(out=gT[:], in_=gT[:], func=AF.Ln), reads=[B_gT], writes=[B_gT])
        P.op("dve", lambda e: e.scalar_tensor_tensor(out=gF[:], in0=gF[:], scalar=0.0, in1=gT[:], op0=ALU.min, op1=ALU.subtract),
             reads=[B_gF, B_gT], writes=[B_gF])
        if pre:
            P.op("dve", lambda e: e.tensor_scalar(out=gF[:], in0=gF[:], scalar1=prefs[:, 0:1], scalar2=None, op0=ALU.mult), reads=[B_gF, B_csm], writes=[B_gF])
        P.op("dve", lambda e: e.tensor_tensor_scan(out=gB[:], data0=ones4[:], data1=gF[:], initial=gst[:, 0:1], op0=ALU.mult, op1=ALU.add),
             reads=[B_gF, B_gst, B_ones], writes=[B_gB])
        P.op("pool", lambda e: e.tensor_tensor(out=gA[:], in0=gA[:], in1=gB[:], op=ALU.subtract), reads=[B_gA, B_gB], writes=[B_gA])
        P.op("dve", lambda e: e.tensor_tensor_scan(out=gM[:], data0=ones4[:], data1=gA[:], initial=gst[:, 1:2], op0=ALU.mult, op1=ALU.max),
             reads=[B_gA, B_gst, B_ones], writes=[B_gM])
        P.op("pool", lambda e: e.tensor_copy(out=gst[:, 0:1], in_=gB[:, 511:512]), reads=[B_gB], writes=[B_gst])
        P.op("pool", lambda e: e.tensor_copy(out=gst[:, 1:2], in_=gM[:, 511:512]), reads=[B_gM], writes=[B_gst])
        P.op("dve", lambda e: e.scalar_tensor_tensor(out=gN[:], in0=gB[:], scalar=-1.0, in1=gM[:], op0=ALU.mult, op1=ALU.subtract),
             reads=[B_gB, B_gM], writes=[B_gN])
        for h in range(4):
            bk = nb("F")
            if pre:
                P.op("pe", lambda e, h=h, bk=bk: e.matmul(ps[bk][:, 0:4], sel4[:, h * 128:(h + 1) * 128], gM[:, 127:512:128], start=True, stop=True),
                     reads=[B_gM, B_csm], writes=[B_ps[bk]], n=16)
                P.op("act", lambda e, h=h, bk=bk: e.activation(out=M_bc[:, h, 127:512:128], in_=ps[bk][:, 0:4], func=AF.Copy), reads=[B_ps[bk]], writes=[B_Mbc[h]])
            else:
                P.op("pe", lambda e, h=h, bk=bk: e.matmul(ps[bk][:], sel4[:, h * 128:(h + 1) * 128], gM[:], start=True, stop=True),
                     reads=[B_gM, B_csm], writes=[B_ps[bk]], n=2048)
                P.op("act", lambda e, h=h, bk=bk: e.activation(out=M_bc[:, h, :], in_=ps[bk][:], func=AF.Copy), reads=[B_ps[bk]], writes=[B_Mbc[h]])

        for t in range(4):
            cols = slice(t * 128, (t + 1) * 128)
            to = (G - 8) * 4 + t
            halo_tile = (G == 7 and t == 3)
            own = not pre
            ti = cnt["tile"]
            cnt["tile"] += 1
            vs = ti % 2
            ts_ = ti % 2
            eo, B_eo = eo2[ti % 2], B_eo2[ti % 2]
            aqs, B_aqs = aqs2[ti % 2], B_aqs2[ti % 2]
            aks, B_aks = aks2[ti % 2], B_aks2[ti % 2]
            concat, B_concat = concat2[ti % 2], B_concat2[ti % 2]
            WT, WI, PT, qTw, kw = WT2[ti % 2], WI2[ti % 2], PT2[ti % 2], qTw2[ti % 2], kw2[ti % 2]
            B_WT, B_WI, B_PT, B_qTw, B_kw = B_WT2[ti % 2], B_WI2[ti % 2], B_PT2[ti % 2], B_qTw2[ti % 2], B_kw2[ti % 2]
            bv = nb("P")
            fns = []
            wr_ = [B_ps[bv]]
            if own:
                bo = nb("P")
                wr_.append(B_ps[bo])
            for c in range(8):
                fns.append(lambda e, c=c, bv=bv: e.matmul(ps[bv][:], xTg[xs][:, c, cols], win[:, c, V0:V0 + 512], start=(c == 0), stop=(c == 7)))
                if own:
                    fns.append(lambda e, c=c, bo=bo: e.matmul(ps[bo][:], xTg[xs][:, c, cols], win[:, c, O0:O0 + 512], start=(c == 0), stop=(c == 7)))
            P.op("pe", fns, reads=[B_xTg[xs], B_wv, B_wo], writes=wr_)
            if own:
                P.op("dve", lambda e, bv=bv, vs=vs: e.tensor_copy(out=vext[vs][:, :, 0:128], in_=ps[bv][:].rearrange("p (h n) -> p h n", h=4)),
                     reads=[B_ps[bv]], writes=[B_vext[vs]])
            else:
                P.op("act", lambda e, bv=bv, vs=vs: e.activation(out=vext[vs][:, :, 0:128], in_=ps[bv][:].rearrange("p (h n) -> p h n", h=4), func=AF.Copy),
                     reads=[B_ps[bv]], writes=[B_vext[vs]])
            if own:
                P.op("act", lambda e, bo=bo: e.activation(out=eo[:], in_=ps[bo][:], func=AF.Exp, scale=-1.0), reads=[B_ps[bo]], writes=[B_eo])
                P.op("act", lambda e: e.activation(out=eo[:], in_=eo[:], func=AF.Ln, bias=onecol[:, 0:1], scale=1.0), reads=[B_eo, B_ones], writes=[B_eo])
                P.op("act", lambda e: e.activation(out=eo[:], in_=eo[:], func=AF.Exp, scale=-1.0), reads=[B_eo], writes=[B_eo])
            if own or halo_tile:
                fns = []
                wr_ = []
                bkv = nb("P")
                wr_.append(B_ps[bkv])
                if own:
                    bq = nb("P")
                    wr_.append(B_ps[bq])
                for c in range(8):
                    if own:
                        fns.append(lambda e, c=c, bq=bq: e.matmul(ps[bq][:], xTg[xs][:, c, cols], win[:, c, AQ0:AQ0 + 512], start=(c == 0), stop=(c == 7)))
                    fns.append(lambda e, c=c, bkv=bkv: e.matmul(ps[bkv][:, 0:256], xTg[xs][:, c, cols], win[:, c, AK0:AK0 + 256], start=(c == 0), stop=(c == 7)))
                P.op("pe", fns, reads=[B_xTg[xs], B_wo], writes=wr_)
                if own:
                    P.op("dve", lambda e, bq=bq: e.tensor_copy(out=aqs[:], in_=ps[bq][:]), reads=[B_ps[bq]], writes=[B_aqs])
                P.op("act", lambda e, bkv=bkv: e.activation(out=aks[:], in_=ps[bkv][:, 0:256], func=AF.Copy), reads=[B_ps[bkv]], writes=[B_aks], n=256)

            bg = nbm("B")
            P.op("pe", [lambda e, bg=bg: e.matmul(ps[bg][:, 0:4], gA[:, cols], identf[0:4, 0:4], start=True, stop=True),
                        lambda e, bg=bg: e.matmul(ps[bg][:, 4:8], gN[:, cols], identf[0:4, 0:4], start=True, stop=True)],
                 reads=[B_gA, B_gN, B_csm], writes=[B_ps[bg]], n=8)
            P.op("dve", lambda e, bg=bg: e.tensor_copy(out=gtm[ts_][:], in_=ps[bg][:, 0:8]), reads=[B_ps[bg]], writes=[B_gtm[ts_]], n=8)
            ccol = t * 128 + 127
            Mp = M_bcp[:, :, 511] if t == 0 else M_bc[:, :, t * 128 - 1]
            B_Mp = B_Mbcp if t == 0 else B_Mbc

            if own:
                bs = nbm("S")
                P.op("pe", [lambda e, h=h, bs=bs: e.matmul(ps[bs][:, h * 128:(h + 1) * 128], qkT[:, 4 + h, cols], qkT[:, h, cols], start=True, stop=True)
                            for h in range(4)], reads=B_qkT, writes=[B_ps[bs]])
                for h in range(4):
                    P.op("act", lambda e, h=h: e.activation(out=WT[:, h * 128:(h + 1) * 128], in_=M_bc[:, h, cols], func=AF.Exp, scale=-1.0,
                                                            bias=gtm[ts_][:, h:h + 1]),
                         reads=[B_Mbc[h], B_gtm[ts_]], writes=[B_WT])
                P.op("pool", lambda e: e.tensor_tensor(out=WT[:].rearrange("p (h n) -> p h n", h=4), in0=WT[:].rearrange("p (h n) -> p h n", h=4),
                                                      in1=m128[:].unsqueeze(1).to_broadcast([128, 4, 128]), op=ALU.mult), reads=[B_WT, B_cid], writes=[B_WT])
                P.op("dve", lambda e, bs=bs: e.scalar_tensor_tensor(out=PT[:], in0=ps[bs][:], scalar=DSC, in1=WT[:], op0=ALU.mult, op1=ALU.mult),
                     reads=[B_ps[bs], B_WT], writes=[B_PT])
                for h in range(4):
                    mp_h = M_bcp[:, h, 511:512] if t == 0 else M_bc[:, h, t * 128 - 1:t * 128]
                    P.op("act", lambda e, h=h, mp_h=mp_h: e.activation(out=WI[:, h * 128:(h + 1) * 128], in_=M_bc[:, h, cols], func=AF.Exp, scale=-1.0, bias=mp_h),
                         reads=[B_Mbc[h]] + B_Mp, writes=[B_WI])
                P.op("pool", lambda e: e.tensor_tensor(out=qTw[:].rearrange("p (h n) -> p h n", h=4), in0=qkT[:, 0:4, cols],
                                                      in1=WI[:].rearrange("p (h n) -> p h n", h=4), op=ALU.mult),
                     reads=B_qkT[0:4] + [B_WI], writes=[B_qTw])
                bn = [nbm("C"), nbm("D")]
                fns = []
                for h in range(4):
                    o_ = (h % 2) * 129
                    fns.append(lambda e, h=h, o_=o_: e.matmul(ps[bn[h // 2]][:, o_:o_ + 129], qTw[:, h * 128:(h + 1) * 128], CTb[:, h, :], start=True, stop=False))
                    fns.append(lambda e, h=h, o_=o_: e.matmul(ps[bn[h // 2]][:, o_:o_ + 129], PT[:, h * 128:(h + 1) * 128], vext[vs][:, h, :], start=False, stop=True))
                P.op("pe", fns, reads=[B_qTw, B_CTb, B_PT, B_vext[vs]], writes=[B_ps[bn[0]], B_ps[bn[1]]])
                s2 = sm2[ts_]
                S2 = [B_sm2[ts_]]
                P.op("act", lambda e, s2=s2: e.activation(out=s2[:, 0:4], in_=gtm[ts_][:, 4:8], func=AF.Exp), reads=[B_gtm[ts_]], writes=S2)
                for b in range(2):
                    P.op("dve", lambda e, b=b, s2=s2: e.tensor_scalar(out=s2[:, 20 + 2 * b:22 + 2 * b], in0=ps[bn[b]][:, 128:258:129], scalar1=-1.0, scalar2=None, op0=ALU.mult),
                         reads=[B_ps[bn[b]]] + S2, writes=S2)
                    P.op("dve", lambda e, b=b, s2=s2: e.tensor_tensor(out=s2[:, 20 + 2 * b:22 + 2 * b], in0=s2[:, 20 + 2 * b:22 + 2 * b], in1=ps[bn[b]][:, 128:258:129], op=ALU.max),
                         reads=[B_ps[bn[b]]] + S2, writes=S2)
                    P.op("dve", lambda e, b=b, s2=s2: e.tensor_tensor(out=s2[:, 4 + 2 * b:6 + 2 * b], in0=s2[:, 20 + 2 * b:22 + 2 * b], in1=s2[:, 2 * b:2 * b + 2], op=ALU.max),
                         reads=S2, writes=S2)
                P.op("dve", lambda e, s2=s2: e.reciprocal(out=s2[:, 8:12], in_=s2[:, 4:8]), reads=S2, writes=S2)
                for h in range(4):
                    o_ = (h % 2) * 129
                    P.op("act", lambda e, h=h, o_=o_, s2=s2: e.activation(out=hh[:, h * 128:(h + 1) * 128], in_=ps[bn[h // 2]][:, o_:o_ + 128], func=AF.Identity,
                                                                         scale=s2[:, 8 + h:9 + h]),
                         reads=[B_ps[bn[h // 2]]] + S2, writes=[B_hh])
                for h in range(4):
                    P.op("dve", lambda e, h=h: e.bn_stats(out=st4[:, h, :], in_=hh[:, h * 128:(h + 1) * 128]), reads=[B_hh], writes=[B_st4])
                for h in range(4):
                    P.op("dve", lambda e, h=h: e.bn_aggr(out=mv4[:, h, :], in_=st4[:, h, :]), reads=[B_st4], writes=[B_mv4])
                rstd_chain(mv4[:, :, 1], s2[:, 12:16], S2 + [B_mv4])
                P.op("dve", lambda e, s2=s2: e.scalar_tensor_tensor(out=s2[:, 16:20], in0=mv4[:, :, 0], scalar=-1.0, in1=s2[:, 12:16], op0=ALU.mult, op1=ALU.mult),
                     reads=S2 + [B_mv4], writes=S2)
                for h in range(4):
                    P.op("act", lambda e, h=h, s2=s2: e.activation(out=hh[:, h * 128:(h + 1) * 128], in_=hh[:, h * 128:(h + 1) * 128], func=AF.Identity,
                                                                 scale=s2[:, 12 + h:13 + h], bias=s2[:, 16 + h:17 + h]),
                         reads=[B_hh] + S2, writes=[B_hh])
                P.op("pool", lambda e: e.tensor_tensor(out=hh[:], in0=hh[:], in1=normw[:], op=ALU.mult), reads=[B_hh, B_csm], writes=[B_hh])
                P.op("dve", lambda e: e.tensor_tensor(out=concat[:, 0:512], in0=hh[:], in1=eo[:], op=ALU.mult), reads=[B_hh, B_eo], writes=[B_concat])

            btp = nbm("B")
            tpv = ps[btp][:].bitcast(BF16)[:, 512:1024]
            P.op("pe", [lambda e, h=h, tpv=tpv: e.transpose(tpv[:, h * 128:(h + 1) * 128], qkT[:, 4 + h, cols], ident[:]) for h in range(4)],
                 reads=B_qkT[4:8] + [B_cid], writes=[B_ps[btp]], n=128)
            s1 = sm[ts_]
            S1 = [B_sm[ts_]]
            P.op("dve", lambda e, s1=s1: e.tensor_tensor(out=s1[:, 0:4], in0=gtm[ts_][:, 0:4], in1=M_bc[:, :, ccol], op=ALU.subtract),
                 reads=[B_gtm[ts_]] + B_Mbc, writes=S1)
            P.op("dve", lambda e, s1=s1, Mp=Mp: e.tensor_tensor(out=s1[:, 4:8], in0=Mp, in1=M_bc[:, :, ccol], op=ALU.subtract),
                 reads=B_Mp + B_Mbc, writes=S1)
            P.op("act", lambda e, s1=s1: e.activation(out=s1[:, 8:16], in_=s1[:, 0:8], func=AF.Exp), reads=S1, writes=S1)
            for h in range(4):
                P.op("dve", lambda e, h=h, s1=s1, tpv=tpv: e.tensor_scalar(out=kw[:, h * 128:(h + 1) * 128], in0=tpv[:, h * 128:(h + 1) * 128],
                                                                          scalar1=s1[:, 8 + h:9 + h], scalar2=DSC, op0=ALU.mult, op1=ALU.mult),
                     reads=[B_ps[btp]] + S1, writes=[B_kw], n=128)
            bu = [nbm("C"), nbm("D")]
            fns = []
            for h in range(4):
                o_ = (h % 2) * 129
                fns.append(lambda e, h=h, o_=o_: e.matmul(ps[bu[h // 2]][:, o_:o_ + 129], kw[:, h * 128:(h + 1) * 128], vext[vs][:, h, :], start=True, stop=True))
            P.op("pe", fns, reads=[B_kw, B_vext[vs]], writes=[B_ps[bu[0]], B_ps[bu[1]]])
            for h in range(4):
                o_ = (h % 2) * 129
                P.op("dve", lambda e, h=h, o_=o_, s1=s1: e.scalar_tensor_tensor(out=CT[:, h, :], in0=CT[:, h, :], scalar=s1[:, 12 + h:13 + h],
                                                                               in1=ps[bu[h // 2]][:, o_:o_ + 129], op0=ALU.mult, op1=ALU.add),
                     reads=[B_CT, B_ps[bu[h // 2]]] + S1, writes=[B_CT])
            if own or halo_tile:
                P.op("pool", lambda e: e.tensor_copy(out=CTb[:], in_=CT[:]), reads=[B_CT], writes=[B_CTb])

            if own or halo_tile:
                ks = (to + 1) % 2
                kp = to % 2
                rsl = (to + 1) % 2
                cosb = lambda n: ropeb[rsl][:, 0:32].unsqueeze(1).to_broadcast([128, n, 32])
                sinb = lambda n: ropeb[rsl][:, 32:64].unsqueeze(1).to_broadcast([128, n, 32])

                def do_rope(src, dst, n, Bsrc, Bdst):
                    v = src.rearrange("p (h t d) -> p h t d", h=n, t=2)
                    o = dst.rearrange("p (h t d) -> p h t d", h=n, t=2)
                    a = rt1[:, 0:n * 32].rearrange("p (h d) -> p h d", h=n)
                    b = rt2[:, 0:n * 32].rearrange("p (h d) -> p h d", h=n)
                    cn, sn = cosb(n), sinb(n)
                    P.op("pool", lambda e: e.tensor_tensor(out=a, in0=v[:, :, 0, :], in1=cn, op=ALU.mult), reads=[Bsrc, B_rope[rsl]], writes=[B_rt1])
                    P.op("pool", lambda e: e.tensor_tensor(out=b, in0=v[:, :, 1, :], in1=sn, op=ALU.mult), reads=[Bsrc, B_rope[rsl]], writes=[B_rt2])
                    P.op("pool", lambda e: e.tensor_tensor(out=o[:, :, 0, :], in0=a, in1=b, op=ALU.subtract), reads=[B_rt1, B_rt2], writes=[Bdst])
                    P.op("pool", lambda e: e.tensor_tensor(out=a, in0=v[:, :, 0, :], in1=sn, op=ALU.mult), reads=[Bsrc, B_rope[rsl]], writes=[B_rt1])
                    P.op("pool", lambda e: e.tensor_tensor(out=b, in0=v[:, :, 1, :], in1=cn, op=ALU.mult), reads=[Bsrc, B_rope[rsl]], writes=[B_rt2])
                    P.op("pool", lambda e: e.tensor_tensor(out=o[:, :, 1, :], in0=a, in1=b, op=ALU.add), reads=[B_rt1, B_rt2], writes=[Bdst])

                do_rope(aks[:, 0:128], krot[:], 2, B_aks, B_krot)
                bk2 = nb("T")
                tk = ps[bk2][:].bitcast(BF16)
                P.op("pe", [lambda e, j=j, tk=tk: e.transpose(tk[0:64, j * 128:(j + 1) * 128], krot[:, j * 64:(j + 1) * 64], ident[:]) for j in range(2)],
                     reads=[B_krot, B_cid], writes=[B_ps[bk2]])
                P.op("act", lambda e, tk=tk, ks=ks: e.activation(out=kT[ks][:], in_=tk[0:64, 0:256], func=AF.Copy), reads=[B_ps[bk2]], writes=[B_kT[ks]])
                P.op("pool", lambda e, ks=ks: e.tensor_copy(out=vaext[ks][:, :, 0:64], in_=aks[:, 128:256].rearrange("p (h d) -> p h d", h=2)),
                     reads=[B_aks], writes=[B_vaext[ks]])
            if own:
                do_rope(aqs[:], qrot[:], 8, B_aqs, B_qrot)
                bq2 = nb("T")
                tq = ps[bq2][:].bitcast(BF16)
                P.op("pe", [lambda e, hq=hq, tq=tq: e.transpose(tq[0:64, hq * 128:(hq + 1) * 128], qrot[:, hq * 64:(hq + 1) * 64], ident[:]) for hq in range(8)],
                     reads=[B_qrot, B_cid], writes=[B_ps[bq2]])
                P.op("act", lambda e, tq=tq: e.activation(out=qT[:], in_=tq[0:64, 0:1024], func=AF.Copy), reads=[B_ps[bq2]], writes=[B_qT])
                for j in range(2):
                    for kb in range(2):
                        slot = kp if kb == 0 else ks
                        bsc = nb("T")
                        msk = mbcur if kb == 1 else (mbprev0 if to == 0 else mbprev)
                        P.op("pe", [lambda e, j=j, slot=slot, bsc=bsc: e.matmul(ps[bsc][:], kT[slot][:, j * 128:(j + 1) * 128], qT[:, j * 512:(j + 1) * 512], start=True, stop=False),
                                    lambda e, bsc=bsc, msk=msk: e.matmul(ps[bsc][:], ident[:], msk[:], start=False, stop=True)],
                             reads=[B_kT[slot], B_qT, B_cid], writes=[B_ps[bsc]])
                        P.op("act", lambda e, bsc=bsc, j=j, kb=kb: e.activation(out=PTa[2 * j + kb][:], in_=ps[bsc][:], func=AF.Exp, scale=0.125),
                             reads=[B_ps[bsc]], writes=[B_PTa[2 * j + kb]])
                bpv = [nb("T"), nb("T")]
                for j in range(2):
                    fns = []
                    for g in range(4):
                        fns.append(lambda e, j=j, g=g: e.matmul(ps[bpv[j]][:, g * 65:(g + 1) * 65], PTa[2 * j][:, g * 128:(g + 1) * 128], vaext[kp][:, j, :], start=True, stop=False))
                        fns.append(lambda e, j=j, g=g: e.matmul(ps[bpv[j]][:, g * 65:(g + 1) * 65], PTa[2 * j + 1][:, g * 128:(g + 1) * 128], vaext[ks][:, j, :], start=False, stop=True))
                    P.op("pe", fns, reads=[B_PTa[2 * j], B_PTa[2 * j + 1], B_vaext[kp], B_vaext[ks]], writes=[B_ps[bpv[j]]])
                s3 = sm3[ts_]
                S3 = [B_sm3[ts_]]
                for j in range(2):
                    P.op("dve", lambda e, j=j, s3=s3: e.tensor_tensor(out=s3[:, 4 * j:4 * j + 4], in0=ps[bpv[j]][:, 64:260:65], in1=esink[:, 4 * j:4 * j + 4], op=ALU.add),
                         reads=[B_ps[bpv[j]], B_esink], writes=S3)
                P.op("dve", lambda e, s3=s3: e.reciprocal(out=s3[:, 8:16], in_=s3[:, 0:8]), reads=S3, writes=S3)
                for j in range(2):
                    P.op("dve", lambda e, j=j, s3=s3: e.tensor_tensor(out=concat[:, 512 + 256 * j:768 + 256 * j].rearrange("p (g d) -> p g d", g=4),
                                                                     in0=ps[bpv[j]][:, 0:260].rearrange("p (g d) -> p g d", g=4)[:, :, 0:64],
                                                                     in1=s3[:, 8 + 4 * j:12 + 4 * j].unsqueeze(2).to_broadcast([128, 4, 64]), op=ALU.mult),
                         reads=[B_ps[bpv[j]]] + S3, writes=[B_concat])

                bct = nb("T")
                tcv = ps[bct][:].bitcast(BF16)
                P.op("pe", [lambda e, c=c, tcv=tcv: e.transpose(tcv[:, c * 128:(c + 1) * 128], concat[:, c * 128:(c + 1) * 128], ident[:]) for c in range(8)],
                     reads=[B_concat, B_cid], writes=[B_ps[bct]])
                P.op("act", lambda e, tcv=tcv: e.activation(out=concatT[:], in_=tcv[:, 0:1024], func=AF.Copy), reads=[B_ps[bct]], writes=[B_concatT])
                by = [nb("T"), nb("T")]
                fns = []
                for n in range(2):
                    for c in range(8):
                        fns.append(lambda e, n=n, c=c: e.matmul(ps[by[n]][:], concatT[:, c * 128:(c + 1) * 128], wout[:, c, n * 512:(n + 1) * 512], start=(c == 0), stop=(c == 7)))
                P.op("pe", fns, reads=[B_concatT, B_wout], writes=[B_ps[by[0]], B_ps[by[1]]])
                rs = to % 2
                for n in range(2):
                    P.op("dve", lambda e, n=n, rs=rs: e.scalar_tensor_tensor(out=r1[rs][:, n * 512:(n + 1) * 512], in0=r1[rs][:, n * 512:(n + 1) * 512], scalar=ALPHA,
                                                                            in1=ps[by[n]][:], op0=ALU.mult, op1=ALU.add),
                         reads=[B_r1[rs], B_ps[by[n]]], writes=[B_r1[rs]])
                for n in range(2):
                    P.op("dve", lambda e, n=n, rs=rs: e.bn_stats(out=lst[:, n, :], in_=r1[rs][:, n * 512:(n + 1) * 512]), reads=[B_r1[rs]], writes=[B_lst])
                P.op("dve", lambda e: e.bn_aggr(out=lmv[:, 0:2], in_=lst[:]), reads=[B_lst], writes=[B_lst])
                rstd_chain(lmv[:, 1:2], lmv[:, 2:3], [B_lst])
                P.op("dve", lambda e: e.tensor_scalar(out=lmv[:, 3:4], in0=lmv[:, 0:1], scalar1=-1.0, scalar2=lmv[:, 2:3], op0=ALU.mult, op1=ALU.mult),
                     reads=[B_lst], writes=[B_lst])
                P.op("act", lambda e, rs=rs: e.activation(out=r1[rs][:], in_=r1[rs][:], func=AF.Identity, bias=lmv[:, 3:4], scale=lmv[:, 2:3]),
                     reads=[B_r1[rs], B_lst], writes=[B_r1[rs]])
                P.op("pool", lambda e, rs=rs: e.tensor_tensor(out=r1[rs][:], in0=r1[rs][:], in1=ln1w[:], op=ALU.mult), reads=[B_r1[rs], B_csm], writes=[B_r1[rs]])
                P.op("pool", lambda e, rs=rs: e.tensor_tensor(out=r1[rs][:], in0=r1[rs][:], in1=ln1b[:], op=ALU.add), reads=[B_r1[rs], B_csm], writes=[B_r1[rs]])
                if to + 1 < NT:
                    load_xtm(to + 1)
                    load_rope(to + 1)
                tok = P.dma("sp", C.x1s[to * 128:(to + 1) * 128, :], r1[rs][:], reads=[B_r1[rs]], writes=[C.B_x1s[to]], key=f"x1st{rs}")
                C.x1_toks.append(tok)


def build(mode="full"):
    nc = bass.Bass("TRN2", target_bir_lowering=False)
    C = Ctx()
    dt = lambda n, s, d=F32, kind="ExternalInput": nc.dram_tensor(n, s, d, kind=kind).ap()
    C.dbg = False
    if mode != "p2":
        C.xT = dt("xT", [D, 2 * T])
        C.xown = dt("xown", [T, D])
        C.win = dt("win", [D, NIN])
        C.wout = dt("wout", [D, D])
        C.mcur = dt("mcur", [128, 512])
        C.mbcur = dt("mbcur", [128, 512])
        C.mbprev = dt("mbprev", [128, 512])
        C.mbprev0 = dt("mbprev0", [128, 512])
        C.sel4 = dt("sel4", [4, 512])
        C.normw = dt("normw", [128, 512])
        C.ln1w = dt("ln1w", [128, D])
        C.ln1b = dt("ln1b", [128, D])
        C.cw = dt("cw", [128, 8, 4])
        C.cb = dt("cb", [128, 8])
        C.sinks = dt("sinks", [128, 8])
        C.gb = dt("gb", [8, 1])
        C.preib = dt("preib", [4, 1])
        C.prefs = dt("prefs", [4, 1])
        C.flag128 = dt("flag128", [128, 1])
        C.rope = dt("rope", [(NT + 1) * 128, 64])
    C.ident = dt("ident", [128, 128])
    if mode != "p1":
        C.ln2w = dt("ln2w", [128, D])
        C.ln2b = dt("ln2b", [128, D])
        C.rb = dt("rb", [128, 36])
        C.wr = dt("wr", [D, 36])
        C.weg = dt("weg", [NE, D, DE])
        C.weu = dt("weu", [NE, D, DE])
        C.wed = dt("wed", [NE, DE, D])
        C.out = dt("out", [T, D], kind="ExternalOutput")
    C.x1s = dt("x1s", [T, D], kind={"full": "Internal", "p1": "ExternalOutput", "p2": "ExternalInput"}[mode])
    C.B_x1s = [Buf(f"dram:x1s_{t}") for t in range(NT)]
    C.B_out = Buf("dram:out")
    C.out_toks = []
    C.x1_toks = []
    with ExitStack() as st:
        P = Prog(nc, st)
        if mode != "p2":
            with ExitStack() as st1:
                phase1(nc, P, C, st1)
                P.barrier()
        if mode != "p1":
            with ExitStack() as st2:
                phase2(nc, P, C, st2)
        P.wait("sp", C.out_toks + C.x1_toks)
        P.run()
    return nc


def _rep(v, n=128):
    return np.ascontiguousarray(np.broadcast_to(np.asarray(v, np.float32)[None, :], (n, v.shape[0])))


def host_inputs(inp, mode="full"):
    f32 = np.float32
    x = np.asarray(inp["x"], f32)
    w_in = np.asarray(inp["w_in"], f32)[0]
    qk, v, o, gi, gf, aq, ak, av = np.split(w_in, [1024, 1536, 2048, 2052, 2056, 2568, 2696], axis=1)
    win = np.ascontiguousarray(np.concatenate([qk, gi, gf, v, o, aq, ak, av], axis=1))
    conv_w = np.asarray(inp["conv_w"], f32)[0]
    cw = np.ascontiguousarray(conv_w.T.reshape(8, 128, 4).transpose(1, 0, 2))
    cb = np.ascontiguousarray(np.asarray(inp["conv_b"], f32)[0].reshape(8, 128).T)
    gb = np.ascontiguousarray(np.asarray(inp["mlstm_gate_bias"], f32)[0].reshape(8, 1))
    kq = np.arange(128)
    m = (kq[:, None] <= kq[None, :]).astype(f32)
    mcur = np.ascontiguousarray(np.tile(m, (1, 4)))
    NEG = -30000.0
    mbcur = np.ascontiguousarray((1.0 - mcur) * NEG)
    mbprev = np.ascontiguousarray(mcur * NEG)
    sel4 = np.zeros((4, 4, 128), f32)
    for h in range(4):
        sel4[h, h, :] = 1.0
    sel4 = sel4.reshape(4, 512)
    inv_freq = (10000.0 ** (-np.arange(32, dtype=f32) / 32.0)).astype(f32)
    wr = np.concatenate([np.asarray(inp["w_group_router"], f32)[0], np.asarray(inp["w_expert_router"], f32)[0]], axis=1)
    rbv = np.concatenate([np.asarray(inp["b_group_router"], f32)[0], np.asarray(inp["b_expert_router"], f32)[0]], axis=0)
    common = dict(ident=np.eye(128, dtype=f32))
    if mode != "p2":
        common.update(win=win, wout=np.ascontiguousarray(np.asarray(inp["w_out"], f32)[0]), mcur=mcur, mbcur=mbcur, mbprev=mbprev, sel4=sel4,
                      normw=_rep(np.asarray(inp["mlstm_norm_w"], f32)[0]), ln1w=_rep(np.asarray(inp["ln1_w"], f32)[0]),
                      ln1b=_rep(np.asarray(inp["ln1_b"], f32)[0]), cw=cw, cb=cb, sinks=_rep(np.asarray(inp["attn_sinks"], f32)[0]), gb=gb)
    if mode != "p1":
        common.update(ln2w=_rep(np.asarray(inp["ln2_w"], f32)[0]), ln2b=_rep(np.asarray(inp["ln2_b"], f32)[0]), rb=_rep(rbv),
                      wr=np.ascontiguousarray(wr), weg=np.asarray(inp["w_exp_gate"], f32)[0], weu=np.asarray(inp["w_exp_up"], f32)[0],
                      wed=np.asarray(inp["w_exp_down"], f32)[0])
    maps = []
    for c in range(NCORES):
        b, h = c // 2, c % 2
        d = dict(common)
        if mode != "p2":
            own = x[b, h * T:(h + 1) * T]
            pre = x[b, 0:T]
            d["xT"] = np.ascontiguousarray(np.concatenate([pre, own], axis=0).T)
            d["xown"] = np.ascontiguousarray(own)
            d["mbprev0"] = mbprev if h == 1 else np.full_like(mbprev, NEG)
            d["preib"] = np.full((4, 1), 0.0 if h == 1 else -1e30, f32)
            d["prefs"] = np.full((4, 1), 1.0 if h == 1 else 0.0, f32)
            d["flag128"] = np.full((128, 1), 1.0 if h == 1 else 0.0, f32)
            pos = (np.arange(-128, T) + h * T).astype(f32)
            ang = pos[:, None] * inv_freq[None, :]
            d["rope"] = np.ascontiguousarray(np.concatenate([np.cos(ang), np.sin(ang)], axis=1).astype(f32))
        maps.append(d)
    return maps


_NC_CACHE = {}


def kernel(**inputs):
    if "full" not in _NC_CACHE:
        _NC_CACHE["full"] = build("full")
    nc = _NC_CACHE["full"]
    maps = host_inputs(inputs, "full")
    res = run_bass_kernel_spmd(nc, maps, core_ids=list(range(NCORES)))
    out = np.stack([np.asarray(r["out"], np.float32) for r in res.results])
    return np.ascontiguousarray(out.reshape(4, 2 * T, D))
```
